# Optimizing a Trainium2 kernel written in Bass

```python
import math
import jax, jax.numpy as jnp
from jax import lax
import numpy as np

D_MODEL = 1024
BATCH = 2
SEQ = 8192
DEPTH = 4

N_MEM = 256
SSM_GROUP_CH = 16
SSM_STATE = 64
SSM_WIDTH = D_MODEL // 2
SSM_GROUPS = SSM_WIDTH // SSM_GROUP_CH
GMLP_WIDTH = D_MODEL // 2
GMLP_GROUPS = 8
GMLP_GROUP_CH = GMLP_WIDTH // GMLP_GROUPS
CHUNK = 128
MEM_HEADS = 4
MEM_WIDTH = D_MODEL // 2
MEM_HEAD_DIM = MEM_WIDTH // MEM_HEADS
N_BRANCH = 3
IN_WIDTH = SSM_WIDTH + 2 * GMLP_WIDTH + MEM_WIDTH
BRANCH_WIDTH = 512
N_GROUPS_MOE = 4
EXP_PER_GROUP = 8
N_EXPERTS = N_GROUPS_MOE * EXP_PER_GROUP
D_EXPERT = 256
TOP_K_INNER = 2
MOE_BLOCK = 128
DEEPNORM_ALPHA = (2.0 * DEPTH) ** 0.25
DEEPNORM_BETA = (8.0 * DEPTH) ** -0.25
LN_EPS = 1e-5

kernel_name = "hybrid_s5_gmlp_memattn_hmoe_deepnorm"


def _layer_norm(x, g, b):
    xf = x.astype(jnp.float32)
    mu = jnp.mean(xf, axis=-1, keepdims=True)
    var = jnp.mean(jnp.square(xf - mu), axis=-1, keepdims=True)
    y = (xf - mu) * lax.rsqrt(var + LN_EPS)
    return (y * g.astype(jnp.float32) + b.astype(jnp.float32)).astype(x.dtype)


def _diag_scan(lam_bar, bu, reverse):
    a = jnp.broadcast_to(lam_bar, bu.shape)

    def combine(left, right):
        a_l, b_l = left
        a_r, b_r = right
        return a_l * a_r, a_r * b_l + b_r

    _, h = lax.associative_scan(combine, (a, bu), axis=1, reverse=reverse)
    return h


def _s5_branch(u, lam_re, lam_im, log_step, b_re, b_im, c_re, c_im, d_skip, w_glu, b_glu):
    dtype = u.dtype
    bsz, seq, _ = u.shape
    f32 = jnp.float32
    uf = u.astype(f32).reshape(bsz, seq, SSM_GROUPS, SSM_GROUP_CH)
    uc = uf.astype(jnp.complex64)
    b_mat = lax.complex(b_re.astype(f32), b_im.astype(f32))
    c_mat = lax.complex(c_re.astype(f32), c_im.astype(f32))
    y = d_skip.astype(f32).reshape(SSM_GROUPS, SSM_GROUP_CH) * uf
    for direction in range(2):
        lam = lax.complex(lam_re[direction].astype(f32), lam_im[direction].astype(f32))
        step = jnp.exp(log_step[direction].astype(f32))[:, None]
        lam_bar = jnp.exp(lam * step)
        b_bar = ((lam_bar - 1.0) / lam)[:, :, None] * b_mat
        bu = jnp.einsum('gph,bsgh->bsgp', b_bar, uc)
        h = _diag_scan(lam_bar, bu, reverse=(direction == 1))
        y = y + jnp.einsum('ghp,bsgp->bsgh', c_mat, h).real
    y = jax.nn.gelu(y.reshape(bsz, seq, SSM_WIDTH))
    gate = jax.nn.sigmoid(y @ w_glu.astype(f32) + b_glu.astype(f32))
    return (y * gate).astype(dtype)


def _gmlp_branch(uv, ln_g, ln_b, w_s, b_s):
    uv = jax.nn.gelu(uv)
    u, v = jnp.split(uv, 2, axis=-1)
    v = _layer_norm(v, ln_g, ln_b)
    bsz, seq, _ = v.shape
    vc = v.reshape(bsz, seq // CHUNK, CHUNK, GMLP_GROUPS, GMLP_GROUP_CH)
    s = jnp.einsum('gij,bcjgd->bcigd', w_s, vc) + b_s.T[None, None, :, :, None]
    return u * s.reshape(bsz, seq, GMLP_WIDTH)


def _memory_attention(q, mem, w_kv):
    bsz, seq, _ = q.shape
    k, v = jnp.split(mem @ w_kv, 2, axis=-1)
    qh = q.reshape(bsz, seq, MEM_HEADS, MEM_HEAD_DIM)
    kh = k.reshape(bsz, N_MEM, MEM_HEADS, MEM_HEAD_DIM)
    vh = v.reshape(bsz, N_MEM, MEM_HEADS, MEM_HEAD_DIM)
    scores = jnp.einsum('bshd,bmhd->bhsm', qh, kh).astype(jnp.float32) * (MEM_HEAD_DIM ** -0.5)
    p = jax.nn.softmax(scores, axis=-1).astype(q.dtype)
    o = jnp.einsum('bhsm,bmhd->bshd', p, vh)
    return o.reshape(bsz, seq, MEM_WIDTH)


def _hier_moe(x, w_rg, b_rg, w_re, b_re, w_gate_e, w_up_e, w_down_e):
    bsz, seq, d = x.shape
    xt = x.reshape(-1, MOE_BLOCK, d)

    def block(xb):
        lg = (xb @ w_rg + b_rg).astype(jnp.float32)
        pg = jax.nn.softmax(lg, axis=-1)
        g_star = jnp.argmax(lg, axis=-1)
        g_oh = jax.nn.one_hot(g_star, N_GROUPS_MOE, dtype=jnp.float32)
        pg_sel = jnp.sum(pg * g_oh, axis=-1)
        le = (jnp.einsum('nd,gde->nge', xb, w_re) + b_re).astype(jnp.float32)
        le_sel = jnp.einsum('ng,nge->ne', g_oh, le)
        top_v, top_i = lax.top_k(le_sel, TOP_K_INNER)
        pe = jax.nn.softmax(top_v, axis=-1)
        expert_id = g_star[:, None] * EXP_PER_GROUP + top_i
        e_oh = jax.nn.one_hot(expert_id, N_EXPERTS, dtype=jnp.float32)
        weights = jnp.einsum('nk,nke->ne', pg_sel[:, None] * pe, e_oh).astype(xb.dtype)
        h = jax.nn.silu(jnp.einsum('nd,edf->nef', xb, w_gate_e)) * jnp.einsum('nd,edf->nef', xb, w_up_e)
        return jnp.einsum('nef,efd->nd', h * weights[:, :, None], w_down_e)

    return lax.map(block, xt).reshape(bsz, seq, d)


def setup_inputs(seed: int = 0) -> dict:
    key = jax.random.key(seed)
    ks = iter(jax.random.split(key, 40))
    L, D = DEPTH, D_MODEL
    f32 = jnp.float32

    def nrm(shape, scale):
        return jax.random.normal(next(ks), shape, f32) * scale

    n_idx = jnp.arange(SSM_STATE, dtype=f32)
    lam_re = -0.5 + nrm((L, 2, SSM_GROUPS, SSM_STATE), 0.01)
    lam_im = math.pi * n_idx + nrm((L, 2, SSM_GROUPS, SSM_STATE), 0.01)
    log_step = jax.random.uniform(next(ks), (L, 2, SSM_GROUPS), f32,
                                  minval=math.log(1e-3), maxval=math.log(1e-1))
    w_kv_k = nrm((L, D, MEM_WIDTH), D ** -0.5)
    w_kv_v = nrm((L, D, MEM_WIDTH), D ** -0.5 * DEEPNORM_BETA)
    return {
        "x": nrm((BATCH, SEQ, D), 1.0),
        "mem": nrm((BATCH, N_MEM, D), 1.0),
        "w_in": nrm((L, D, IN_WIDTH), D ** -0.5),
        "w_gate": nrm((L, D, N_BRANCH * D), D ** -0.5),
        "b_gate": nrm((L, N_BRANCH * D), 0.1),
        "ssm_lam_re": lam_re,
        "ssm_lam_im": lam_im,
        "ssm_log_step": log_step,
        "ssm_b_re": nrm((L, SSM_GROUPS, SSM_STATE, SSM_GROUP_CH), (2.0 * SSM_GROUP_CH) ** -0.5),
        "ssm_b_im": nrm((L, SSM_GROUPS, SSM_STATE, SSM_GROUP_CH), (2.0 * SSM_GROUP_CH) ** -0.5),
        "ssm_c_re": nrm((L, SSM_GROUPS, SSM_GROUP_CH, SSM_STATE), (2.0 * SSM_STATE) ** -0.5),
        "ssm_c_im": nrm((L, SSM_GROUPS, SSM_GROUP_CH, SSM_STATE), (2.0 * SSM_STATE) ** -0.5),
        "ssm_d": nrm((L, SSM_WIDTH), 1.0),
        "w_glu": nrm((L, SSM_WIDTH, SSM_WIDTH), SSM_WIDTH ** -0.5),
        "b_glu": nrm((L, SSM_WIDTH), 0.01),
        "gmlp_ln_g": 1.0 + nrm((L, GMLP_WIDTH), 0.01),
        "gmlp_ln_b": nrm((L, GMLP_WIDTH), 0.01),
        "w_spatial": nrm((L, GMLP_GROUPS, CHUNK, CHUNK), CHUNK ** -0.5),
        "b_spatial": 1.0 + nrm((L, GMLP_GROUPS, CHUNK), 0.01),
        "w_kv": jnp.concatenate([w_kv_k, w_kv_v], axis=-1),
        "w_br": nrm((L, N_BRANCH, BRANCH_WIDTH, D), BRANCH_WIDTH ** -0.5 * DEEPNORM_BETA),
        "w_out": nrm((L, D, D), D ** -0.5 * DEEPNORM_BETA),
        "ln1_g": 1.0 + nrm((L, D), 0.01),
        "ln1_b": nrm((L, D), 0.01),
        "w_router_g": nrm((L, D, N_GROUPS_MOE), D ** -0.5),
        "b_router_g": nrm((L, N_GROUPS_MOE), 0.01),
        "w_router_e": nrm((L, N_GROUPS_MOE, D, EXP_PER_GROUP), D ** -0.5),
        "b_router_e": nrm((L, N_GROUPS_MOE, EXP_PER_GROUP), 0.01),
        "w_exp_gate": nrm((L, N_EXPERTS, D, D_EXPERT), D ** -0.5),
        "w_exp_up": nrm((L, N_EXPERTS, D, D_EXPERT), D ** -0.5),
        "w_exp_down": nrm((L, N_EXPERTS, D_EXPERT, D), D_EXPERT ** -0.5 * DEEPNORM_BETA),
        "ln2_g": 1.0 + nrm((L, D), 0.01),
        "ln2_b": nrm((L, D), 0.01),
    }


def reference(x, mem, w_in, w_gate, b_gate, ssm_lam_re, ssm_lam_im, ssm_log_step,
              ssm_b_re, ssm_b_im, ssm_c_re, ssm_c_im, ssm_d, w_glu, b_glu,
              gmlp_ln_g, gmlp_ln_b, w_spatial, b_spatial, w_kv, w_br, w_out,
              ln1_g, ln1_b, w_router_g, b_router_g, w_router_e, b_router_e,
              w_exp_gate, w_exp_up, w_exp_down, ln2_g, ln2_b):
    bsz, seq, d = x.shape
    for l in range(DEPTH):
        proj = x @ w_in[l]
        u_ssm = proj[..., :SSM_WIDTH]
        uv_gmlp = proj[..., SSM_WIDTH:SSM_WIDTH + 2 * GMLP_WIDTH]
        q_mem = proj[..., SSM_WIDTH + 2 * GMLP_WIDTH:]
        y_ssm = _s5_branch(u_ssm, ssm_lam_re[l], ssm_lam_im[l], ssm_log_step[l],
                           ssm_b_re[l], ssm_b_im[l], ssm_c_re[l], ssm_c_im[l],
                           ssm_d[l], w_glu[l], b_glu[l])
        y_gmlp = _gmlp_branch(uv_gmlp, gmlp_ln_g[l], gmlp_ln_b[l], w_spatial[l], b_spatial[l])
        y_mem = _memory_attention(q_mem, mem, w_kv[l])
        gates = jax.nn.sigmoid(x @ w_gate[l] + b_gate[l]).reshape(bsz, seq, N_BRANCH, d)
        merged = (gates[:, :, 0] * (y_ssm @ w_br[l, 0])
                  + gates[:, :, 1] * (y_gmlp @ w_br[l, 1])
                  + gates[:, :, 2] * (y_mem @ w_br[l, 2]))
        x = _layer_norm(DEEPNORM_ALPHA * x + merged @ w_out[l], ln1_g[l], ln1_b[l])
        moe_out = _hier_moe(x, w_router_g[l], b_router_g[l], w_router_e[l], b_router_e[l],
                            w_exp_gate[l], w_exp_up[l], w_exp_down[l])
        x = _layer_norm(DEEPNORM_ALPHA * x + moe_out, ln2_g[l], ln2_b[l])
    return x
```

```python
import numpy as np
from contextlib import ExitStack
import concourse.bass as bass
import concourse.mybir as mybir
from concourse.bass_utils import run_bass_kernel_spmd

F32 = mybir.dt.float32
BF16 = mybir.dt.bfloat16
I32 = mybir.dt.int32
AF = mybir.ActivationFunctionType
ALU = mybir.AluOpType
AX = mybir.AxisListType


class Buf:
    __slots__ = ("name", "w", "r", "excl")

    def __init__(self, name="", excl=False):
        self.name = name
        self.excl = excl
        self.w = None
        self.r = {}


import os as _os0
SAME_ENGINE_SYNC = _os0.environ.get('SAME_ENGINE_SYNC', '1') == '1'


class _Rec:
    def __init__(self):
        self.call = None

    def __getattr__(self, name):
        def f(*a, **k):
            self.call = (name, a, k)
            return self
        return f


class Prog:
    ENGS = ("pe", "act", "dve", "pool", "sp")

    def __init__(self, nc, n_dma_sems=16, same_engine_sync=SAME_ENGINE_SYNC):
        self.nc = nc
        self.es = ExitStack()
        self.q = {e: [] for e in self.ENGS}
        self.cnt = {}
        self.seen = {e: {} for e in self.ENGS}
        self.sem = {}
        for e in self.ENGS:
            self.sem["c_" + e] = self.es.enter_context(nc.semaphore("c_" + e))
            self.cnt["c_" + e] = 0
        self.dma_keys = {}
        self.dma_rr = {}
        for qe in ("sp", "pool", "act"):
            self.dma_keys[qe] = []
            self.dma_rr[qe] = 0
            for i in range(n_dma_sems if qe != "act" else 4):
                k = "d_%s_%d" % (qe, i)
                self.sem[k] = self.es.enter_context(nc.semaphore(k))
                self.cnt[k] = 0
                self.dma_keys[qe].append(k)
        self.same_engine_sync = same_engine_sync
        self.n_ops = 0
        self.pending = {e: {} for e in self.ENGS}
        self.open_scopes = []

    def sbuf(self, name, shape, dtype):
        return self.es.enter_context(self.nc.sbuf_tensor(name, list(shape), dtype))

    def psum(self, name, shape, dtype=F32):
        return self.es.enter_context(self.nc.psum_tensor(name, list(shape), dtype))

    def _waits(self, eng, reads, writes):
        deps = {}
        for b in reads:
            if b.w is not None:
                k, v = b.w
                deps[k] = max(deps.get(k, 0), v)
        for b in writes:
            if b.w is not None:
                k, v = b.w
                deps[k] = max(deps.get(k, 0), v)
            for k, v in b.r.items():
                deps[k] = max(deps.get(k, 0), v)
        for k, v in self.pending[eng].items():
            deps[k] = max(deps.get(k, 0), v)
        self.pending[eng] = {}
        waits = []
        own = "c_" + eng
        for k, v in deps.items():
            if k == own and (eng == "pe" or not self.same_engine_sync):
                continue
            if self.seen[eng].get(k, 0) >= v:
                continue
            self.seen[eng][k] = v
            waits.append((k, v))
        return waits

    def _commit(self, tick, reads, writes):
        k, v = tick
        for b in reads:
            b.r[k] = max(b.r.get(k, 0), v)
        for b in writes:
            b.w = tick
            b.r = {}

    def op(self, eng, fn, reads=(), writes=()):
        ex = [b for b in reads if b.excl]
        if ex:
            writes = list(writes) + ex
        waits = self._waits(eng, reads, writes)
        key = "c_" + eng
        self.cnt[key] += 1
        tick = (key, self.cnt[key])
        rec = _Rec()
        fn(rec)
        self.q[eng].append((waits, rec.call, key, 1))
        self._commit(tick, reads, writes)
        self.n_ops += 1

    def dma(self, eng, out, in_, reads=(), writes=(), **kw):
        waits = self._waits(eng, reads, writes)
        key = self.dma_keys[eng][self.dma_rr[eng]]
        self.dma_rr[eng] = (self.dma_rr[eng] + 1) % len(self.dma_keys[eng])
        prev = self.cnt[key]
        if prev > 0 and self.seen[eng].get(key, 0) < prev:
            self.seen[eng][key] = prev
            waits.append((key, prev))
        self.cnt[key] += 16
        tick = (key, self.cnt[key])
        kk = dict(kw)
        kk["out"] = out
        kk["in_"] = in_
        self.q[eng].append((waits, ("dma_start", (), kk), key, 16))
        self._commit(tick, reads, writes)
        self.n_ops += 1

    def barrier(self):
        for e in self.ENGS:
            for k, v in self.cnt.items():
                if v > 0:
                    self.pending[e][k] = max(self.pending[e].get(k, 0), v)

    def finish(self, final_bufs=()):
        fin = []
        for k, v in self.cnt.items():
            if v > 0 and self.seen["sp"].get(k, 0) < v and k != "c_sp":
                fin.append((k, v))
        nc = self.nc
        sem = self.sem
        q = self.q
        with nc.Block() as block:
            def emit(e, lst, extra=()):
                for waits, call, key, inc in lst:
                    for k, v in waits:
                        e.wait_ge(sem[k], v)
                    name, a, kw = call
                    getattr(e, name)(*a, **kw).then_inc(sem[key], inc)
                for k, v in extra:
                    e.wait_ge(sem[k], v)

            @block.sync
            def _(e):
                emit(e, q["sp"], fin)

            @block.scalar
            def _(e):
                emit(e, q["act"])

            @block.vector
            def _(e):
                emit(e, q["dve"])

            @block.gpsimd
            def _(e):
                emit(e, q["pool"])

            @block.tensor
            def _(e):
                emit(e, q["pe"])
        while self.open_scopes:
            self.open_scopes.pop().close()
        self.es.close()


D = 1024
NT = 2048
TB = 512
NTB = NT // TB
NCH = NT // 8
SCAN_K = 8
SCAN_L = NCH // SCAN_K
DEPTH = 4
ALPHA = (2.0 * DEPTH) ** 0.25
LN_EPS = 1e-5
KVEC = [0, -1, -2, -3, -4, -5, -6, -7, 0, 1, 2, 3, 4, 5, 6, 7, 1, 7, 8]
NK = len(KVEC)
MAGIC = 12582912.0
TWO_PI_HI = 6.28125
TWO_PI_LO = 2.0 * np.pi - 6.28125
GELU_C = 0.044715
GELU_S = 2.0 * 0.7978845608028654


def host_consts():
    ident = np.eye(128, dtype=np.float32)
    wide = np.zeros((128, 8, 240), np.float32)
    for a in range(8):
        for hc in range(16):
            wide[16 * a + hc, a, 112 + hc] = 1.0
    s_idx = np.arange(128) // 16
    maskF = (s_idx[None, :] >= s_idx[:, None]).astype(np.float32)
    maskB = (s_idx[:, None] >= s_idx[None, :]).astype(np.float32)
    maskF = maskF - maskB
    kvec = np.tile(np.array(KVEC, np.float32)[None, :], (128, 1))
    return dict(c_ident=ident, c_wide=np.ascontiguousarray(wide.reshape(128, 8 * 240)),
                c_maskF=maskF, c_maskB=maskB, c_kvec=kvec)


def host_s5_params(inp, l):
    f = np.float32

    def dp(a):
        return np.ascontiguousarray(np.transpose(a, (0, 2, 1)).reshape(128, 32)).astype(f)
    lam_re = dp(inp["ssm_lam_re"][l])
    lam_im = dp(inp["ssm_lam_im"][l])
    lstep = np.ascontiguousarray(np.broadcast_to(inp["ssm_log_step"][l][:, None, :], (2, 64, 32)).reshape(128, 32)).astype(f)

    def rep_b(a):
        t = np.transpose(a, (1, 0, 2))
        return np.ascontiguousarray(np.concatenate([t, t], 0).reshape(128, 32 * 16)).astype(f)

    def rep_c(a):
        t = np.transpose(a, (2, 0, 1))
        return np.ascontiguousarray(np.concatenate([t, t], 0).reshape(128, 32 * 16)).astype(f)
    dm = inp["ssm_d"][l].reshape(32, 16)
    dm = np.ascontiguousarray(np.tile(dm.T, (8, 1))).astype(f)
    return dict(lam_re=lam_re, lam_im=lam_im, lstep=lstep,
                b_re=rep_b(inp["ssm_b_re"][l]), b_im=rep_b(inp["ssm_b_im"][l]),
                c_re=rep_c(inp["ssm_c_re"][l]), c_im=rep_c(inp["ssm_c_im"][l]), dm=dm)


class Ctx:
    def __init__(self, P):
        self.P = P
        self.t = {}
        self.b = {}
        self.ps = []
        self.psb = []
        self.ps_rr = 0
        self.scopes = P.open_scopes
        self.uid = 0
        self.ps_lim = 8

    def sb(self, name, shape, dtype, nbuf=None):
        es = self.scopes[-1] if self.scopes else self.P.es
        self.uid += 1
        self.t[name] = es.enter_context(self.P.nc.sbuf_tensor("%s_%d" % (name, self.uid), list(shape), dtype))
        self.b[name] = Buf(name)
        return self.t[name]

    def push(self):
        self.scopes.append(ExitStack())

    def pop(self):
        self.P.barrier()
        self.scopes.pop().close()

    def alloc_psum(self):
        for i in range(8):
            self.ps.append(self.P.psum("psb%d" % i, [128, 512], F32))
            self.psb.append(Buf("psb%d" % i, excl=True))

    def next_ps(self):
        i = self.ps_rr
        self.ps_rr = (self.ps_rr + 1) % self.ps_lim
        return self.ps[i], self.psb[i]


def v_tt(P, eng, out, a, b, op, r, w):
    P.op(eng, lambda e: e.tensor_tensor(out=out, in0=a, in1=b, op=op), reads=r, writes=w)


def v_ts(P, eng, out, a, s1, s2, op0, op1, r, w):
    if s2 is None:
        P.op(eng, lambda e: e.tensor_scalar(out=out, in0=a, scalar1=s1, scalar2=None, op0=op0), reads=r, writes=w)
    else:
        P.op(eng, lambda e: e.tensor_scalar(out=out, in0=a, scalar1=s1, scalar2=s2, op0=op0, op1=op1), reads=r, writes=w)


def v_cp(P, eng, out, a, r, w):
    if eng == "act":
        P.op(eng, lambda e: e.copy(out=out, in_=a), reads=r, writes=w)
    else:
        P.op(eng, lambda e: e.tensor_copy(out=out, in_=a), reads=r, writes=w)


def v_act(P, out, a, func, r, w, bias=None, scale=None):
    kw = {}
    if bias is not None:
        kw["bias"] = bias
    if scale is not None:
        kw["scale"] = scale
    P.op("act", lambda e: e.activation(out=out, in_=a, func=func, **kw), reads=r, writes=w)


def mm(P, out, lhsT, rhs, start, stop, r, w):
    P.op("pe", lambda e: e.matmul(out, lhsT, rhs, start=start, stop=stop), reads=r, writes=w)


def cmul(P, eng, o_re, o_im, a_re, a_im, b_re, b_im, tmp):
    v_tt(P, eng, o_re[0], a_re[0], b_re[0], ALU.mult, [a_re[1], b_re[1]], [o_re[1]])
    v_tt(P, eng, tmp[0], a_im[0], b_im[0], ALU.mult, [a_im[1], b_im[1]], [tmp[1]])
    v_tt(P, eng, o_re[0], o_re[0], tmp[0], ALU.subtract, [o_re[1], tmp[1]], [o_re[1]])
    v_tt(P, eng, o_im[0], a_re[0], b_im[0], ALU.mult, [a_re[1], b_im[1]], [o_im[1]])
    v_tt(P, eng, tmp[0], a_im[0], b_re[0], ALU.mult, [a_im[1], b_re[1]], [tmp[1]])
    v_tt(P, eng, o_im[0], o_im[0], tmp[0], ALU.add, [o_im[1], tmp[1]], [o_im[1]])


def cmul_big(P, eng, o_re, o_im, a_re, a_im, b_re, b_im, tmp):
    for g0 in (0, 16):
        def sl(v):
            return (v[0][:, g0:g0 + 16], v[1])
        cmul(P, eng, sl(o_re), sl(o_im), sl(a_re), sl(a_im), sl(b_re), sl(b_im), (tmp[0], tmp[1]))


def alloc_s5_persist(C):
    C.sb("q_A2", [128, 2, 2, 32], F32)
    C.sb("q_P", [128, 3, 2, 32], F32)
    C.sb("w_Glre", [128, 32, 8, 16], BF16)
    C.sb("w_GlimN", [128, 32, 8, 16], BF16)
    C.sb("w_Msum", [128, 32, 128], BF16)
    C.sb("w_Wl", [128, 32, 2, 128], BF16)


def alloc_s5_prep(C):
    for n in ("p_lam_re", "p_lam_im", "p_lstep", "p_dm"):
        C.sb(n, [128, 32], F32)
    for n in ("p_b_re", "p_b_im", "p_c_re", "p_c_im"):
        C.sb(n, [128, 32, 16], F32)
    for n in ("q_step", "q_a", "q_b", "q_den", "q_em1", "q_t1", "q_t2", "q_bre", "q_bim"):
        C.sb(n, [128, 32], F32)
    for n in ("q_ka", "q_kb", "q_n", "q_r", "q_mag", "q_Ere", "q_Eim"):
        C.sb(n, [128, 32, NK], F32)
    for n in ("q_bBre", "q_bBim", "q_t3"):
        C.sb(n, [128, 32, 16], F32)
    for n in ("s_are", "s_aim", "s_bre", "s_bim", "s_cre", "s_cim"):
        C.sb(n, [128, 32, 8, 16], F32)
    C.sb("s_tmp", [128, 16, 8, 16], F32)
    C.sb("s_m1", [128, 128], F32)
    C.sb("s_m2", [128, 128], F32)


def emit_s5_prep(P, C, prm):
    T, B = C.t, C.b

    def V(n, ap=None):
        return (T[n][:] if ap is None else ap, B[n])
    for n in ("lam_re", "lam_im", "lstep", "dm"):
        P.dma("sp", T["p_" + n][:], prm[n], writes=[B["p_" + n]])
    for n in ("b_re", "b_im", "c_re", "c_im"):
        P.dma("sp", T["p_" + n][:].rearrange("p g h -> p (g h)"), prm[n], writes=[B["p_" + n]])
    e = "dve"
    v_act(P, T["q_step"][:], T["p_lstep"][:], AF.Exp, [B["p_lstep"]], [B["q_step"]])
    v_tt(P, e, T["q_a"][:], T["p_lam_re"][:], T["q_step"][:], ALU.mult, [B["p_lam_re"], B["q_step"]], [B["q_a"]])
    v_tt(P, e, T["q_b"][:], T["p_lam_im"][:], T["q_step"][:], ALU.mult, [B["p_lam_im"], B["q_step"]], [B["q_b"]])
    kv = T["c_kvec"][:].unsqueeze(1).broadcast_to([128, 32, NK])
    sh3 = [128, 32, NK]
    v_tt(P, e, T["q_ka"][:], T["q_a"][:].unsqueeze(2).broadcast_to(sh3), kv, ALU.mult, [B["q_a"], B["c_kvec"]], [B["q_ka"]])
    v_tt(P, e, T["q_kb"][:], T["q_b"][:].unsqueeze(2).broadcast_to(sh3), kv, ALU.mult, [B["q_b"], B["c_kvec"]], [B["q_kb"]])
    v_act(P, T["q_mag"][:], T["q_ka"][:], AF.Exp, [B["q_ka"]], [B["q_mag"]])
    inv2pi = float(1.0 / (2.0 * np.pi))
    for which, off, dst in (("sin", 0.0, "q_Eim"), ("cos", 0.25, "q_Ere")):
        v_ts(P, e, T["q_n"][:], T["q_kb"][:], inv2pi, off, ALU.mult, ALU.add, [B["q_kb"]], [B["q_n"]])
        v_ts(P, e, T["q_n"][:], T["q_n"][:], MAGIC, None, ALU.add, None, [B["q_n"]], [B["q_n"]])
        v_ts(P, e, T["q_n"][:], T["q_n"][:], -MAGIC, None, ALU.add, None, [B["q_n"]], [B["q_n"]])
        P.op(e, lambda en: en.scalar_tensor_tensor(out=T["q_r"][:], in0=T["q_n"][:], scalar=-TWO_PI_HI, in1=T["q_kb"][:],
                                                   op0=ALU.mult, op1=ALU.add), reads=[B["q_n"], B["q_kb"]], writes=[B["q_r"]])
        P.op(e, lambda en: en.scalar_tensor_tensor(out=T["q_r"][:], in0=T["q_n"][:], scalar=-TWO_PI_LO, in1=T["q_r"][:],
                                                   op0=ALU.mult, op1=ALU.add), reads=[B["q_n"], B["q_r"]], writes=[B["q_r"]])
        if off != 0.0:
            v_ts(P, e, T["q_r"][:], T["q_r"][:], float(np.pi / 2), None, ALU.add, None, [B["q_r"]], [B["q_r"]])
        v_ts(P, e, T["q_r"][:], T["q_r"][:], float(np.pi), float(-np.pi), ALU.min, ALU.max, [B["q_r"]], [B["q_r"]])
        v_act(P, T[dst][:], T["q_r"][:], AF.Sin, [B["q_r"]], [B[dst]])
    v_tt(P, e, T["q_Ere"][:], T["q_mag"][:], T["q_Ere"][:], ALU.mult, [B["q_mag"], B["q_Ere"]], [B["q_Ere"]])
    v_tt(P, e, T["q_Eim"][:], T["q_mag"][:], T["q_Eim"][:], ALU.mult, [B["q_mag"], B["q_Eim"]], [B["q_Eim"]])
    Ere, Eim = T["q_Ere"], T["q_Eim"]
    v_tt(P, e, T["q_den"][:], T["p_lam_re"][:], T["p_lam_re"][:], ALU.mult, [B["p_lam_re"]], [B["q_den"]])
    v_tt(P, e, T["q_t1"][:], T["p_lam_im"][:], T["p_lam_im"][:], ALU.mult, [B["p_lam_im"]], [B["q_t1"]])
    v_tt(P, e, T["q_den"][:], T["q_den"][:], T["q_t1"][:], ALU.add, [B["q_den"], B["q_t1"]], [B["q_den"]])
    P.op(e, lambda en: en.reciprocal(out=T["q_den"][:], in_=T["q_den"][:]), reads=[B["q_den"]], writes=[B["q_den"]])
    v_ts(P, e, T["q_em1"][:], Ere[:, :, 16], -1.0, None, ALU.add, None, [B["q_Ere"]], [B["q_em1"]])
    v_tt(P, e, T["q_bre"][:], T["q_em1"][:], T["p_lam_re"][:], ALU.mult, [B["q_em1"], B["p_lam_re"]], [B["q_bre"]])
    v_tt(P, e, T["q_t1"][:], Eim[:, :, 16], T["p_lam_im"][:], ALU.mult, [B["q_Eim"], B["p_lam_im"]], [B["q_t1"]])
    v_tt(P, e, T["q_bre"][:], T["q_bre"][:], T["q_t1"][:], ALU.add, [B["q_bre"], B["q_t1"]], [B["q_bre"]])
    v_tt(P, e, T["q_bim"][:], Eim[:, :, 16], T["p_lam_re"][:], ALU.mult, [B["q_Eim"], B["p_lam_re"]], [B["q_bim"]])
    v_tt(P, e, T["q_t2"][:], T["q_em1"][:], T["p_lam_im"][:], ALU.mult, [B["q_em1"], B["p_lam_im"]], [B["q_t2"]])
    v_tt(P, e, T["q_bim"][:], T["q_bim"][:], T["q_t2"][:], ALU.subtract, [B["q_bim"], B["q_t2"]], [B["q_bim"]])
    v_tt(P, e, T["q_bre"][:], T["q_bre"][:], T["q_den"][:], ALU.mult, [B["q_bre"], B["q_den"]], [B["q_bre"]])
    v_tt(P, e, T["q_bim"][:], T["q_bim"][:], T["q_den"][:], ALU.mult, [B["q_bim"], B["q_den"]], [B["q_bim"]])
    s16 = [128, 32, 16]
    bre_b = (T["q_bre"][:].unsqueeze(2).broadcast_to(s16), B["q_bre"])
    bim_b = (T["q_bim"][:].unsqueeze(2).broadcast_to(s16), B["q_bim"])
    cmul(P, e, V("q_bBre"), V("q_bBim"), bre_b, bim_b, V("p_b_re"), V("p_b_im"), V("q_t3"))
    A2 = T["q_A2"]
    v_cp(P, e, A2[:, 0, 0, :], Ere[:, :, 18], [B["q_Ere"]], [B["q_A2"]])
    v_cp(P, e, A2[:, 0, 1, :], Ere[:, :, 18], [B["q_Ere"]], [B["q_A2"]])
    v_ts(P, e, A2[:, 1, 0, :], Eim[:, :, 18], -1.0, None, ALU.mult, None, [B["q_Eim"]], [B["q_A2"]])
    v_cp(P, e, A2[:, 1, 1, :], Eim[:, :, 18], [B["q_Eim"]], [B["q_A2"]])
    Pm = T["q_P"]
    v_cp(P, e, Pm[:, 0, 0, :], Ere[:, :, 18], [B["q_Ere"]], [B["q_P"]])
    v_cp(P, e, Pm[:, 0, 1, :], Eim[:, :, 18], [B["q_Eim"]], [B["q_P"]])

    def csq(dst_re, dst_im, a_re, a_im, b_re, b_im):
        v_tt(P, e, T["q_t1"][:], a_re, b_re, ALU.mult, [B["q_P"]], [B["q_t1"]])
        v_tt(P, e, T["q_t2"][:], a_im, b_im, ALU.mult, [B["q_P"]], [B["q_t2"]])
        v_tt(P, e, T["q_t1"][:], T["q_t1"][:], T["q_t2"][:], ALU.subtract, [B["q_t1"], B["q_t2"]], [B["q_t1"]])
        v_tt(P, e, T["q_t2"][:], a_re, b_im, ALU.mult, [B["q_P"]], [B["q_t2"]])
        v_tt(P, e, T["q_den"][:], a_im, b_re, ALU.mult, [B["q_P"]], [B["q_den"]])
        v_tt(P, e, dst_im, T["q_t2"][:], T["q_den"][:], ALU.add, [B["q_t2"], B["q_den"]], [B["q_P"]])
        v_cp(P, e, dst_re, T["q_t1"][:], [B["q_t1"]], [B["q_P"]])
    for _ in range(8):
        csq(Pm[:, 0, 0, :], Pm[:, 0, 1, :], Pm[:, 0, 0, :], Pm[:, 0, 1, :], Pm[:, 0, 0, :], Pm[:, 0, 1, :])
    csq(Pm[:, 1, 0, :], Pm[:, 1, 1, :], Pm[:, 0, 0, :], Pm[:, 0, 1, :], Pm[:, 0, 0, :], Pm[:, 0, 1, :])
    csq(Pm[:, 2, 0, :], Pm[:, 2, 1, :], Pm[:, 1, 0, :], Pm[:, 1, 1, :], Pm[:, 0, 0, :], Pm[:, 0, 1, :])
    s4 = [128, 32, 8, 16]

    def Ek(lo, hi):
        return ((Ere[:, :, lo:hi].unsqueeze(3).broadcast_to(s4), B["q_Ere"]),
                (Eim[:, :, lo:hi].unsqueeze(3).broadcast_to(s4), B["q_Eim"]))

    def E1(idx):
        return ((Ere[:, :, idx:idx + 1].unsqueeze(3).broadcast_to(s4), B["q_Ere"]),
                (Eim[:, :, idx:idx + 1].unsqueeze(3).broadcast_to(s4), B["q_Eim"]))
    bB_re = (T["q_bBre"][:].unsqueeze(2).broadcast_to(s4), B["q_bBre"])
    bB_im = (T["q_bBim"][:].unsqueeze(2).broadcast_to(s4), B["q_bBim"])
    def ctab(o_re_n, o_im_n, a_re, a_im, b_re, b_im, rev_bwd):
        for lo, hi, rev in ((0, 64, False), (64, 128, rev_bwd)):
            o_re = (T[o_re_n][lo:hi, :, ::-1, :] if rev else T[o_re_n][lo:hi], B[o_re_n])
            o_im = (T[o_im_n][lo:hi, :, ::-1, :] if rev else T[o_im_n][lo:hi], B[o_im_n])
            cmul_big(P, e, o_re, o_im, (a_re[0][lo:hi], a_re[1]), (a_im[0][lo:hi], a_im[1]),
                     (b_re[0][lo:hi], b_re[1]), (b_im[0][lo:hi], b_im[1]), (T["s_tmp"][lo:hi], B["s_tmp"]))
    er, ei = Ek(0, 8)
    ctab("s_are", "s_aim", er, ei, bB_re, bB_im, True)
    c_re = (T["p_c_re"][:].unsqueeze(2).broadcast_to(s4), B["p_c_re"])
    c_im = (T["p_c_im"][:].unsqueeze(2).broadcast_to(s4), B["p_c_im"])
    er, ei = Ek(8, 16)
    ctab("s_bre", "s_bim", er, ei, c_re, c_im, True)
    e1r, e1i = E1(16)
    ctab("s_cre", "s_cim", e1r, e1i, V("s_bre"), V("s_bim"), False)
    v_cp(P, "act", T["w_Glre"][:], T["s_cre"][:], [B["s_cre"]], [B["w_Glre"]])
    v_ts(P, e, T["w_GlimN"][:], T["s_cim"][:], -1.0, None, ALU.mult, None, [B["s_cim"]], [B["w_GlimN"]])
    e7r, e7i = E1(17)
    ctab("s_cre", "s_cim", e7r, e7i, V("s_are"), V("s_aim"), False)
    v_ts(P, e, T["s_bim"][:], T["s_bim"][:], -1.0, None, ALU.mult, None, [B["s_bim"]], [B["s_bim"]])
    idf = T["c_ident"]

    def fl(ap):
        return ap.rearrange("p s h -> p (s h)")
    for g in range(32):
        psA, pbA = C.next_ps()
        mm(P, psA[:, 0:128], fl(T["s_are"][0:64, g]), fl(T["s_bre"][0:64, g]), True, False, [B["s_are"], B["s_bre"]], [pbA])
        mm(P, psA[:, 0:128], fl(T["s_aim"][0:64, g]), fl(T["s_bim"][0:64, g]), False, True, [B["s_aim"], B["s_bim"]], [pbA])
        mm(P, psA[:, 128:256], fl(T["s_are"][:, g]), fl(T["s_bre"][:, g]), True, False, [B["s_are"], B["s_bre"]], [pbA])
        mm(P, psA[:, 128:256], fl(T["s_aim"][:, g]), fl(T["s_bim"][:, g]), False, True, [B["s_aim"], B["s_bim"]], [pbA])
        for ri, tn in ((0, "s_cre"), (1, "s_cim")):
            o = 256 + ri * 128
            mm(P, psA[:, o:o + 128], fl(T[tn][:, g]), idf[:, :], True, True, [B[tn], B["c_ident"]], [pbA])
        v_tt(P, e, T["s_m1"][:], psA[:, 0:128], T["c_maskF"][:], ALU.mult, [pbA, B["c_maskF"]], [B["s_m1"]])
        v_tt(P, e, T["s_m2"][:], psA[:, 128:256], T["c_maskB"][:], ALU.mult, [pbA, B["c_maskB"]], [B["s_m2"]])
        v_tt(P, "pool", T["s_m1"][:], T["s_m1"][:], T["s_m2"][:], ALU.add, [B["s_m1"], B["s_m2"]], [B["s_m1"]])
        P.op("dve", lambda en, g=g: en.scalar_tensor_tensor(out=T["w_Msum"][:, g, :], in0=T["c_ident"][:], scalar=T["p_dm"][:, g:g + 1],
                                                             in1=T["s_m1"][:], op0=ALU.mult, op1=ALU.add),
             reads=[B["c_ident"], B["p_dm"], B["s_m1"]], writes=[B["w_Msum"]])
        v_cp(P, "act", T["w_Wl"][:, g].rearrange("p r m -> p (r m)"), psA[:, 256:512], [pbA], [B["w_Wl"]])


def alloc_s5_main(C):
    C.sb("a_U", [128, 4, NT], BF16)
    C.sb("a_X8", [128, 32, NCH], BF16)
    C.sb("a_Hs", [128, NCH + 1 + SCAN_L, 2, 32], F32)
    C.sb("a_I", [128, SCAN_K + 1, 2, 32], F32)
    C.sb("a_P32", [128, 2, 2, 32], F32)
    C.sb("a_st", [128, 2, 2, 32], F32)
    C.sb("a_st9", [128, 2, SCAN_K + 1, 2, 32], F32)
    C.sb("a_st1", [128, 2, 2, 32], F32)
    C.bX8 = [Buf("x8_%d" % g) for g in range(32)]


def emit_s5_scan(P, C):
    T, B = C.t, C.b
    Hs, A2, st, I, P32 = T["a_Hs"], T["q_A2"], T["a_st9"], T["a_I"], T["a_P32"]
    K, L = SCAN_K, SCAN_L
    V = Hs[:, 1:1 + (K + 1) * L].rearrange("p (b i) r g -> p b i r g", b=K + 1)
    hb = [B["a_Hs"]]
    s9 = [128, K + 1, 2, 32]
    Are = A2[:, 0].unsqueeze(1).broadcast_to(s9)
    Aim = A2[:, 1].unsqueeze(1).broadcast_to(s9)
    P.op("dve", lambda e: e.memset(Hs[:, 1 + K * L:1 + (K + 1) * L], 0.0), writes=hb)
    v_cp(P, "dve", V[:, K, 0, 0, :], A2[:, 0, 0, :], [B["q_A2"]], hb)
    v_cp(P, "dve", V[:, K, 0, 1, :], A2[:, 1, 1, :], [B["q_A2"]], hb)
    for i in range(1, L):
        v_tt(P, "dve", st[:, 0], V[:, :, i - 1], Are, ALU.mult, hb + [B["q_A2"]], [B["a_st9"]])
        v_tt(P, "dve", st[:, 1], V[:, :, i - 1, ::-1, :], Aim, ALU.mult, hb + [B["q_A2"]], [C.b_st2])
        v_tt(P, "dve", V[:, :, i], V[:, :, i], st[:, 0], ALU.add, hb + [B["a_st9"]], hb)
        v_tt(P, "dve", V[:, :, i], V[:, :, i], st[:, 1], ALU.add, hb + [C.b_st2], hb)
    v_cp(P, "dve", P32[:, 0, 0, :], V[:, K, L - 1, 0, :], hb, [B["a_P32"]])
    v_cp(P, "dve", P32[:, 0, 1, :], V[:, K, L - 1, 0, :], hb, [B["a_P32"]])
    v_ts(P, "dve", P32[:, 1, 0, :], V[:, K, L - 1, 1, :], -1.0, None, ALU.mult, None, hb, [B["a_P32"]])
    v_cp(P, "dve", P32[:, 1, 1, :], V[:, K, L - 1, 1, :], hb, [B["a_P32"]])
    ib = [B["a_I"]]
    s1 = T["a_st1"]
    v_cp(P, "dve", I[:, 0], Hs[:, 0], hb, ib)
    for b_ in range(K):
        v_tt(P, "dve", s1[:, 0], I[:, b_], P32[:, 0], ALU.mult, ib + [B["a_P32"]], [B["a_st1"]])
        v_tt(P, "dve", s1[:, 1], I[:, b_, ::-1, :], P32[:, 1], ALU.mult, ib + [B["a_P32"]], [B["a_st1"]])
        v_tt(P, "dve", I[:, b_ + 1], V[:, b_, L - 1], s1[:, 0], ALU.add, hb + [B["a_st1"]], ib)
        v_tt(P, "dve", I[:, b_ + 1], I[:, b_ + 1], s1[:, 1], ALU.add, ib + [B["a_st1"]], ib)
    sF = [128, K, L, 32]
    Tm = T["a_T"]
    Pw = V[:, K]

    def pw(r):
        return Pw[:, :, r, :].unsqueeze(1).broadcast_to(sF)

    def ii(r):
        return I[:, 0:K, r, :].unsqueeze(2).broadcast_to(sF)
    for k, (pr_, ir_, out_r, op) in enumerate(((0, 0, 0, ALU.add), (1, 1, 0, ALU.subtract), (0, 1, 1, ALU.add), (1, 0, 1, ALU.add))):
        tm, tb_ = Tm[:], C.b_T[0]
        v_tt(P, "pool", tm, pw(pr_), ii(ir_), ALU.mult, hb + ib, [tb_])
        v_tt(P, "dve", V[:, 0:K, :, out_r, :], V[:, 0:K, :, out_r, :], tm, op, hb + [tb_], [C.b_Hfix[out_r]])
    P.op("dve", lambda e: e.tensor_copy(out=I[:, 0, 0, 0:1], in_=I[:, 0, 0, 0:1]), reads=[C.b_Hfix[0], C.b_Hfix[1]] + ib, writes=hb + ib)


def emit_s5_front(P, C, xb, xb_buf, win, win_buf):
    T, B = C.t, C.b
    U, X8, Hs = T["a_U"], T["a_X8"], T["a_Hs"]
    C.b_st2 = Buf("st2")
    for tb in range(NTB):
        for m in range(4):
            ps, pb = C.next_ps()
            for kd in range(8):
                mm(P, ps[:], win[:, kd, m * 128:(m + 1) * 128], xb[:, kd, tb * TB:(tb + 1) * TB], kd == 0, kd == 7,
                   [win_buf, xb_buf], [pb])
            v_cp(P, "act" if (m % 2) else "dve", U[:, m, tb * TB:(tb + 1) * TB], ps[:], [pb], [B["a_U"]])


def emit_s5_mid(P, C):
    T, B = C.t, C.b
    U, X8, Hs = T["a_U"], T["a_X8"], T["a_Hs"]
    C.push()
    C.sb("a_T", [128, SCAN_K, SCAN_L, 32], F32)
    C.b_T = [Buf("aT0")]
    C.b_Hfix = [Buf("hfix0"), Buf("hfix1")]
    wide = T["c_wide_bf"]
    for g2 in range(16):
        ps, pb = C.next_ps()
        for gg in range(2):
            g = 2 * g2 + gg
            gl, m = g % 8, g // 8
            Uv = U[:, m, :].rearrange("p (c s) -> p s c", s=8)
            for s in range(8):
                mm(P, ps[:, gg * 256:(gg + 1) * 256], wide[:, gl, 112 - 16 * s:112 - 16 * s + 128], Uv[:, s, :], s == 0, s == 7,
                   [B["c_wide_bf"], B["a_U"]], [pb])
        v_cp(P, "act" if (g2 % 2) else "dve", X8[:, 2 * g2:2 * g2 + 2, :].rearrange("p g c -> p (g c)"), ps[:], [pb],
             [C.bX8[2 * g2], C.bX8[2 * g2 + 1]])
    Wl = T["w_Wl"]
    for g in range(32):
        ps, pb = C.next_ps()
        for ri in range(2):
            mm(P, ps[:, ri * 256:(ri + 1) * 256], Wl[:, g, ri, :], X8[:, g, :], True, True, [B["w_Wl"], C.bX8[g]], [pb])
        psv = ps[:].rearrange("p (r c) -> p c r", r=2)
        v_cp(P, "act", Hs[0:64, 1:NCH + 1, :, g], psv[0:64], [pb], [B["a_Hs"]])
        v_cp(P, "dve", Hs[64:128, 1:NCH + 1, :, g], psv[64:128, ::-1, :], [pb], [B["a_Hs"]])
    emit_s5_scan(P, C)
    C.pop()


import os as _os
S5_STOP = int(_os.environ.get("S5_STOP", "0"))


def emit_s5_back(P, C, wglu, wglu_buf, bglu, bglu_buf):
    T, B = C.t, C.b
    U, X8, Hs = T["a_U"], T["a_X8"], T["a_Hs"]
    HsB = C.sb("a_HsB", [128, NCH + 1, 2, 32], BF16)
    v_cp(P, "act", HsB[0:64, 0:NCH], Hs[0:64, 0:NCH], [B["a_Hs"]], [B["a_HsB"]])
    v_cp(P, "dve", HsB[64:128, 0:NCH], Hs[64:128, NCH - 1::-1], [B["a_Hs"]], [B["a_HsB"]])
    for g2 in range(16):
        ps, pb = C.next_ps()
        for gg in range(2):
            g = 2 * g2 + gg
            o = ps[:, gg * 256:(gg + 1) * 256]
            mm(P, o, T["w_Msum"][:, g, :], X8[:, g, :], True, False, [B["w_Msum"], C.bX8[g]], [pb])
            for ri, tn in ((0, "w_Glre"), (1, "w_GlimN")):
                mm(P, o, T[tn][:, g].rearrange("p j h -> p (j h)"), HsB[:, 0:NCH, ri, g], False, ri == 1, [B[tn], B["a_HsB"]], [pb])
        v_cp(P, "act" if (g2 % 2) else "dve", X8[:, 2 * g2:2 * g2 + 2, :].rearrange("p g c -> p (g c)"), ps[:], [pb],
             [C.bX8[2 * g2], C.bX8[2 * g2 + 1]])
    if S5_STOP == 3:
        return
    wide = T["c_wide_bf"]
    for m in range(4):
        Uv = U[:, m, :].rearrange("p (c s) -> p s c", s=8)
        for j2 in range(4):
            ps, pb = C.next_ps()
            for jj in range(2):
                j = 2 * j2 + jj
                for gl in range(8):
                    mm(P, ps[:, jj * 256:(jj + 1) * 256], wide[:, j, 112 - 16 * gl:112 - 16 * gl + 128], X8[:, m * 8 + gl, :], gl == 0, gl == 7,
                       [B["c_wide_bf"], C.bX8[m * 8 + gl]], [pb])
            emit_gelu(P, C, Uv[:, 2 * j2:2 * j2 + 2, :], ps[:].rearrange("p (j c) -> p j c", j=2), pb, [B["a_U"]], [128, 2, 256])
    if S5_STOP == 4:
        return
    for tb in range(NTB):
        sl = slice(tb * TB, (tb + 1) * TB)
        pss = []
        for m in range(4):
            ps, pb = C.next_ps()
            for k in range(4):
                mm(P, ps[:], wglu[:, k, m * 128:(m + 1) * 128], U[:, k, sl], k == 0, k == 3, [wglu_buf, B["a_U"]], [pb])
            pss.append((ps, pb))
        for m in range(4):
            ps, pb = pss[m]
            v_act(P, T["g_gate"][:, m, :], ps[:], AF.Sigmoid, [pb, bglu_buf], [B["g_gate"]], bias=bglu[:, m:m + 1])
        for m in range(4):
            v_tt(P, "dve", U[:, m, sl], U[:, m, sl], T["g_gate"][:, m, :], ALU.mult, [B["a_U"], B["g_gate"]], [B["a_U"]])


def emit_gelu(P, C, out, src, src_buf, out_bufs, shape):
    T, B = C.t, C.b
    n = 1
    for d_ in shape[1:]:
        n *= d_
    x = T["g_x"][:, 0:n]
    t = T["g_t"][:, 0:n]
    if len(shape) == 3:
        x = x.rearrange("p (a b) -> p a b", a=shape[1])
        t = t.rearrange("p (a b) -> p a b", a=shape[1])
    v_cp(P, "act", x, src, [src_buf], [B["g_x"]])
    v_tt(P, "pool", t, x, x, ALU.mult, [B["g_x"]], [B["g_t"]])
    v_ts(P, "pool", t, t, GELU_C * GELU_S, GELU_S, ALU.mult, ALU.add, [B["g_t"]], [B["g_t"]])
    v_tt(P, "pool", t, t, x, ALU.mult, [B["g_t"], B["g_x"]], [B["g_t"]])
    v_act(P, t, t, AF.Sigmoid, [B["g_t"]], [B["g_t"]])
    v_tt(P, "dve", out, x, t, ALU.mult, [B["g_x"], B["g_t"]], out_bufs)


def alloc_gelu(C):
    C.sb("g_x", [128, 512], F32)
    C.sb("g_t", [128, 512], F32)
    C.sb("g_gate", [128, 4, 512], BF16)


def load_consts(P, C, cst):
    T, B = C.t, C.b
    C.sb("c_ident", [128, 128], F32)
    C.sb("c_ident_bf", [128, 128], BF16)
    C.sb("c_wide_bf", [128, 8, 240], BF16)
    C.sb("c_maskF", [128, 128], F32)
    C.sb("c_maskB", [128, 128], F32)
    C.sb("c_kvec", [128, NK], F32)
    P.dma("sp", T["c_ident"][:], cst["c_ident"], writes=[B["c_ident"]])
    P.dma("pool", T["c_ident_bf"][:], cst["c_ident"], writes=[B["c_ident_bf"]])
    P.dma("pool", T["c_wide_bf"][:].rearrange("p a w -> p (a w)"), cst["c_wide"], writes=[B["c_wide_bf"]])
    P.dma("sp", T["c_maskF"][:], cst["c_maskF"], writes=[B["c_maskF"]])
    P.dma("sp", T["c_maskB"][:], cst["c_maskB"], writes=[B["c_maskB"]])
    P.dma("sp", T["c_kvec"][:], cst["c_kvec"], writes=[B["c_kvec"]])


def load_w(P, C, name, shape, src_ap, dtype=BF16, eng=None):
    t = C.sb(name, shape, dtype)
    if eng is None:
        eng = "pool" if dtype != F32 else "sp"
    dst = t[:]
    P.dma(eng, dst, src_ap, writes=[C.b[name]])
    return t


def kview(ap2d, ncols=None):
    return ap2d.rearrange("(k p) c -> p k c", p=128)


def emit_rstd(P, C, var_ap, var_buf):
    v_ts(P, "dve", var_ap, var_ap, LN_EPS, None, ALU.add, None, [var_buf], [var_buf])
    v_act(P, var_ap, var_ap, AF.Sqrt, [var_buf], [var_buf])
    P.op("dve", lambda e: e.reciprocal(out=var_ap, in_=var_ap), reads=[var_buf], writes=[var_buf])


def emit_gmlp(P, C, xb, W, ygm_out, ygm_buf):
    T, B = C.t, C.b
    wuv = load_w(P, C, "wuv", [128, 8, 1024], kview(W["w_in"])[:, :, 512:1536])
    wsT = load_w(P, C, "wsT", [128, 8, 128], W["wsT"].rearrange("p (g i) -> p g i", g=8))
    lng = load_w(P, C, "lng", [128, 512], W["lng"], F32)
    lnb = load_w(P, C, "lnb", [128, 512], W["lnb"], F32)
    bsT = load_w(P, C, "bsT", [128, 8], W["bsT"], F32)
    C.sb("m_u", [128, 512], BF16)
    C.sb("m_v", [128, 512], F32)
    C.sb("m_vb", [128, 512], BF16)
    C.sb("m_sq", [128, 512], F32)
    C.sb("m_st", [128, 4], F32)
    C.sb("m_t", [128, 512], F32)
    C.sb("m_y", [128, 512], BF16)
    C.sb("m_yf", [128, 4, 128], BF16)
    idb = T["c_ident_bf"]
    for tt in range(NT // 128):
        tok = slice(tt * 128, (tt + 1) * 128)
        psu, pbu = C.next_ps()
        psv, pbv = C.next_ps()
        for kd in range(8):
            mm(P, psu[:], xb[:, kd, tok], wuv[:, kd, 0:512], kd == 0, kd == 7, [B["xb"], B["wuv"]], [pbu])
        for kd in range(8):
            mm(P, psv[:], xb[:, kd, tok], wuv[:, kd, 512:1024], kd == 0, kd == 7, [B["xb"], B["wuv"]], [pbv])
        emit_gelu(P, C, T["m_u"][:], psu[:], pbu, [B["m_u"]], [128, 512])
        emit_gelu(P, C, T["m_v"][:], psv[:], pbv, [B["m_v"]], [128, 512])
        st = T["m_st"]
        P.op("dve", lambda e: e.reduce_sum(out=st[:, 0:1], in_=T["m_v"][:], axis=AX.X), reads=[B["m_v"]], writes=[B["m_st"]])
        v_tt(P, "pool", T["m_sq"][:], T["m_v"][:], T["m_v"][:], ALU.mult, [B["m_v"]], [B["m_sq"]])
        P.op("dve", lambda e: e.reduce_sum(out=st[:, 1:2], in_=T["m_sq"][:], axis=AX.X), reads=[B["m_sq"]], writes=[B["m_st"]])
        v_ts(P, "dve", st[:, 0:2], st[:, 0:2], 1.0 / 512.0, None, ALU.mult, None, [B["m_st"]], [B["m_st"]])
        v_tt(P, "dve", st[:, 2:3], st[:, 0:1], st[:, 0:1], ALU.mult, [B["m_st"]], [B["m_st"]])
        v_tt(P, "dve", st[:, 1:2], st[:, 1:2], st[:, 2:3], ALU.subtract, [B["m_st"]], [B["m_st"]])
        emit_rstd(P, C, st[:, 1:2], B["m_st"])
        P.op("dve", lambda e: e.tensor_scalar(out=T["m_t"][:], in0=T["m_v"][:], scalar1=st[:, 0:1], scalar2=st[:, 1:2],
                                              op0=ALU.subtract, op1=ALU.mult), reads=[B["m_v"], B["m_st"]], writes=[B["m_t"]])
        v_tt(P, "pool", T["m_t"][:], T["m_t"][:], lng[:], ALU.mult, [B["m_t"], B["lng"]], [B["m_t"]])
        v_tt(P, "dve", T["m_vb"][:], T["m_t"][:], lnb[:], ALU.add, [B["m_t"], B["lnb"]], [B["m_vb"]])
        pss, pbs = C.next_ps()
        for g in range(8):
            mm(P, pss[:, g * 64:(g + 1) * 64], wsT[:, g, :], T["m_vb"][:, g * 64:(g + 1) * 64], True, True, [B["wsT"], B["m_vb"]], [pbs])
        v_tt(P, "dve", T["m_t"][:].rearrange("p (g d) -> p g d", g=8), pss[:].rearrange("p (g d) -> p g d", g=8),
             bsT[:].unsqueeze(2).broadcast_to([128, 8, 64]), ALU.add, [pbs, B["bsT"]], [B["m_t"]])
        v_tt(P, "dve", T["m_y"][:], T["m_t"][:], T["m_u"][:], ALU.mult, [B["m_t"], B["m_u"]], [B["m_y"]])
        pst, pbt = C.next_ps()
        for m in range(4):
            mm(P, pst[:, m * 128:(m + 1) * 128], T["m_y"][:, m * 128:(m + 1) * 128], idb[:], True, True, [B["m_y"], B["c_ident_bf"]], [pbt])
        v_cp(P, "act", T["m_yf"][:].rearrange("p m t -> p (m t)"), pst[:], [pbt], [B["m_yf"]])
        P.dma("sp", ygm_out.rearrange("(m p) t -> p m t", p=128)[:, :, tok], T["m_yf"][:], reads=[B["m_yf"]], writes=[ygm_buf])


def emit_memattn(P, C, xb, W, memT, ymem_out, ymem_buf):
    T, B = C.t, C.b
    wq = load_w(P, C, "wq", [128, 8, 512], kview(W["w_in"])[:, :, 1536:2048])
    wkv = load_w(P, C, "wkv", [128, 8, 1024], kview(W["w_kv"]))
    mT = load_w(P, C, "mT", [128, 8, 256], kview(memT))
    C.sb("k_kT", [128, 4, 256], BF16)
    C.sb("k_v", [128, 2, 512], BF16)
    C.sb("k_q", [128, 4, NT], BF16)
    C.sb("k_e", [128, 2, 512], BF16)
    C.sb("k_rs", [128, 512], F32)
    C.sb("k_o", [128, 4, 512], BF16)
    C.sb("k_ones", [128, 128], BF16)
    P.op("dve", lambda e: e.memset(T["k_ones"][:], 1.0), writes=[B["k_ones"]])
    for h in range(4):
        ps, pb = C.next_ps()
        for kd in range(8):
            mm(P, ps[:, 0:256], wkv[:, kd, h * 128:(h + 1) * 128], mT[:, kd, :], kd == 0, kd == 7, [B["wkv"], B["mT"]], [pb])
        v_cp(P, "act", T["k_kT"][:, h, :], ps[:, 0:256], [pb], [B["k_kT"]])
    for mt in range(2):
        ps, pb = C.next_ps()
        for kd in range(8):
            mm(P, ps[:], mT[:, kd, mt * 128:(mt + 1) * 128], wkv[:, kd, 512:1024], kd == 0, kd == 7, [B["mT"], B["wkv"]], [pb])
        v_cp(P, "dve", T["k_v"][:, mt, :], ps[:], [pb], [B["k_v"]])
    for tb in range(NTB):
        sl = slice(tb * TB, (tb + 1) * TB)
        for h in range(4):
            ps, pb = C.next_ps()
            for kd in range(8):
                mm(P, ps[:], wq[:, kd, h * 128:(h + 1) * 128], xb[:, kd, sl], kd == 0, kd == 7, [B["wq"], B["xb"]], [pb])
            v_cp(P, "act" if h % 2 else "dve", T["k_q"][:, h, sl], ps[:], [pb], [B["k_q"]])
    scale = float(128.0 ** -0.5)
    for tb in range(NTB):
        sl = slice(tb * TB, (tb + 1) * TB)
        for h in range(4):
            for mt in range(2):
                ps, pb = C.next_ps()
                mm(P, ps[:], T["k_kT"][:, h, mt * 128:(mt + 1) * 128], T["k_q"][:, h, sl], True, True, [B["k_kT"], B["k_q"]], [pb])
                v_act(P, T["k_e"][:, mt, :], ps[:], AF.Exp, [pb], [B["k_e"]], scale=scale)
            pss, pbs = C.next_ps()
            pso, pbo = C.next_ps()
            for mt in range(2):
                mm(P, pss[:], T["k_ones"][:], T["k_e"][:, mt, :], mt == 0, mt == 1, [B["k_ones"], B["k_e"]], [pbs])
            for mt in range(2):
                mm(P, pso[:], T["k_v"][:, mt, h * 128:(h + 1) * 128], T["k_e"][:, mt, :], mt == 0, mt == 1, [B["k_v"], B["k_e"]], [pbo])
            P.op("dve", lambda e, pss=pss: e.reciprocal(out=T["k_rs"][:], in_=pss[:]), reads=[pbs], writes=[B["k_rs"]])
            v_tt(P, "dve", T["k_o"][:, h, :], pso[:], T["k_rs"][:], ALU.mult, [pbo, B["k_rs"]], [B["k_o"]])
        P.dma("sp", ymem_out.rearrange("(m p) t -> p m t", p=128)[:, :, sl], T["k_o"][:], reads=[B["k_o"]], writes=[ymem_buf])


def emit_ln_block(P, C, z, zbuf, nblk, g_t, b_t, pref, sink):
    T, B = C.t, C.b
    ps1, pb1 = C.next_ps()
    ps2, pb2 = C.next_ps()
    for m in range(8):
        mm(P, ps1[:, 0:nblk], T["k_onesf"][:], z[:, m, 0:nblk], m == 0, m == 7, [B["k_onesf"], zbuf], [pb1])
    for m in range(8):
        sq, sqb = T[pref + "sq%d" % (m % 2)], B[pref + "sq%d" % (m % 2)]
        v_tt(P, "pool", sq[:, 0:nblk], z[:, m, 0:nblk], z[:, m, 0:nblk], ALU.mult, [zbuf], [sqb])
        mm(P, ps2[:, 0:nblk], T["k_onesf"][:], sq[:, 0:nblk], m == 0, m == 7, [B["k_onesf"], sqb], [pb2])
    mean, var = T[pref + "mean"], T[pref + "var"]
    v_ts(P, "dve", mean[:, 0:nblk], ps1[:, 0:nblk], 1.0 / D, None, ALU.mult, None, [pb1], [B[pref + "mean"]])
    v_ts(P, "dve", var[:, 0:nblk], ps2[:, 0:nblk], 1.0 / D, None, ALU.mult, None, [pb2], [B[pref + "var"]])
    sq, sqb = T[pref + "sq0"], B[pref + "sq0"]
    v_tt(P, "pool", sq[:, 0:nblk], mean[:, 0:nblk], mean[:, 0:nblk], ALU.mult, [B[pref + "mean"]], [sqb])
    v_tt(P, "dve", var[:, 0:nblk], var[:, 0:nblk], sq[:, 0:nblk], ALU.subtract, [B[pref + "var"], sqb], [B[pref + "var"]])
    emit_rstd(P, C, var[:, 0:nblk], B[pref + "var"])
    for m in range(8):
        t, tb_ = T[pref + "sq%d" % (m % 2)], B[pref + "sq%d" % (m % 2)]
        o, ob = T[pref + "o%d" % (m % 2)], B[pref + "o%d" % (m % 2)]
        v_tt(P, "dve", t[:, 0:nblk], z[:, m, 0:nblk], mean[:, 0:nblk], ALU.subtract, [zbuf, B[pref + "mean"]], [tb_])
        v_tt(P, "dve", t[:, 0:nblk], t[:, 0:nblk], var[:, 0:nblk], ALU.mult, [tb_, B[pref + "var"]], [tb_])
        v_act(P, o[:, 0:nblk], t[:, 0:nblk], AF.Identity, [tb_, B[g_t[1]], B[b_t[1]]], [ob],
              bias=b_t[0][:, m:m + 1], scale=g_t[0][:, m:m + 1])
        sink(m, o[:, 0:nblk], ob)


def alloc_ln(C, pref, nblk):
    for n in ("sq0", "sq1", "o0", "o1", "mean", "var"):
        C.sb(pref + n, [128, nblk], F32)
    if "k_onesf" not in C.t or True:
        C.sb("k_onesf", [128, 128], F32)
        C.P.op("dve", lambda e: e.memset(C.t["k_onesf"][:], 1.0), writes=[C.b["k_onesf"]])


def emit_merge(P, C, xb, W, xT_in, ybr, ybr_bufs, x1_out, x1_buf):
    T, B = C.t, C.b
    wbr = [load_w(P, C, "wbr%d" % i, [128, 4, 1024], kview(W["w_br"][i])) for i in range(3)]
    wout = load_w(P, C, "wout", [128, 8, 1024], kview(W["w_out"]))
    bg = load_w(P, C, "bg", [128, 24], W["bgT"], F32)
    g1 = load_w(P, C, "ln1g", [128, 8], W["ln1gT"], F32)
    b1 = load_w(P, C, "ln1b", [128, 8], W["ln1bT"], F32)
    wgs = [load_w(P, C, "wg%d" % i, [128, 8, 1024], kview(W["w_gate"])[:, :, i * 1024:(i + 1) * 1024]) for i in range(3)]
    for i in range(3):
        C.sb("ybr%d" % i, [128, 4, TB], BF16)
    C.sb("e_sig", [128, 512], F32)
    C.sb("e_tmp", [128, 512], F32)
    C.sb("e_acc", [128, 512], F32)
    C.sb("e_mb", [128, 8, 512], BF16)
    C.sb("e_z", [128, 8, 512], F32)
    C.sb("e_x0", [128, 512], F32)
    C.sb("e_x1", [128, 512], F32)
    alloc_ln(C, "l1_", 512)
    xv = xT_in.rearrange("(m p) t -> p m t", p=128)
    ov = x1_out.rearrange("(m p) t -> p m t", p=128)
    for tb in range(NTB):
        sl = slice(tb * TB, (tb + 1) * TB)
        for i in range(3):
            P.dma("sp", T["ybr%d" % i][:], ybr[i].rearrange("(m p) t -> p m t", p=128)[:, :, sl], reads=[ybr_bufs[i]], writes=[B["ybr%d" % i]])
        for m in range(8):
            for i in range(3):
                psg, pbg = C.next_ps()
                for kd in range(8):
                    mm(P, psg[:], wgs[i][:, kd, m * 128:(m + 1) * 128], xb[:, kd, sl], kd == 0, kd == 7, [B["wg%d" % i], B["xb"]], [pbg])
                psb, pbb = C.next_ps()
                for k in range(4):
                    mm(P, psb[:], wbr[i][:, k, m * 128:(m + 1) * 128], T["ybr%d" % i][:, k, :], k == 0, k == 3, [B["wbr%d" % i], B["ybr%d" % i]], [pbb])
                v_act(P, T["e_sig"][:], psg[:], AF.Sigmoid, [pbg, B["bg"]], [B["e_sig"]], bias=bg[:, i * 8 + m:i * 8 + m + 1])
                if i == 0:
                    v_tt(P, "dve", T["e_acc"][:], psb[:], T["e_sig"][:], ALU.mult, [pbb, B["e_sig"]], [B["e_acc"]])
                else:
                    v_tt(P, "dve", T["e_tmp"][:], psb[:], T["e_sig"][:], ALU.mult, [pbb, B["e_sig"]], [B["e_tmp"]])
                    if i == 1:
                        v_tt(P, "pool", T["e_acc"][:], T["e_acc"][:], T["e_tmp"][:], ALU.add, [B["e_acc"], B["e_tmp"]], [B["e_acc"]])
                    else:
                        v_tt(P, "dve", T["e_mb"][:, m, :], T["e_acc"][:], T["e_tmp"][:], ALU.add, [B["e_acc"], B["e_tmp"]], [B["e_mb"]])
        for m in range(8):
            pso, pbo = C.next_ps()
            for k in range(8):
                mm(P, pso[:], wout[:, k, m * 128:(m + 1) * 128], T["e_mb"][:, k, :], k == 0, k == 7, [B["wout"], B["e_mb"]], [pbo])
            ex, exb = T["e_x%d" % (m % 2)], B["e_x%d" % (m % 2)]
            P.dma("sp", ex[:], xv[:, m, sl], writes=[exb])
            P.op("dve", lambda e, m=m, pso=pso, ex=ex: e.scalar_tensor_tensor(out=T["e_z"][:, m, :], in0=ex[:], scalar=ALPHA, in1=pso[:],
                                                                             op0=ALU.mult, op1=ALU.add), reads=[exb, pbo], writes=[B["e_z"]])

        def sink(m, ap, buf, sl=sl):
            P.dma("sp", ov[:, m, sl], ap, reads=[buf], writes=[x1_buf])
        emit_ln_block(P, C, T["e_z"], B["e_z"], 512, (g1, "ln1g"), (b1, "ln1b"), "l1_", sink)


def emit_moe(P, C, W, x1_in, x1_buf, x_out, xout_buf):
    T, B = C.t, C.b
    acc = C.sb("x_acc", [128, 8, NT], F32)
    x1b = C.sb("x_1b", [128, 8, NT], BF16)
    P.dma("sp", acc[:], x1_in.rearrange("(m p) t -> p m t", p=128), reads=[x1_buf], writes=[B["x_acc"]])
    wr = load_w(P, C, "wr", [128, 8, 36], kview(W["wr"]), F32)
    br = load_w(P, C, "br", [128, 36], W["br"], F32)
    g2 = load_w(P, C, "ln2g", [128, 8], W["ln2gT"], F32)
    b2 = load_w(P, C, "ln2b", [128, 8], W["ln2bT"], F32)
    selE = load_w(P, C, "selE", [128, 32, 128], W["c_selE"].rearrange("p (e m) -> p e m", e=32))
    alloc_ln(C, "l2_", 512)
    NTT = NT // 128
    lg = C.sb("r_lg", [128, NTT, 36], F32)
    for half in range(2):
        ps, pb = C.next_ps()
        for t8 in range(8):
            tt = half * 8 + t8
            for kd in range(8):
                mm(P, ps[:, t8 * 36:(t8 + 1) * 36], acc[:, kd, tt * 128:(tt + 1) * 128], wr[:, kd, :], kd == 0, kd == 7, [B["x_acc"], B["wr"]], [pb])
        v_tt(P, "dve", lg[:, half * 8:(half + 1) * 8, :], ps[:, 0:288].rearrange("p (t c) -> p t c", c=36),
             br[:].unsqueeze(1).broadcast_to([128, 8, 36]), ALU.add, [pb, B["br"]], [B["r_lg"]])
    for m in range(8):
        v_cp(P, "act" if m % 2 else "dve", x1b[:, m, :], acc[:, m, :], [B["x_acc"]], [B["x_1b"]])
    for m in range(8):
        v_ts(P, "pool", acc[:, m, :], acc[:, m, :], ALPHA, None, ALU.mult, None, [B["x_acc"], B["x_1b"]], [B["x_acc"]])
    for n, shp in (("r_mg", [128, NTT]), ("r_goh", [128, NTT, 4]), ("r_eg", [128, NTT, 4]), ("r_pg", [128, NTT]),
                   ("r_t48", [128, NTT, 4, 8]), ("r_les", [128, NTT, 8]), ("r_m1", [128, NTT]), ("r_oh1", [128, NTT, 8]),
                   ("r_le2", [128, NTT, 8]), ("r_m2", [128, NTT]), ("r_oh2", [128, NTT, 8]), ("r_pe1", [128, NTT]),
                   ("r_pe2", [128, NTT]), ("r_w8", [128, NTT, 8]), ("r_W32", [128, NTT, 4, 8])):
        C.sb(n, shp, F32)
    e = "dve"
    lgg = lg[:, :, 0:4]
    le = lg[:, :, 4:36].rearrange("p t (g e) -> p t g e", g=4)
    s3 = [128, NTT, 4]
    s8 = [128, NTT, 8]
    s48 = [128, NTT, 4, 8]
    P.op(e, lambda en: en.tensor_reduce(out=T["r_mg"][:], in_=lgg, axis=AX.X, op=ALU.max), reads=[B["r_lg"]], writes=[B["r_mg"]])
    v_tt(P, e, T["r_goh"][:], lgg, T["r_mg"][:].unsqueeze(2).broadcast_to(s3), ALU.is_equal, [B["r_lg"], B["r_mg"]], [B["r_goh"]])
    v_tt(P, e, T["r_eg"][:], lgg, T["r_mg"][:].unsqueeze(2).broadcast_to(s3), ALU.subtract, [B["r_lg"], B["r_mg"]], [B["r_eg"]])
    v_act(P, T["r_eg"][:], T["r_eg"][:], AF.Exp, [B["r_eg"]], [B["r_eg"]])
    P.op(e, lambda en: en.reduce_sum(out=T["r_pg"][:], in_=T["r_eg"][:], axis=AX.X), reads=[B["r_eg"]], writes=[B["r_pg"]])
    P.op(e, lambda en: en.reciprocal(out=T["r_pg"][:], in_=T["r_pg"][:]), reads=[B["r_pg"]], writes=[B["r_pg"]])
    v_tt(P, e, T["r_t48"][:], le, T["r_goh"][:].unsqueeze(3).broadcast_to(s48), ALU.mult, [B["r_lg"], B["r_goh"]], [B["r_t48"]])
    P.op(e, lambda en: en.reduce_sum(out=T["r_les"][:], in_=T["r_t48"][:].rearrange("p t g e -> p t e g"), axis=AX.X),
         reads=[B["r_t48"]], writes=[B["r_les"]])
    P.op(e, lambda en: en.tensor_reduce(out=T["r_m1"][:], in_=T["r_les"][:], axis=AX.X, op=ALU.max), reads=[B["r_les"]], writes=[B["r_m1"]])
    v_tt(P, e, T["r_oh1"][:], T["r_les"][:], T["r_m1"][:].unsqueeze(2).broadcast_to(s8), ALU.is_equal, [B["r_les"], B["r_m1"]], [B["r_oh1"]])
    P.op(e, lambda en: en.scalar_tensor_tensor(out=T["r_le2"][:], in0=T["r_oh1"][:], scalar=-1.0e30, in1=T["r_les"][:], op0=ALU.mult, op1=ALU.add),
         reads=[B["r_oh1"], B["r_les"]], writes=[B["r_le2"]])
    P.op(e, lambda en: en.tensor_reduce(out=T["r_m2"][:], in_=T["r_le2"][:], axis=AX.X, op=ALU.max), reads=[B["r_le2"]], writes=[B["r_m2"]])
    v_tt(P, e, T["r_oh2"][:], T["r_le2"][:], T["r_m2"][:].unsqueeze(2).broadcast_to(s8), ALU.is_equal, [B["r_le2"], B["r_m2"]], [B["r_oh2"]])
    v_tt(P, e, T["r_pe1"][:], T["r_m2"][:], T["r_m1"][:], ALU.subtract, [B["r_m1"], B["r_m2"]], [B["r_pe1"]])
    v_act(P, T["r_pe1"][:], T["r_pe1"][:], AF.Exp, [B["r_pe1"]], [B["r_pe1"]])
    v_ts(P, e, T["r_pe1"][:], T["r_pe1"][:], 1.0, None, ALU.add, None, [B["r_pe1"]], [B["r_pe1"]])
    P.op(e, lambda en: en.reciprocal(out=T["r_pe1"][:], in_=T["r_pe1"][:]), reads=[B["r_pe1"]], writes=[B["r_pe1"]])
    v_ts(P, e, T["r_pe2"][:], T["r_pe1"][:], -1.0, 1.0, ALU.mult, ALU.add, [B["r_pe1"]], [B["r_pe2"]])
    v_tt(P, e, T["r_pe1"][:], T["r_pe1"][:], T["r_pg"][:], ALU.mult, [B["r_pe1"], B["r_pg"]], [B["r_pe1"]])
    v_tt(P, e, T["r_pe2"][:], T["r_pe2"][:], T["r_pg"][:], ALU.mult, [B["r_pe2"], B["r_pg"]], [B["r_pe2"]])
    v_tt(P, e, T["r_w8"][:], T["r_oh1"][:], T["r_pe1"][:].unsqueeze(2).broadcast_to(s8), ALU.mult, [B["r_oh1"], B["r_pe1"]], [B["r_w8"]])
    v_tt(P, e, T["r_oh2"][:], T["r_oh2"][:], T["r_pe2"][:].unsqueeze(2).broadcast_to(s8), ALU.mult, [B["r_oh2"], B["r_pe2"]], [B["r_oh2"]])
    v_tt(P, e, T["r_w8"][:], T["r_w8"][:], T["r_oh2"][:], ALU.add, [B["r_w8"], B["r_oh2"]], [B["r_w8"]])
    v_tt(P, e, T["r_W32"][:], T["r_goh"][:].unsqueeze(3).broadcast_to(s48), T["r_w8"][:].unsqueeze(2).broadcast_to(s48), ALU.mult,
         [B["r_goh"], B["r_w8"]], [B["r_W32"]])
    wT = C.sb("r_wT", [128, NT], BF16)
    P.op("dve", lambda en: en.memset(wT[:], 0.0), writes=[B["r_wT"]])
    for q in range(NTT // 4):
        ps, pb = C.next_ps()
        for j in range(4):
            tt = q * 4 + j
            mm(P, ps[0:32, j * 128:(j + 1) * 128], T["r_W32"][:, tt].rearrange("p g e -> p (g e)"), T["c_ident"][:], True, True,
               [B["r_W32"], B["c_ident"]], [pb])
        v_cp(P, "dve", wT[0:32, q * 512:(q + 1) * 512], ps[0:32, :], [pb], [B["r_wT"]])
    NB = 256
    for par in range(2):
        for j in range(2):
            C.sb("xg%d%d" % (par, j), [128, 8, 256], BF16)
            C.sb("xu%d%d" % (par, j), [128, 8, 256], BF16)
            C.sb("xd%d%d" % (par, j), [128, 2, 1024], BF16)
    for n in ("x_sg", "x_h"):
        C.sb(n, [128, 2, NB], F32)
    for j in range(2):
        C.sb("x_h2%d" % j, [128, 2, NB], BF16)
    for j in range(2):
        C.sb("x_ev%d" % j, [128, 2, NB], F32)
    C.ps_lim = 4
    C.ps_rr = 0
    ev_rr = 0
    for pair in range(16):
        par = pair % 2
        for j in range(2):
            ex = pair * 2 + j
            P.dma("pool", T["xg%d%d" % (par, j)][:], kview(W["w_exp_gate"][ex]), writes=[B["xg%d%d" % (par, j)]])
            P.dma("pool", T["xu%d%d" % (par, j)][:], kview(W["w_exp_up"][ex]), writes=[B["xu%d%d" % (par, j)]])
            P.dma("pool", T["xd%d%d" % (par, j)][:], kview(W["w_exp_down"][ex]), writes=[B["xd%d%d" % (par, j)]])
        for tb in range(NT // NB):
            sl = slice(tb * NB, (tb + 1) * NB)
            for j in range(2):
                ex = pair * 2 + j
                wg_, wu_ = T["xg%d%d" % (par, j)], T["xu%d%d" % (par, j)]
                psg, pbg = C.next_ps()
                psu, pbu = C.next_ps()
                psw, pbw = C.next_ps()
                for f in range(2):
                    for kd in range(8):
                        mm(P, psg[:, f * NB:(f + 1) * NB], wg_[:, kd, f * 128:(f + 1) * 128], x1b[:, kd, sl], kd == 0, kd == 7,
                           [B["xg%d%d" % (par, j)], B["x_1b"]], [pbg])
                for f in range(2):
                    for kd in range(8):
                        mm(P, psu[:, f * NB:(f + 1) * NB], wu_[:, kd, f * 128:(f + 1) * 128], x1b[:, kd, sl], kd == 0, kd == 7,
                           [B["xu%d%d" % (par, j)], B["x_1b"]], [pbu])
                mm(P, psw[:, 0:NB], selE[:, ex, :], wT[:, sl], True, True, [B["selE"], B["r_wT"]], [pbw])
                v_act(P, T["x_sg"][:].rearrange("p f n -> p (f n)"), psg[:], AF.Silu, [pbg], [B["x_sg"]])
                v_tt(P, "dve", T["x_h"][:].rearrange("p f n -> p (f n)"), T["x_sg"][:].rearrange("p f n -> p (f n)"), psu[:], ALU.mult,
                     [B["x_sg"], pbu], [B["x_h"]])
                v_tt(P, "dve", T["x_h2%d" % j][:], T["x_h"][:], psw[:, 0:NB].unsqueeze(1).broadcast_to([128, 2, NB]), ALU.mult,
                     [B["x_h"], pbw], [B["x_h2%d" % j]])
            for m in range(8):
                psa, pba = C.ps[4 + m // 2], C.psb[4 + m // 2]
                o = psa[:, (m % 2) * NB:(m % 2 + 1) * NB]
                for j in range(2):
                    for f in range(2):
                        mm(P, o, T["xd%d%d" % (par, j)][:, f, m * 128:(m + 1) * 128], T["x_h2%d" % j][:, f, :], j == 0 and f == 0, j == 1 and f == 1,
                           [B["xd%d%d" % (par, j)], B["x_h2%d" % j]], [pba])
            for bk in range(4):
                psa, pba = C.ps[4 + bk], C.psb[4 + bk]
                ev, evb = T["x_ev%d" % ev_rr], B["x_ev%d" % ev_rr]
                ev_rr = 1 - ev_rr
                v_cp(P, "act", ev[:].rearrange("p f n -> p (f n)"), psa[:], [pba], [evb])
                v_tt(P, "pool", acc[:, 2 * bk:2 * bk + 2, sl], acc[:, 2 * bk:2 * bk + 2, sl], ev[:], ALU.add, [B["x_acc"], evb], [B["x_acc"]])
    C.ps_lim = 8
    ov = x_out.rearrange("(m p) t -> p m t", p=128)
    for tb in range(NTB):
        sl = slice(tb * TB, (tb + 1) * TB)

        def sink(m, ap, buf, sl=sl):
            P.dma("sp", ov[:, m, sl], ap, reads=[buf], writes=[xout_buf])
        emit_ln_block(P, C, acc[:, :, sl], B["x_acc"], 512, (g2, "ln2g"), (b2, "ln2b"), "l2_", sink)


def host_layer_weights(inp, l):
    f = np.float32
    c = np.ascontiguousarray
    W = {}
    W["w_in"] = c(inp["w_in"][l])
    W["w_gate"] = c(inp["w_gate"][l])
    W["bgT"] = c(inp["b_gate"][l].reshape(24, 128).T)
    W["w_glu"] = c(inp["w_glu"][l])
    W["bglu"] = c(inp["b_glu"][l].reshape(4, 128).T)
    W["wsT"] = c(np.transpose(inp["w_spatial"][l], (2, 0, 1)).reshape(128, 1024))
    W["lng"] = c(np.broadcast_to(inp["gmlp_ln_g"][l][None, :], (128, 512)))
    W["lnb"] = c(np.broadcast_to(inp["gmlp_ln_b"][l][None, :], (128, 512)))
    W["bsT"] = c(inp["b_spatial"][l].T)
    W["w_kv"] = c(inp["w_kv"][l])
    W["w_br"] = c(inp["w_br"][l])
    W["w_out"] = c(inp["w_out"][l])
    W["ln1gT"] = c(inp["ln1_g"][l].reshape(8, 128).T)
    W["ln1bT"] = c(inp["ln1_b"][l].reshape(8, 128).T)
    W["wr"] = c(np.concatenate([inp["w_router_g"][l], np.transpose(inp["w_router_e"][l], (1, 0, 2)).reshape(D, 32)], axis=1))
    brow = np.concatenate([inp["b_router_g"][l], inp["b_router_e"][l].reshape(32)])
    W["br"] = c(np.broadcast_to(brow[None, :], (128, 36)))
    W["w_exp_gate"] = c(inp["w_exp_gate"][l])
    W["w_exp_up"] = c(inp["w_exp_up"][l])
    W["w_exp_down"] = c(inp["w_exp_down"][l])
    W["ln2gT"] = c(inp["ln2_g"][l].reshape(8, 128).T)
    W["ln2bT"] = c(inp["ln2_b"][l].reshape(8, 128).T)
    for k, v in host_s5_params(inp, l).items():
        W["s5_" + k] = v
    return {k: np.asarray(v, dtype=f) for k, v in W.items()}


def host_const_all():
    cst = host_consts()
    selE = np.zeros((128, 32, 128), np.float32)
    for e_ in range(32):
        selE[e_, e_, :] = 1.0
    cst["c_selE"] = np.ascontiguousarray(selE.reshape(128, 32 * 128))
    return cst


W_SHAPES = {
    "w_in": [1024, 2048], "w_gate": [1024, 3072], "bgT": [128, 24], "w_glu": [512, 512], "bglu": [128, 4], "wsT": [128, 1024],
    "lng": [128, 512], "lnb": [128, 512], "bsT": [128, 8], "w_kv": [1024, 1024], "w_br": [3, 512, 1024], "w_out": [1024, 1024],
    "ln1gT": [128, 8], "ln1bT": [128, 8], "wr": [1024, 36], "br": [128, 36], "w_exp_gate": [32, 1024, 256], "w_exp_up": [32, 1024, 256],
    "w_exp_down": [32, 256, 1024], "ln2gT": [128, 8], "ln2bT": [128, 8],
    "s5_lam_re": [128, 32], "s5_lam_im": [128, 32], "s5_lstep": [128, 32], "s5_dm": [128, 32],
    "s5_b_re": [128, 512], "s5_b_im": [128, 512], "s5_c_re": [128, 512], "s5_c_im": [128, 512],
}
A_KEYS = ["w_in", "s5_lam_re", "s5_lam_im", "s5_lstep", "s5_dm", "s5_b_re", "s5_b_im", "s5_c_re", "s5_c_im"]
CONST_SHAPES = {"c_ident": [128, 128], "c_wide": [128, 1920], "c_maskF": [128, 128], "c_maskB": [128, 128], "c_kvec": [128, NK],
                "c_selE": [128, 4096]}


def emit_s5_stage(P, C, W, xT, pred, lout, ybr0, ybr0_buf, stop_after_scan):
    T, B = C.t, C.b
    C.push()
    alloc_s5_persist(C)
    C.push()
    alloc_s5_prep(C)
    emit_s5_prep(P, C, {k[3:]: W[k] for k in W if k.startswith("s5_")})
    C.pop()
    alloc_s5_main(C)
    Hs = T["a_Hs"]
    if pred is None:
        P.op("dve", lambda e: e.memset(Hs[:, 0], 0.0), writes=[B["a_Hs"]])
    else:
        pr = C.sb("a_pr", [128, 3, 2, 32], F32)
        P.dma("sp", pr[:], pred.rearrange("m p (r g) -> p m r g", r=2), writes=[B["a_pr"]])
        st = T["a_st"]
        v_cp(P, "dve", Hs[:, 0], pr[:, 0], [B["a_pr"]], [B["a_Hs"]])
        Pm = T["q_P"]
        for m in range(2):
            cmul(P, "dve", (st[:, 0, 0], B["a_st"]), (st[:, 0, 1], B["a_st"]), (Pm[:, m, 0], B["q_P"]), (Pm[:, m, 1], B["q_P"]),
                 (pr[:, m + 1, 0], B["a_pr"]), (pr[:, m + 1, 1], B["a_pr"]), (st[:, 1, 0], B["a_st"]))
            v_tt(P, "dve", Hs[:, 0], Hs[:, 0], st[:, 0], ALU.add, [B["a_Hs"], B["a_st"]], [B["a_Hs"]])
    C.push()
    xb = load_w(P, C, "xb", [128, 8, NT], kview(xT))
    win = load_w(P, C, "win", [128, 8, 512], kview(W["w_in"])[:, :, 0:512])
    emit_s5_front(P, C, xb, B["xb"], win, B["win"])
    C.pop()
    emit_s5_mid(P, C)
    if stop_after_scan:
        P.dma("sp", lout, Hs[:, NCH].rearrange("p r g -> p (r g)"), reads=[B["a_Hs"]], writes=[Buf("lout")])
        C.pop()
        return
    alloc_gelu(C)
    wglu = load_w(P, C, "wglu", [128, 4, 512], kview(W["w_glu"]))
    bglu = load_w(P, C, "bglu", [128, 4], W["bglu"], F32)
    emit_s5_back(P, C, wglu, B["wglu"], bglu, B["bglu"])
    P.dma("sp", ybr0.rearrange("(m p) t -> p m t", p=128), T["a_U"][:], reads=[B["a_U"]], writes=[ybr0_buf])
    C.pop()


def build_program(mode, stages=("s5", "gmlp", "attn", "merge", "moe")):
    nc = bass.Bass("TRN2", target_bir_lowering=False)

    def din(n, shape):
        return nc.dram_tensor(n, list(shape), F32, kind="ExternalInput").ap()
    xT = din("xT", [D, NT])
    cst = {k: din(k, v) for k, v in CONST_SHAPES.items()}
    keys = A_KEYS if mode == "A" else list(W_SHAPES.keys())
    W = {k: din(k, W_SHAPES[k]) for k in keys}
    W["c_selE"] = cst["c_selE"]
    P = Prog(nc)
    C = Ctx(P)
    C.alloc_psum()
    load_consts(P, C, cst)
    T, B = C.t, C.b
    if mode == "A":
        lout = nc.dram_tensor("lout", [128, 64], F32, kind="ExternalOutput").ap()
        emit_s5_stage(P, C, W, xT, None, lout, None, None, True)
        P.finish()
        return nc
    memT = din("memT", [D, 256])
    pred = din("pred", [3, 128, 64])
    xout = nc.dram_tensor("xout", [D, NT], F32, kind="ExternalOutput").ap()
    ybr = [nc.dram_tensor("ybr%d" % i, [512, NT], BF16).ap() for i in range(3)]
    ybr_b = [Buf("ybr%d" % i) for i in range(3)]
    x1T = nc.dram_tensor("x1T", [D, NT], F32).ap()
    x1_b = Buf("x1T")
    xo_b = Buf("xout")
    if "s5" in stages:
        emit_s5_stage(P, C, W, xT, pred, None, ybr[0], ybr_b[0], False)
    if "gmlp" in stages:
        C.push()
        xb = load_w(P, C, "xb", [128, 8, NT], kview(xT))
        alloc_gelu(C)
        emit_gmlp(P, C, xb, W, ybr[1], ybr_b[1])
        C.pop()
    if "attn" in stages:
        C.push()
        xb = load_w(P, C, "xb", [128, 8, NT], kview(xT))
        emit_memattn(P, C, xb, W, memT, ybr[2], ybr_b[2])
        C.pop()
    if "merge" in stages:
        C.push()
        xb = load_w(P, C, "xb", [128, 8, NT], kview(xT))
        emit_merge(P, C, xb, W, xT, ybr, ybr_b, x1T, x1_b)
        C.pop()
    if "moe" in stages:
        C.push()
        emit_moe(P, C, W, x1T, x1_b, xout, xo_b)
        C.pop()
    print("n_ops", P.n_ops)
    P.finish()
    return nc


_PROGS = {}


def _prog(mode):
    if mode not in _PROGS:
        _PROGS[mode] = build_program(mode)
    return _PROGS[mode]


def kernel(**inputs):
    inp = {k: np.asarray(v) for k, v in inputs.items()}
    x = inp["x"].astype(np.float32)
    mem = inp["mem"].astype(np.float32)
    n = 8
    cst = host_const_all()
    xT = []
    memT = []
    for c_ in range(n):
        b, j = c_ // 4, c_ % 4
        xT.append(np.ascontiguousarray(x[b, j * NT:(j + 1) * NT].T))
        memT.append(np.ascontiguousarray(mem[b].T))
    for l in range(DEPTH):
        Wl = host_layer_weights(inp, l)
        ncA = _prog("A")
        mapsA = []
        for c_ in range(n):
            m_ = {"xT": xT[c_]}
            m_.update(cst)
            m_.update({k: Wl[k] for k in A_KEYS})
            mapsA.append(m_)
        resA = run_bass_kernel_spmd(ncA, mapsA, core_ids=list(range(n)))
        lo = [np.asarray(r["lout"], dtype=np.float32) for r in resA.results]
        ncB = _prog("B")
        mapsB = []
        for c_ in range(n):
            b, j = c_ // 4, c_ % 4
            pred = np.zeros((3, 128, 64), np.float32)
            for m in range(3):
                if j - 1 - m >= 0:
                    pred[m, 0:64] = lo[b * 4 + j - 1 - m][0:64]
                if j + 1 + m <= 3:
                    pred[m, 64:128] = lo[b * 4 + j + 1 + m][64:128]
            m_ = {"xT": xT[c_], "memT": memT[c_], "pred": pred}
            m_.update(cst)
            m_.update(Wl)
            mapsB.append(m_)
        resB = run_bass_kernel_spmd(ncB, mapsB, core_ids=list(range(n)))
        xT = [np.asarray(r["xout"], dtype=np.float32) for r in resB.results]
    out = np.zeros((2, 8192, D), np.float32)
    for c_ in range(n):
        b, j = c_ // 4, c_ % 4
        out[b, j * NT:(j + 1) * NT] = xT[c_].T
    return out


def prog_coll(P, kind, ins, outs, reads=(), writes=()):
    eng = "pool"
    waits = P._waits(eng, reads, writes)
    key = P.dma_keys[eng][P.dma_rr[eng]]
    P.dma_rr[eng] = (P.dma_rr[eng] + 1) % len(P.dma_keys[eng])
    prev = P.cnt[key]
    if prev > 0 and P.seen[eng].get(key, 0) < prev:
        P.seen[eng][key] = prev
        waits.append((key, prev))
    P.cnt[key] += 16
    tick = (key, P.cnt[key])
    kk = dict(op=ALU.bypass, replica_groups=[list(range(8))], ins=ins, outs=outs)
    P.q[eng].append((waits, ("collective_compute", (kind,), kk), key, 16))
    P._commit(tick, reads, writes)
    P.n_ops += 1
```

```python
import numpy as np
from contextlib import ExitStack
import concourse.bass as bass
import concourse.mybir as mybir
from concourse.bass_utils import run_bass_kernel_spmd

F32 = mybir.dt.float32
BF16 = mybir.dt.bfloat16
I32 = mybir.dt.int32
AF = mybir.ActivationFunctionType
ALU = mybir.AluOpType
AX = mybir.AxisListType


class Buf:
    __slots__ = ("name", "w", "r", "excl")

    def __init__(self, name="", excl=False):
        self.name = name
        self.excl = excl
        self.w = None
        self.r = {}


import os as _os0
SAME_ENGINE_SYNC = _os0.environ.get('SAME_ENGINE_SYNC', '1') == '1'


class _Rec:
    def __init__(self):
        self.call = None

    def __getattr__(self, name):
        def f(*a, **k):
            self.call = (name, a, k)
            return self
        return f


class Prog:
    ENGS = ("pe", "act", "dve", "pool", "sp")

    def __init__(self, nc, n_dma_sems=16, same_engine_sync=SAME_ENGINE_SYNC):
        self.nc = nc
        self.es = ExitStack()
        self.q = {e: [] for e in self.ENGS}
        self.cnt = {}
        self.seen = {e: {} for e in self.ENGS}
        self.sem = {}
        for e in self.ENGS:
            self.sem["c_" + e] = self.es.enter_context(nc.semaphore("c_" + e))
            self.cnt["c_" + e] = 0
        self.dma_keys = {}
        self.dma_rr = {}
        for qe in ("sp", "pool", "act"):
            self.dma_keys[qe] = []
            self.dma_rr[qe] = 0
            for i in range(n_dma_sems if qe != "act" else 4):
                k = "d_%s_%d" % (qe, i)
                self.sem[k] = self.es.enter_context(nc.semaphore(k))
                self.cnt[k] = 0
                self.dma_keys[qe].append(k)
        self.same_engine_sync = same_engine_sync
        self.n_ops = 0
        self.pending = {e: {} for e in self.ENGS}
        self.open_scopes = []

    def sbuf(self, name, shape, dtype):
        return self.es.enter_context(self.nc.sbuf_tensor(name, list(shape), dtype))

    def psum(self, name, shape, dtype=F32):
        return self.es.enter_context(self.nc.psum_tensor(name, list(shape), dtype))

    def _waits(self, eng, reads, writes):
        deps = {}
        for b in reads:
            if b.w is not None:
                k, v = b.w
                deps[k] = max(deps.get(k, 0), v)
        for b in writes:
            if b.w is not None:
                k, v = b.w
                deps[k] = max(deps.get(k, 0), v)
            for k, v in b.r.items():
                deps[k] = max(deps.get(k, 0), v)
        for k, v in self.pending[eng].items():
            deps[k] = max(deps.get(k, 0), v)
        self.pending[eng] = {}
        waits = []
        own = "c_" + eng
        for k, v in deps.items():
            if k == own and (eng == "pe" or not self.same_engine_sync):
                continue
            if self.seen[eng].get(k, 0) >= v:
                continue
            self.seen[eng][k] = v
            waits.append((k, v))
        return waits

    def _commit(self, tick, reads, writes):
        k, v = tick
        for b in reads:
            b.r[k] = max(b.r.get(k, 0), v)
        for b in writes:
            b.w = tick
            b.r = {}

    def op(self, eng, fn, reads=(), writes=()):
        ex = [b for b in reads if b.excl]
        if ex:
            writes = list(writes) + ex
        waits = self._waits(eng, reads, writes)
        key = "c_" + eng
        self.cnt[key] += 1
        tick = (key, self.cnt[key])
        rec = _Rec()
        fn(rec)
        self.q[eng].append((waits, rec.call, key, 1))
        self._commit(tick, reads, writes)
        self.n_ops += 1

    def dma(self, eng, out, in_, reads=(), writes=(), **kw):
        waits = self._waits(eng, reads, writes)
        key = self.dma_keys[eng][self.dma_rr[eng]]
        self.dma_rr[eng] = (self.dma_rr[eng] + 1) % len(self.dma_keys[eng])
        prev = self.cnt[key]
        if prev > 0 and self.seen[eng].get(key, 0) < prev:
            self.seen[eng][key] = prev
            waits.append((key, prev))
        self.cnt[key] += 16
        tick = (key, self.cnt[key])
        kk = dict(kw)
        kk["out"] = out
        kk["in_"] = in_
        self.q[eng].append((waits, ("dma_start", (), kk), key, 16))
        self._commit(tick, reads, writes)
        self.n_ops += 1

    def barrier(self):
        for e in self.ENGS:
            for k, v in self.cnt.items():
                if v > 0:
                    self.pending[e][k] = max(self.pending[e].get(k, 0), v)

    def finish(self, final_bufs=()):
        fin = []
        for k, v in self.cnt.items():
            if v > 0 and self.seen["sp"].get(k, 0) < v and k != "c_sp":
                fin.append((k, v))
        nc = self.nc
        sem = self.sem
        q = self.q
        with nc.Block() as block:
            def emit(e, lst, extra=()):
                for waits, call, key, inc in lst:
                    for k, v in waits:
                        e.wait_ge(sem[k], v)
                    name, a, kw = call
                    getattr(e, name)(*a, **kw).then_inc(sem[key], inc)
                for k, v in extra:
                    e.wait_ge(sem[k], v)

            @block.sync
            def _(e):
                emit(e, q["sp"], fin)

            @block.scalar
            def _(e):
                emit(e, q["act"])

            @block.vector
            def _(e):
                emit(e, q["dve"])

            @block.gpsimd
            def _(e):
                emit(e, q["pool"])

            @block.tensor
            def _(e):
                emit(e, q["pe"])
        while self.open_scopes:
            self.open_scopes.pop().close()
        self.es.close()


D = 1024
NT = 2048
TB = 512
NTB = NT // TB
NCH = NT // 8
SCAN_K = 8
SCAN_L = NCH // SCAN_K
DEPTH = 4
ALPHA = (2.0 * DEPTH) ** 0.25
LN_EPS = 1e-5
KVEC = [0, -1, -2, -3, -4, -5, -6, -7, 0, 1, 2, 3, 4, 5, 6, 7, 1, 7, 8]
NK = len(KVEC)
MAGIC = 12582912.0
TWO_PI_HI = 6.28125
TWO_PI_LO = 2.0 * np.pi - 6.28125
GELU_C = 0.044715
GELU_S = 2.0 * 0.7978845608028654


def host_consts():
    ident = np.eye(128, dtype=np.float32)
    wide = np.zeros((128, 8, 240), np.float32)
    for a in range(8):
        for hc in range(16):
            wide[16 * a + hc, a, 112 + hc] = 1.0
    s_idx = np.arange(128) // 16
    maskF = (s_idx[None, :] >= s_idx[:, None]).astype(np.float32)
    maskB = (s_idx[:, None] >= s_idx[None, :]).astype(np.float32)
    maskF = maskF - maskB
    kvec = np.tile(np.array(KVEC, np.float32)[None, :], (128, 1))
    return dict(c_ident=ident, c_wide=np.ascontiguousarray(wide.reshape(128, 8 * 240)),
                c_maskF=maskF, c_maskB=maskB, c_kvec=kvec)


def host_s5_params(inp, l):
    f = np.float32

    def dp(a):
        return np.ascontiguousarray(np.transpose(a, (0, 2, 1)).reshape(128, 32)).astype(f)
    lam_re = dp(inp["ssm_lam_re"][l])
    lam_im = dp(inp["ssm_lam_im"][l])
    lstep = np.ascontiguousarray(np.broadcast_to(inp["ssm_log_step"][l][:, None, :], (2, 64, 32)).reshape(128, 32)).astype(f)

    def rep_b(a):
        t = np.transpose(a, (1, 0, 2))
        return np.ascontiguousarray(np.concatenate([t, t], 0).reshape(128, 32 * 16)).astype(f)

    def rep_c(a):
        t = np.transpose(a, (2, 0, 1))
        return np.ascontiguousarray(np.concatenate([t, t], 0).reshape(128, 32 * 16)).astype(f)
    dm = inp["ssm_d"][l].reshape(32, 16)
    dm = np.ascontiguousarray(np.tile(dm.T, (8, 1))).astype(f)
    return dict(lam_re=lam_re, lam_im=lam_im, lstep=lstep,
                b_re=rep_b(inp["ssm_b_re"][l]), b_im=rep_b(inp["ssm_b_im"][l]),
                c_re=rep_c(inp["ssm_c_re"][l]), c_im=rep_c(inp["ssm_c_im"][l]), dm=dm)


class Ctx:
    def __init__(self, P):
        self.P = P
        self.t = {}
        self.b = {}
        self.ps = []
        self.psb = []
        self.ps_rr = 0
        self.scopes = P.open_scopes
        self.uid = 0
        self.ps_lim = 8

    def sb(self, name, shape, dtype, nbuf=None):
        es = self.scopes[-1] if self.scopes else self.P.es
        self.uid += 1
        self.t[name] = es.enter_context(self.P.nc.sbuf_tensor("%s_%d" % (name, self.uid), list(shape), dtype))
        self.b[name] = Buf(name)
        return self.t[name]

    def push(self):
        self.scopes.append(ExitStack())

    def pop(self):
        self.P.barrier()
        self.scopes.pop().close()

    def alloc_psum(self):
        for i in range(8):
            self.ps.append(self.P.psum("psb%d" % i, [128, 512], F32))
            self.psb.append(Buf("psb%d" % i, excl=True))

    def next_ps(self):
        i = self.ps_rr
        self.ps_rr = (self.ps_rr + 1) % self.ps_lim
        return self.ps[i], self.psb[i]


def v_tt(P, eng, out, a, b, op, r, w):
    P.op(eng, lambda e: e.tensor_tensor(out=out, in0=a, in1=b, op=op), reads=r, writes=w)


def v_ts(P, eng, out, a, s1, s2, op0, op1, r, w):
    if s2 is None:
        P.op(eng, lambda e: e.tensor_scalar(out=out, in0=a, scalar1=s1, scalar2=None, op0=op0), reads=r, writes=w)
    else:
        P.op(eng, lambda e: e.tensor_scalar(out=out, in0=a, scalar1=s1, scalar2=s2, op0=op0, op1=op1), reads=r, writes=w)


def v_cp(P, eng, out, a, r, w):
    if eng == "act":
        P.op(eng, lambda e: e.copy(out=out, in_=a), reads=r, writes=w)
    else:
        P.op(eng, lambda e: e.tensor_copy(out=out, in_=a), reads=r, writes=w)


def v_act(P, out, a, func, r, w, bias=None, scale=None):
    kw = {}
    if bias is not None:
        kw["bias"] = bias
    if scale is not None:
        kw["scale"] = scale
    P.op("act", lambda e: e.activation(out=out, in_=a, func=func, **kw), reads=r, writes=w)


def mm(P, out, lhsT, rhs, start, stop, r, w):
    P.op("pe", lambda e: e.matmul(out, lhsT, rhs, start=start, stop=stop), reads=r, writes=w)


def cmul(P, eng, o_re, o_im, a_re, a_im, b_re, b_im, tmp):
    v_tt(P, eng, o_re[0], a_re[0], b_re[0], ALU.mult, [a_re[1], b_re[1]], [o_re[1]])
    v_tt(P, eng, tmp[0], a_im[0], b_im[0], ALU.mult, [a_im[1], b_im[1]], [tmp[1]])
    v_tt(P, eng, o_re[0], o_re[0], tmp[0], ALU.subtract, [o_re[1], tmp[1]], [o_re[1]])
    v_tt(P, eng, o_im[0], a_re[0], b_im[0], ALU.mult, [a_re[1], b_im[1]], [o_im[1]])
    v_tt(P, eng, tmp[0], a_im[0], b_re[0], ALU.mult, [a_im[1], b_re[1]], [tmp[1]])
    v_tt(P, eng, o_im[0], o_im[0], tmp[0], ALU.add, [o_im[1], tmp[1]], [o_im[1]])


def cmul_big(P, eng, o_re, o_im, a_re, a_im, b_re, b_im, tmp):
    for g0 in (0, 16):
        def sl(v):
            return (v[0][:, g0:g0 + 16], v[1])
        cmul(P, eng, sl(o_re), sl(o_im), sl(a_re), sl(a_im), sl(b_re), sl(b_im), (tmp[0], tmp[1]))


def alloc_s5_persist(C):
    C.sb("q_A2", [128, 2, 2, 32], F32)
    C.sb("q_P", [128, 3, 2, 32], F32)
    C.sb("w_Glre", [128, 32, 8, 16], BF16)
    C.sb("w_GlimN", [128, 32, 8, 16], BF16)
    C.sb("w_Msum", [128, 32, 128], BF16)
    C.sb("w_Wl", [128, 32, 2, 128], BF16)


def alloc_s5_prep(C):
    for n in ("p_lam_re", "p_lam_im", "p_lstep", "p_dm"):
        C.sb(n, [128, 32], F32)
    for n in ("p_b_re", "p_b_im", "p_c_re", "p_c_im"):
        C.sb(n, [128, 32, 16], F32)
    for n in ("q_step", "q_a", "q_b", "q_den", "q_em1", "q_t1", "q_t2", "q_bre", "q_bim"):
        C.sb(n, [128, 32], F32)
    for n in ("q_ka", "q_kb", "q_n", "q_r", "q_mag", "q_Ere", "q_Eim"):
        C.sb(n, [128, 32, NK], F32)
    for n in ("q_bBre", "q_bBim", "q_t3"):
        C.sb(n, [128, 32, 16], F32)
    for n in ("s_are", "s_aim", "s_bre", "s_bim", "s_cre", "s_cim"):
        C.sb(n, [128, 32, 8, 16], F32)
    C.sb("s_tmp", [128, 16, 8, 16], F32)
    C.sb("s_m1", [128, 128], F32)
    C.sb("s_m2", [128, 128], F32)


def emit_s5_prep(P, C, prm):
    T, B = C.t, C.b

    def V(n, ap=None):
        return (T[n][:] if ap is None else ap, B[n])
    for n in ("lam_re", "lam_im", "lstep", "dm"):
        P.dma("sp", T["p_" + n][:], prm[n], writes=[B["p_" + n]])
    for n in ("b_re", "b_im", "c_re", "c_im"):
        P.dma("sp", T["p_" + n][:].rearrange("p g h -> p (g h)"), prm[n], writes=[B["p_" + n]])
    e = "dve"
    v_act(P, T["q_step"][:], T["p_lstep"][:], AF.Exp, [B["p_lstep"]], [B["q_step"]])
    v_tt(P, e, T["q_a"][:], T["p_lam_re"][:], T["q_step"][:], ALU.mult, [B["p_lam_re"], B["q_step"]], [B["q_a"]])
    v_tt(P, e, T["q_b"][:], T["p_lam_im"][:], T["q_step"][:], ALU.mult, [B["p_lam_im"], B["q_step"]], [B["q_b"]])
    kv = T["c_kvec"][:].unsqueeze(1).broadcast_to([128, 32, NK])
    sh3 = [128, 32, NK]
    v_tt(P, e, T["q_ka"][:], T["q_a"][:].unsqueeze(2).broadcast_to(sh3), kv, ALU.mult, [B["q_a"], B["c_kvec"]], [B["q_ka"]])
    v_tt(P, e, T["q_kb"][:], T["q_b"][:].unsqueeze(2).broadcast_to(sh3), kv, ALU.mult, [B["q_b"], B["c_kvec"]], [B["q_kb"]])
    v_act(P, T["q_mag"][:], T["q_ka"][:], AF.Exp, [B["q_ka"]], [B["q_mag"]])
    inv2pi = float(1.0 / (2.0 * np.pi))
    for which, off, dst in (("sin", 0.0, "q_Eim"), ("cos", 0.25, "q_Ere")):
        v_ts(P, e, T["q_n"][:], T["q_kb"][:], inv2pi, off, ALU.mult, ALU.add, [B["q_kb"]], [B["q_n"]])
        v_ts(P, e, T["q_n"][:], T["q_n"][:], MAGIC, None, ALU.add, None, [B["q_n"]], [B["q_n"]])
        v_ts(P, e, T["q_n"][:], T["q_n"][:], -MAGIC, None, ALU.add, None, [B["q_n"]], [B["q_n"]])
        P.op(e, lambda en: en.scalar_tensor_tensor(out=T["q_r"][:], in0=T["q_n"][:], scalar=-TWO_PI_HI, in1=T["q_kb"][:],
                                                   op0=ALU.mult, op1=ALU.add), reads=[B["q_n"], B["q_kb"]], writes=[B["q_r"]])
        P.op(e, lambda en: en.scalar_tensor_tensor(out=T["q_r"][:], in0=T["q_n"][:], scalar=-TWO_PI_LO, in1=T["q_r"][:],
                                                   op0=ALU.mult, op1=ALU.add), reads=[B["q_n"], B["q_r"]], writes=[B["q_r"]])
        if off != 0.0:
            v_ts(P, e, T["q_r"][:], T["q_r"][:], float(np.pi / 2), None, ALU.add, None, [B["q_r"]], [B["q_r"]])
        v_ts(P, e, T["q_r"][:], T["q_r"][:], float(np.pi), float(-np.pi), ALU.min, ALU.max, [B["q_r"]], [B["q_r"]])
        v_act(P, T[dst][:], T["q_r"][:], AF.Sin, [B["q_r"]], [B[dst]])
    v_tt(P, e, T["q_Ere"][:], T["q_mag"][:], T["q_Ere"][:], ALU.mult, [B["q_mag"], B["q_Ere"]], [B["q_Ere"]])
    v_tt(P, e, T["q_Eim"][:], T["q_mag"][:], T["q_Eim"][:], ALU.mult, [B["q_mag"], B["q_Eim"]], [B["q_Eim"]])
    Ere, Eim = T["q_Ere"], T["q_Eim"]
    v_tt(P, e, T["q_den"][:], T["p_lam_re"][:], T["p_lam_re"][:], ALU.mult, [B["p_lam_re"]], [B["q_den"]])
    v_tt(P, e, T["q_t1"][:], T["p_lam_im"][:], T["p_lam_im"][:], ALU.mult, [B["p_lam_im"]], [B["q_t1"]])
    v_tt(P, e, T["q_den"][:], T["q_den"][:], T["q_t1"][:], ALU.add, [B["q_den"], B["q_t1"]], [B["q_den"]])
    P.op(e, lambda en: en.reciprocal(out=T["q_den"][:], in_=T["q_den"][:]), reads=[B["q_den"]], writes=[B["q_den"]])
    v_ts(P, e, T["q_em1"][:], Ere[:, :, 16], -1.0, None, ALU.add, None, [B["q_Ere"]], [B["q_em1"]])
    v_tt(P, e, T["q_bre"][:], T["q_em1"][:], T["p_lam_re"][:], ALU.mult, [B["q_em1"], B["p_lam_re"]], [B["q_bre"]])
    v_tt(P, e, T["q_t1"][:], Eim[:, :, 16], T["p_lam_im"][:], ALU.mult, [B["q_Eim"], B["p_lam_im"]], [B["q_t1"]])
    v_tt(P, e, T["q_bre"][:], T["q_bre"][:], T["q_t1"][:], ALU.add, [B["q_bre"], B["q_t1"]], [B["q_bre"]])
    v_tt(P, e, T["q_bim"][:], Eim[:, :, 16], T["p_lam_re"][:], ALU.mult, [B["q_Eim"], B["p_lam_re"]], [B["q_bim"]])
    v_tt(P, e, T["q_t2"][:], T["q_em1"][:], T["p_lam_im"][:], ALU.mult, [B["q_em1"], B["p_lam_im"]], [B["q_t2"]])
    v_tt(P, e, T["q_bim"][:], T["q_bim"][:], T["q_t2"][:], ALU.subtract, [B["q_bim"], B["q_t2"]], [B["q_bim"]])
    v_tt(P, e, T["q_bre"][:], T["q_bre"][:], T["q_den"][:], ALU.mult, [B["q_bre"], B["q_den"]], [B["q_bre"]])
    v_tt(P, e, T["q_bim"][:], T["q_bim"][:], T["q_den"][:], ALU.mult, [B["q_bim"], B["q_den"]], [B["q_bim"]])
    s16 = [128, 32, 16]
    bre_b = (T["q_bre"][:].unsqueeze(2).broadcast_to(s16), B["q_bre"])
    bim_b = (T["q_bim"][:].unsqueeze(2).broadcast_to(s16), B["q_bim"])
    cmul(P, e, V("q_bBre"), V("q_bBim"), bre_b, bim_b, V("p_b_re"), V("p_b_im"), V("q_t3"))
    A2 = T["q_A2"]
    v_cp(P, e, A2[:, 0, 0, :], Ere[:, :, 18], [B["q_Ere"]], [B["q_A2"]])
    v_cp(P, e, A2[:, 0, 1, :], Ere[:, :, 18], [B["q_Ere"]], [B["q_A2"]])
    v_ts(P, e, A2[:, 1, 0, :], Eim[:, :, 18], -1.0, None, ALU.mult, None, [B["q_Eim"]], [B["q_A2"]])
    v_cp(P, e, A2[:, 1, 1, :], Eim[:, :, 18], [B["q_Eim"]], [B["q_A2"]])
    Pm = T["q_P"]
    v_cp(P, e, Pm[:, 0, 0, :], Ere[:, :, 18], [B["q_Ere"]], [B["q_P"]])
    v_cp(P, e, Pm[:, 0, 1, :], Eim[:, :, 18], [B["q_Eim"]], [B["q_P"]])

    def csq(dst_re, dst_im, a_re, a_im, b_re, b_im):
        v_tt(P, e, T["q_t1"][:], a_re, b_re, ALU.mult, [B["q_P"]], [B["q_t1"]])
        v_tt(P, e, T["q_t2"][:], a_im, b_im, ALU.mult, [B["q_P"]], [B["q_t2"]])
        v_tt(P, e, T["q_t1"][:], T["q_t1"][:], T["q_t2"][:], ALU.subtract, [B["q_t1"], B["q_t2"]], [B["q_t1"]])
        v_tt(P, e, T["q_t2"][:], a_re, b_im, ALU.mult, [B["q_P"]], [B["q_t2"]])
        v_tt(P, e, T["q_den"][:], a_im, b_re, ALU.mult, [B["q_P"]], [B["q_den"]])
        v_tt(P, e, dst_im, T["q_t2"][:], T["q_den"][:], ALU.add, [B["q_t2"], B["q_den"]], [B["q_P"]])
        v_cp(P, e, dst_re, T["q_t1"][:], [B["q_t1"]], [B["q_P"]])
    for _ in range(8):
        csq(Pm[:, 0, 0, :], Pm[:, 0, 1, :], Pm[:, 0, 0, :], Pm[:, 0, 1, :], Pm[:, 0, 0, :], Pm[:, 0, 1, :])
    csq(Pm[:, 1, 0, :], Pm[:, 1, 1, :], Pm[:, 0, 0, :], Pm[:, 0, 1, :], Pm[:, 0, 0, :], Pm[:, 0, 1, :])
    csq(Pm[:, 2, 0, :], Pm[:, 2, 1, :], Pm[:, 1, 0, :], Pm[:, 1, 1, :], Pm[:, 0, 0, :], Pm[:, 0, 1, :])
    s4 = [128, 32, 8, 16]

    def Ek(lo, hi):
        return ((Ere[:, :, lo:hi].unsqueeze(3).broadcast_to(s4), B["q_Ere"]),
                (Eim[:, :, lo:hi].unsqueeze(3).broadcast_to(s4), B["q_Eim"]))

    def E1(idx):
        return ((Ere[:, :, idx:idx + 1].unsqueeze(3).broadcast_to(s4), B["q_Ere"]),
                (Eim[:, :, idx:idx + 1].unsqueeze(3).broadcast_to(s4), B["q_Eim"]))
    bB_re = (T["q_bBre"][:].unsqueeze(2).broadcast_to(s4), B["q_bBre"])
    bB_im = (T["q_bBim"][:].unsqueeze(2).broadcast_to(s4), B["q_bBim"])
    def ctab(o_re_n, o_im_n, a_re, a_im, b_re, b_im, rev_bwd):
        for lo, hi, rev in ((0, 64, False), (64, 128, rev_bwd)):
            o_re = (T[o_re_n][lo:hi, :, ::-1, :] if rev else T[o_re_n][lo:hi], B[o_re_n])
            o_im = (T[o_im_n][lo:hi, :, ::-1, :] if rev else T[o_im_n][lo:hi], B[o_im_n])
            cmul_big(P, e, o_re, o_im, (a_re[0][lo:hi], a_re[1]), (a_im[0][lo:hi], a_im[1]),
                     (b_re[0][lo:hi], b_re[1]), (b_im[0][lo:hi], b_im[1]), (T["s_tmp"][lo:hi], B["s_tmp"]))
    er, ei = Ek(0, 8)
    ctab("s_are", "s_aim", er, ei, bB_re, bB_im, True)
    c_re = (T["p_c_re"][:].unsqueeze(2).broadcast_to(s4), B["p_c_re"])
    c_im = (T["p_c_im"][:].unsqueeze(2).broadcast_to(s4), B["p_c_im"])
    er, ei = Ek(8, 16)
    ctab("s_bre", "s_bim", er, ei, c_re, c_im, True)
    e1r, e1i = E1(16)
    ctab("s_cre", "s_cim", e1r, e1i, V("s_bre"), V("s_bim"), False)
    v_cp(P, "act", T["w_Glre"][:], T["s_cre"][:], [B["s_cre"]], [B["w_Glre"]])
    v_ts(P, e, T["w_GlimN"][:], T["s_cim"][:], -1.0, None, ALU.mult, None, [B["s_cim"]], [B["w_GlimN"]])
    e7r, e7i = E1(17)
    ctab("s_cre", "s_cim", e7r, e7i, V("s_are"), V("s_aim"), False)
    v_ts(P, e, T["s_bim"][:], T["s_bim"][:], -1.0, None, ALU.mult, None, [B["s_bim"]], [B["s_bim"]])
    idf = T["c_ident"]

    def fl(ap):
        return ap.rearrange("p s h -> p (s h)")
    for g in range(32):
        psA, pbA = C.next_ps()
        mm(P, psA[:, 0:128], fl(T["s_are"][0:64, g]), fl(T["s_bre"][0:64, g]), True, False, [B["s_are"], B["s_bre"]], [pbA])
        mm(P, psA[:, 0:128], fl(T["s_aim"][0:64, g]), fl(T["s_bim"][0:64, g]), False, True, [B["s_aim"], B["s_bim"]], [pbA])
        mm(P, psA[:, 128:256], fl(T["s_are"][:, g]), fl(T["s_bre"][:, g]), True, False, [B["s_are"], B["s_bre"]], [pbA])
        mm(P, psA[:, 128:256], fl(T["s_aim"][:, g]), fl(T["s_bim"][:, g]), False, True, [B["s_aim"], B["s_bim"]], [pbA])
        for ri, tn in ((0, "s_cre"), (1, "s_cim")):
            o = 256 + ri * 128
            mm(P, psA[:, o:o + 128], fl(T[tn][:, g]), idf[:, :], True, True, [B[tn], B["c_ident"]], [pbA])
        v_tt(P, e, T["s_m1"][:], psA[:, 0:128], T["c_maskF"][:], ALU.mult, [pbA, B["c_maskF"]], [B["s_m1"]])
        v_tt(P, e, T["s_m2"][:], psA[:, 128:256], T["c_maskB"][:], ALU.mult, [pbA, B["c_maskB"]], [B["s_m2"]])
        v_tt(P, "pool", T["s_m1"][:], T["s_m1"][:], T["s_m2"][:], ALU.add, [B["s_m1"], B["s_m2"]], [B["s_m1"]])
        P.op("dve", lambda en, g=g: en.scalar_tensor_tensor(out=T["w_Msum"][:, g, :], in0=T["c_ident"][:], scalar=T["p_dm"][:, g:g + 1],
                                                             in1=T["s_m1"][:], op0=ALU.mult, op1=ALU.add),
             reads=[B["c_ident"], B["p_dm"], B["s_m1"]], writes=[B["w_Msum"]])
        v_cp(P, "act", T["w_Wl"][:, g].rearrange("p r m -> p (r m)"), psA[:, 256:512], [pbA], [B["w_Wl"]])


def alloc_s5_main(C):
    C.sb("a_U", [128, 4, NT], BF16)
    C.sb("a_X8", [128, 32, NCH], BF16)
    C.sb("a_Hs", [128, NCH + 1 + SCAN_L, 2, 32], F32)
    C.sb("a_I", [128, SCAN_K + 1, 2, 32], F32)
    C.sb("a_P32", [128, 2, 2, 32], F32)
    C.sb("a_st", [128, 2, 2, 32], F32)
    C.sb("a_st9", [128, 2, SCAN_K + 1, 2, 32], F32)
    C.sb("a_st1", [128, 2, 2, 32], F32)
    C.bX8 = [Buf("x8_%d" % g) for g in range(32)]


def emit_s5_scan(P, C):
    T, B = C.t, C.b
    Hs, A2, st, I, P32 = T["a_Hs"], T["q_A2"], T["a_st9"], T["a_I"], T["a_P32"]
    K, L = SCAN_K, SCAN_L
    V = Hs[:, 1:1 + (K + 1) * L].rearrange("p (b i) r g -> p b i r g", b=K + 1)
    hb = [B["a_Hs"]]
    s9 = [128, K + 1, 2, 32]
    Are = A2[:, 0].unsqueeze(1).broadcast_to(s9)
    Aim = A2[:, 1].unsqueeze(1).broadcast_to(s9)
    P.op("dve", lambda e: e.memset(Hs[:, 1 + K * L:1 + (K + 1) * L], 0.0), writes=hb)
    v_cp(P, "dve", V[:, K, 0, 0, :], A2[:, 0, 0, :], [B["q_A2"]], hb)
    v_cp(P, "dve", V[:, K, 0, 1, :], A2[:, 1, 1, :], [B["q_A2"]], hb)
    for i in range(1, L):
        v_tt(P, "dve", st[:, 0], V[:, :, i - 1], Are, ALU.mult, hb + [B["q_A2"]], [B["a_st9"]])
        v_tt(P, "dve", st[:, 1], V[:, :, i - 1, ::-1, :], Aim, ALU.mult, hb + [B["q_A2"]], [C.b_st2])
        v_tt(P, "dve", V[:, :, i], V[:, :, i], st[:, 0], ALU.add, hb + [B["a_st9"]], hb)
        v_tt(P, "dve", V[:, :, i], V[:, :, i], st[:, 1], ALU.add, hb + [C.b_st2], hb)
    v_cp(P, "dve", P32[:, 0, 0, :], V[:, K, L - 1, 0, :], hb, [B["a_P32"]])
    v_cp(P, "dve", P32[:, 0, 1, :], V[:, K, L - 1, 0, :], hb, [B["a_P32"]])
    v_ts(P, "dve", P32[:, 1, 0, :], V[:, K, L - 1, 1, :], -1.0, None, ALU.mult, None, hb, [B["a_P32"]])
    v_cp(P, "dve", P32[:, 1, 1, :], V[:, K, L - 1, 1, :], hb, [B["a_P32"]])
    ib = [B["a_I"]]
    s1 = T["a_st1"]
    v_cp(P, "dve", I[:, 0], Hs[:, 0], hb, ib)
    for b_ in range(K):
        v_tt(P, "dve", s1[:, 0], I[:, b_], P32[:, 0], ALU.mult, ib + [B["a_P32"]], [B["a_st1"]])
        v_tt(P, "dve", s1[:, 1], I[:, b_, ::-1, :], P32[:, 1], ALU.mult, ib + [B["a_P32"]], [B["a_st1"]])
        v_tt(P, "dve", I[:, b_ + 1], V[:, b_, L - 1], s1[:, 0], ALU.add, hb + [B["a_st1"]], ib)
        v_tt(P, "dve", I[:, b_ + 1], I[:, b_ + 1], s1[:, 1], ALU.add, ib + [B["a_st1"]], ib)
    sF = [128, K, L, 32]
    Tm = T["a_T"]
    Pw = V[:, K]

    def pw(r):
        return Pw[:, :, r, :].unsqueeze(1).broadcast_to(sF)

    def ii(r):
        return I[:, 0:K, r, :].unsqueeze(2).broadcast_to(sF)
    for k, (pr_, ir_, out_r, op) in enumerate(((0, 0, 0, ALU.add), (1, 1, 0, ALU.subtract), (0, 1, 1, ALU.add), (1, 0, 1, ALU.add))):
        tm, tb_ = Tm[:], C.b_T[0]
        v_tt(P, "pool", tm, pw(pr_), ii(ir_), ALU.mult, hb + ib, [tb_])
        v_tt(P, "dve", V[:, 0:K, :, out_r, :], V[:, 0:K, :, out_r, :], tm, op, hb + [tb_], [C.b_Hfix[out_r]])
    P.op("dve", lambda e: e.tensor_copy(out=I[:, 0, 0, 0:1], in_=I[:, 0, 0, 0:1]), reads=[C.b_Hfix[0], C.b_Hfix[1]] + ib, writes=hb + ib)


def emit_s5_front(P, C, xb, xb_buf, win, win_buf):
    T, B = C.t, C.b
    U, X8, Hs = T["a_U"], T["a_X8"], T["a_Hs"]
    C.b_st2 = Buf("st2")
    for tb in range(NTB):
        for m in range(4):
            ps, pb = C.next_ps()
            for kd in range(8):
                mm(P, ps[:], win[:, kd, m * 128:(m + 1) * 128], xb[:, kd, tb * TB:(tb + 1) * TB], kd == 0, kd == 7,
                   [win_buf, xb_buf], [pb])
            v_cp(P, "act" if (m % 2) else "dve", U[:, m, tb * TB:(tb + 1) * TB], ps[:], [pb], [B["a_U"]])


def emit_s5_mid(P, C):
    T, B = C.t, C.b
    U, X8, Hs = T["a_U"], T["a_X8"], T["a_Hs"]
    C.push()
    C.sb("a_T", [128, SCAN_K, SCAN_L, 32], F32)
    C.b_T = [Buf("aT0")]
    C.b_Hfix = [Buf("hfix0"), Buf("hfix1")]
    wide = T["c_wide_bf"]
    for g2 in range(16):
        ps, pb = C.next_ps()
        for gg in range(2):
            g = 2 * g2 + gg
            gl, m = g % 8, g // 8
            Uv = U[:, m, :].rearrange("p (c s) -> p s c", s=8)
            for s in range(8):
                mm(P, ps[:, gg * 256:(gg + 1) * 256], wide[:, gl, 112 - 16 * s:112 - 16 * s + 128], Uv[:, s, :], s == 0, s == 7,
                   [B["c_wide_bf"], B["a_U"]], [pb])
        v_cp(P, "act" if (g2 % 2) else "dve", X8[:, 2 * g2:2 * g2 + 2, :].rearrange("p g c -> p (g c)"), ps[:], [pb],
             [C.bX8[2 * g2], C.bX8[2 * g2 + 1]])
    Wl = T["w_Wl"]
    for g in range(32):
        ps, pb = C.next_ps()
        for ri in range(2):
            mm(P, ps[:, ri * 256:(ri + 1) * 256], Wl[:, g, ri, :], X8[:, g, :], True, True, [B["w_Wl"], C.bX8[g]], [pb])
        psv = ps[:].rearrange("p (r c) -> p c r", r=2)
        v_cp(P, "act", Hs[0:64, 1:NCH + 1, :, g], psv[0:64], [pb], [B["a_Hs"]])
        v_cp(P, "dve", Hs[64:128, 1:NCH + 1, :, g], psv[64:128, ::-1, :], [pb], [B["a_Hs"]])
    emit_s5_scan(P, C)
    C.pop()


import os as _os
S5_STOP = int(_os.environ.get("S5_STOP", "0"))


def emit_s5_back(P, C, wglu, wglu_buf, bglu, bglu_buf):
    T, B = C.t, C.b
    U, X8, Hs = T["a_U"], T["a_X8"], T["a_Hs"]
    HsB = C.sb("a_HsB", [128, NCH + 1, 2, 32], BF16)
    v_cp(P, "act", HsB[0:64, 0:NCH], Hs[0:64, 0:NCH], [B["a_Hs"]], [B["a_HsB"]])
    v_cp(P, "dve", HsB[64:128, 0:NCH], Hs[64:128, NCH - 1::-1], [B["a_Hs"]], [B["a_HsB"]])
    for g2 in range(16):
        ps, pb = C.next_ps()
        for gg in range(2):
            g = 2 * g2 + gg
            o = ps[:, gg * 256:(gg + 1) * 256]
            mm(P, o, T["w_Msum"][:, g, :], X8[:, g, :], True, False, [B["w_Msum"], C.bX8[g]], [pb])
            for ri, tn in ((0, "w_Glre"), (1, "w_GlimN")):
                mm(P, o, T[tn][:, g].rearrange("p j h -> p (j h)"), HsB[:, 0:NCH, ri, g], False, ri == 1, [B[tn], B["a_HsB"]], [pb])
        v_cp(P, "act" if (g2 % 2) else "dve", X8[:, 2 * g2:2 * g2 + 2, :].rearrange("p g c -> p (g c)"), ps[:], [pb],
             [C.bX8[2 * g2], C.bX8[2 * g2 + 1]])
    if S5_STOP == 3:
        return
    wide = T["c_wide_bf"]
    for m in range(4):
        Uv = U[:, m, :].rearrange("p (c s) -> p s c", s=8)
        for j2 in range(4):
            ps, pb = C.next_ps()
            for jj in range(2):
                j = 2 * j2 + jj
                for gl in range(8):
                    mm(P, ps[:, jj * 256:(jj + 1) * 256], wide[:, j, 112 - 16 * gl:112 - 16 * gl + 128], X8[:, m * 8 + gl, :], gl == 0, gl == 7,
                       [B["c_wide_bf"], C.bX8[m * 8 + gl]], [pb])
            emit_gelu(P, C, Uv[:, 2 * j2:2 * j2 + 2, :], ps[:].rearrange("p (j c) -> p j c", j=2), pb, [B["a_U"]], [128, 2, 256])
    if S5_STOP == 4:
        return
    for tb in range(NTB):
        sl = slice(tb * TB, (tb + 1) * TB)
        pss = []
        for m in range(4):
            ps, pb = C.next_ps()
            for k in range(4):
                mm(P, ps[:], wglu[:, k, m * 128:(m + 1) * 128], U[:, k, sl], k == 0, k == 3, [wglu_buf, B["a_U"]], [pb])
            pss.append((ps, pb))
        for m in range(4):
            ps, pb = pss[m]
            v_act(P, T["g_gate"][:, m, :], ps[:], AF.Sigmoid, [pb, bglu_buf], [B["g_gate"]], bias=bglu[:, m:m + 1])
        for m in range(4):
            v_tt(P, "dve", U[:, m, sl], U[:, m, sl], T["g_gate"][:, m, :], ALU.mult, [B["a_U"], B["g_gate"]], [B["a_U"]])


def emit_gelu(P, C, out, src, src_buf, out_bufs, shape):
    T, B = C.t, C.b
    n = 1
    for d_ in shape[1:]:
        n *= d_
    x = T["g_x"][:, 0:n]
    t = T["g_t"][:, 0:n]
    if len(shape) == 3:
        x = x.rearrange("p (a b) -> p a b", a=shape[1])
        t = t.rearrange("p (a b) -> p a b", a=shape[1])
    v_cp(P, "act", x, src, [src_buf], [B["g_x"]])
    v_tt(P, "pool", t, x, x, ALU.mult, [B["g_x"]], [B["g_t"]])
    v_ts(P, "pool", t, t, GELU_C * GELU_S, GELU_S, ALU.mult, ALU.add, [B["g_t"]], [B["g_t"]])
    v_tt(P, "pool", t, t, x, ALU.mult, [B["g_t"], B["g_x"]], [B["g_t"]])
    v_act(P, t, t, AF.Sigmoid, [B["g_t"]], [B["g_t"]])
    v_tt(P, "dve", out, x, t, ALU.mult, [B["g_x"], B["g_t"]], out_bufs)


def alloc_gelu(C):
    C.sb("g_x", [128, 512], F32)
    C.sb("g_t", [128, 512], F32)
    C.sb("g_gate", [128, 4, 512], BF16)


def load_consts(P, C, cst):
    T, B = C.t, C.b
    C.sb("c_ident", [128, 128], F32)
    C.sb("c_ident_bf", [128, 128], BF16)
    C.sb("c_wide_bf", [128, 8, 240], BF16)
    C.sb("c_maskF", [128, 128], F32)
    C.sb("c_maskB", [128, 128], F32)
    C.sb("c_kvec", [128, NK], F32)
    P.dma("sp", T["c_ident"][:], cst["c_ident"], writes=[B["c_ident"]])
    P.dma("pool", T["c_ident_bf"][:], cst["c_ident"], writes=[B["c_ident_bf"]])
    P.dma("pool", T["c_wide_bf"][:].rearrange("p a w -> p (a w)"), cst["c_wide"], writes=[B["c_wide_bf"]])
    P.dma("sp", T["c_maskF"][:], cst["c_maskF"], writes=[B["c_maskF"]])
    P.dma("sp", T["c_maskB"][:], cst["c_maskB"], writes=[B["c_maskB"]])
    P.dma("sp", T["c_kvec"][:], cst["c_kvec"], writes=[B["c_kvec"]])


def load_w(P, C, name, shape, src_ap, dtype=BF16, eng=None):
    t = C.sb(name, shape, dtype)
    if eng is None:
        eng = "pool" if dtype != F32 else "sp"
    dst = t[:]
    P.dma(eng, dst, src_ap, writes=[C.b[name]])
    return t


def kview(ap2d, ncols=None):
    return ap2d.rearrange("(k p) c -> p k c", p=128)


def emit_rstd(P, C, var_ap, var_buf):
    v_ts(P, "dve", var_ap, var_ap, LN_EPS, None, ALU.add, None, [var_buf], [var_buf])
    v_act(P, var_ap, var_ap, AF.Sqrt, [var_buf], [var_buf])
    P.op("dve", lambda e: e.reciprocal(out=var_ap, in_=var_ap), reads=[var_buf], writes=[var_buf])


def emit_gmlp(P, C, xb, W, ygm_out, ygm_buf):
    T, B = C.t, C.b
    wuv = load_w(P, C, "wuv", [128, 8, 1024], kview(W["w_in"])[:, :, 512:1536])
    wsT = load_w(P, C, "wsT", [128, 8, 128], W["wsT"].rearrange("p (g i) -> p g i", g=8))
    lng = load_w(P, C, "lng", [128, 512], W["lng"], F32)
    lnb = load_w(P, C, "lnb", [128, 512], W["lnb"], F32)
    bsT = load_w(P, C, "bsT", [128, 8], W["bsT"], F32)
    C.sb("m_u", [128, 512], BF16)
    C.sb("m_v", [128, 512], F32)
    C.sb("m_vb", [128, 512], BF16)
    C.sb("m_sq", [128, 512], F32)
    C.sb("m_st", [128, 4], F32)
    C.sb("m_t", [128, 512], F32)
    C.sb("m_y", [128, 512], BF16)
    C.sb("m_yf", [128, 4, 128], BF16)
    idb = T["c_ident_bf"]
    for tt in range(NT // 128):
        tok = slice(tt * 128, (tt + 1) * 128)
        psu, pbu = C.next_ps()
        psv, pbv = C.next_ps()
        for kd in range(8):
            mm(P, psu[:], xb[:, kd, tok], wuv[:, kd, 0:512], kd == 0, kd == 7, [B["xb"], B["wuv"]], [pbu])
        for kd in range(8):
            mm(P, psv[:], xb[:, kd, tok], wuv[:, kd, 512:1024], kd == 0, kd == 7, [B["xb"], B["wuv"]], [pbv])
        emit_gelu(P, C, T["m_u"][:], psu[:], pbu, [B["m_u"]], [128, 512])
        emit_gelu(P, C, T["m_v"][:], psv[:], pbv, [B["m_v"]], [128, 512])
        st = T["m_st"]
        P.op("dve", lambda e: e.reduce_sum(out=st[:, 0:1], in_=T["m_v"][:], axis=AX.X), reads=[B["m_v"]], writes=[B["m_st"]])
        v_tt(P, "pool", T["m_sq"][:], T["m_v"][:], T["m_v"][:], ALU.mult, [B["m_v"]], [B["m_sq"]])
        P.op("dve", lambda e: e.reduce_sum(out=st[:, 1:2], in_=T["m_sq"][:], axis=AX.X), reads=[B["m_sq"]], writes=[B["m_st"]])
        v_ts(P, "dve", st[:, 0:2], st[:, 0:2], 1.0 / 512.0, None, ALU.mult, None, [B["m_st"]], [B["m_st"]])
        v_tt(P, "dve", st[:, 2:3], st[:, 0:1], st[:, 0:1], ALU.mult, [B["m_st"]], [B["m_st"]])
        v_tt(P, "dve", st[:, 1:2], st[:, 1:2], st[:, 2:3], ALU.subtract, [B["m_st"]], [B["m_st"]])
        emit_rstd(P, C, st[:, 1:2], B["m_st"])
        P.op("dve", lambda e: e.tensor_scalar(out=T["m_t"][:], in0=T["m_v"][:], scalar1=st[:, 0:1], scalar2=st[:, 1:2],
                                              op0=ALU.subtract, op1=ALU.mult), reads=[B["m_v"], B["m_st"]], writes=[B["m_t"]])
        v_tt(P, "pool", T["m_t"][:], T["m_t"][:], lng[:], ALU.mult, [B["m_t"], B["lng"]], [B["m_t"]])
        v_tt(P, "dve", T["m_vb"][:], T["m_t"][:], lnb[:], ALU.add, [B["m_t"], B["lnb"]], [B["m_vb"]])
        pss, pbs = C.next_ps()
        for g in range(8):
            mm(P, pss[:, g * 64:(g + 1) * 64], wsT[:, g, :], T["m_vb"][:, g * 64:(g + 1) * 64], True, True, [B["wsT"], B["m_vb"]], [pbs])
        v_tt(P, "dve", T["m_t"][:].rearrange("p (g d) -> p g d", g=8), pss[:].rearrange("p (g d) -> p g d", g=8),
             bsT[:].unsqueeze(2).broadcast_to([128, 8, 64]), ALU.add, [pbs, B["bsT"]], [B["m_t"]])
        v_tt(P, "dve", T["m_y"][:], T["m_t"][:], T["m_u"][:], ALU.mult, [B["m_t"], B["m_u"]], [B["m_y"]])
        pst, pbt = C.next_ps()
        for m in range(4):
            mm(P, pst[:, m * 128:(m + 1) * 128], T["m_y"][:, m * 128:(m + 1) * 128], idb[:], True, True, [B["m_y"], B["c_ident_bf"]], [pbt])
        v_cp(P, "act", T["m_yf"][:].rearrange("p m t -> p (m t)"), pst[:], [pbt], [B["m_yf"]])
        P.dma("sp", ygm_out.rearrange("(m p) t -> p m t", p=128)[:, :, tok], T["m_yf"][:], reads=[B["m_yf"]], writes=[ygm_buf])


def emit_memattn(P, C, xb, W, memT, ymem_out, ymem_buf):
    T, B = C.t, C.b
    wq = load_w(P, C, "wq", [128, 8, 512], kview(W["w_in"])[:, :, 1536:2048])
    wkv = load_w(P, C, "wkv", [128, 8, 1024], kview(W["w_kv"]))
    mT = load_w(P, C, "mT", [128, 8, 256], kview(memT))
    C.sb("k_kT", [128, 4, 256], BF16)
    C.sb("k_v", [128, 2, 512], BF16)
    C.sb("k_q", [128, 4, NT], BF16)
    C.sb("k_e", [128, 2, 512], BF16)
    C.sb("k_rs", [128, 512], F32)
    C.sb("k_o", [128, 4, 512], BF16)
    C.sb("k_ones", [128, 128], BF16)
    P.op("dve", lambda e: e.memset(T["k_ones"][:], 1.0), writes=[B["k_ones"]])
    for h in range(4):
        ps, pb = C.next_ps()
        for kd in range(8):
            mm(P, ps[:, 0:256], wkv[:, kd, h * 128:(h + 1) * 128], mT[:, kd, :], kd == 0, kd == 7, [B["wkv"], B["mT"]], [pb])
        v_cp(P, "act", T["k_kT"][:, h, :], ps[:, 0:256], [pb], [B["k_kT"]])
    for mt in range(2):
        ps, pb = C.next_ps()
        for kd in range(8):
            mm(P, ps[:], mT[:, kd, mt * 128:(mt + 1) * 128], wkv[:, kd, 512:1024], kd == 0, kd == 7, [B["mT"], B["wkv"]], [pb])
        v_cp(P, "dve", T["k_v"][:, mt, :], ps[:], [pb], [B["k_v"]])
    for tb in range(NTB):
        sl = slice(tb * TB, (tb + 1) * TB)
        for h in range(4):
            ps, pb = C.next_ps()
            for kd in range(8):
                mm(P, ps[:], wq[:, kd, h * 128:(h + 1) * 128], xb[:, kd, sl], kd == 0, kd == 7, [B["wq"], B["xb"]], [pb])
            v_cp(P, "act" if h % 2 else "dve", T["k_q"][:, h, sl], ps[:], [pb], [B["k_q"]])
    scale = float(128.0 ** -0.5)
    for tb in range(NTB):
        sl = slice(tb * TB, (tb + 1) * TB)
        for h in range(4):
            for mt in range(2):
                ps, pb = C.next_ps()
                mm(P, ps[:], T["k_kT"][:, h, mt * 128:(mt + 1) * 128], T["k_q"][:, h, sl], True, True, [B["k_kT"], B["k_q"]], [pb])
                v_act(P, T["k_e"][:, mt, :], ps[:], AF.Exp, [pb], [B["k_e"]], scale=scale)
            pss, pbs = C.next_ps()
            pso, pbo = C.next_ps()
            for mt in range(2):
                mm(P, pss[:], T["k_ones"][:], T["k_e"][:, mt, :], mt == 0, mt == 1, [B["k_ones"], B["k_e"]], [pbs])
            for mt in range(2):
                mm(P, pso[:], T["k_v"][:, mt, h * 128:(h + 1) * 128], T["k_e"][:, mt, :], mt == 0, mt == 1, [B["k_v"], B["k_e"]], [pbo])
            P.op("dve", lambda e, pss=pss: e.reciprocal(out=T["k_rs"][:], in_=pss[:]), reads=[pbs], writes=[B["k_rs"]])
            v_tt(P, "dve", T["k_o"][:, h, :], pso[:], T["k_rs"][:], ALU.mult, [pbo, B["k_rs"]], [B["k_o"]])
        P.dma("sp", ymem_out.rearrange("(m p) t -> p m t", p=128)[:, :, sl], T["k_o"][:], reads=[B["k_o"]], writes=[ymem_buf])


def emit_ln_block(P, C, z, zbuf, nblk, g_t, b_t, pref, sink):
    T, B = C.t, C.b
    ps1, pb1 = C.next_ps()
    ps2, pb2 = C.next_ps()
    for m in range(8):
        mm(P, ps1[:, 0:nblk], T["k_onesf"][:], z[:, m, 0:nblk], m == 0, m == 7, [B["k_onesf"], zbuf], [pb1])
    for m in range(8):
        sq, sqb = T[pref + "sq%d" % (m % 2)], B[pref + "sq%d" % (m % 2)]
        v_tt(P, "pool", sq[:, 0:nblk], z[:, m, 0:nblk], z[:, m, 0:nblk], ALU.mult, [zbuf], [sqb])
        mm(P, ps2[:, 0:nblk], T["k_onesf"][:], sq[:, 0:nblk], m == 0, m == 7, [B["k_onesf"], sqb], [pb2])
    mean, var = T[pref + "mean"], T[pref + "var"]
    v_ts(P, "dve", mean[:, 0:nblk], ps1[:, 0:nblk], 1.0 / D, None, ALU.mult, None, [pb1], [B[pref + "mean"]])
    v_ts(P, "dve", var[:, 0:nblk], ps2[:, 0:nblk], 1.0 / D, None, ALU.mult, None, [pb2], [B[pref + "var"]])
    sq, sqb = T[pref + "sq0"], B[pref + "sq0"]
    v_tt(P, "pool", sq[:, 0:nblk], mean[:, 0:nblk], mean[:, 0:nblk], ALU.mult, [B[pref + "mean"]], [sqb])
    v_tt(P, "dve", var[:, 0:nblk], var[:, 0:nblk], sq[:, 0:nblk], ALU.subtract, [B[pref + "var"], sqb], [B[pref + "var"]])
    emit_rstd(P, C, var[:, 0:nblk], B[pref + "var"])
    for m in range(8):
        t, tb_ = T[pref + "sq%d" % (m % 2)], B[pref + "sq%d" % (m % 2)]
        o, ob = T[pref + "o%d" % (m % 2)], B[pref + "o%d" % (m % 2)]
        v_tt(P, "dve", t[:, 0:nblk], z[:, m, 0:nblk], mean[:, 0:nblk], ALU.subtract, [zbuf, B[pref + "mean"]], [tb_])
        v_tt(P, "dve", t[:, 0:nblk], t[:, 0:nblk], var[:, 0:nblk], ALU.mult, [tb_, B[pref + "var"]], [tb_])
        v_act(P, o[:, 0:nblk], t[:, 0:nblk], AF.Identity, [tb_, B[g_t[1]], B[b_t[1]]], [ob],
              bias=b_t[0][:, m:m + 1], scale=g_t[0][:, m:m + 1])
        sink(m, o[:, 0:nblk], ob)


def alloc_ln(C, pref, nblk):
    for n in ("sq0", "sq1", "o0", "o1", "mean", "var"):
        C.sb(pref + n, [128, nblk], F32)
    if "k_onesf" not in C.t or True:
        C.sb("k_onesf", [128, 128], F32)
        C.P.op("dve", lambda e: e.memset(C.t["k_onesf"][:], 1.0), writes=[C.b["k_onesf"]])


def emit_merge(P, C, xb, W, xT_in, ybr, ybr_bufs, x1_out, x1_buf):
    T, B = C.t, C.b
    wbr = [load_w(P, C, "wbr%d" % i, [128, 4, 1024], kview(W["w_br"][i])) for i in range(3)]
    wout = load_w(P, C, "wout", [128, 8, 1024], kview(W["w_out"]))
    bg = load_w(P, C, "bg", [128, 24], W["bgT"], F32)
    g1 = load_w(P, C, "ln1g", [128, 8], W["ln1gT"], F32)
    b1 = load_w(P, C, "ln1b", [128, 8], W["ln1bT"], F32)
    wgs = [load_w(P, C, "wg%d" % i, [128, 8, 1024], kview(W["w_gate"])[:, :, i * 1024:(i + 1) * 1024]) for i in range(3)]
    for i in range(3):
        C.sb("ybr%d" % i, [128, 4, TB], BF16)
    C.sb("e_sig", [128, 512], F32)
    C.sb("e_tmp", [128, 512], F32)
    C.sb("e_acc", [128, 512], F32)
    C.sb("e_mb", [128, 8, 512], BF16)
    C.sb("e_z", [128, 8, 512], F32)
    C.sb("e_x0", [128, 512], F32)
    C.sb("e_x1", [128, 512], F32)
    alloc_ln(C, "l1_", 512)
    xv = xT_in.rearrange("(m p) t -> p m t", p=128)
    ov = x1_out.rearrange("(m p) t -> p m t", p=128)
    for tb in range(NTB):
        sl = slice(tb * TB, (tb + 1) * TB)
        for i in range(3):
            P.dma("sp", T["ybr%d" % i][:], ybr[i].rearrange("(m p) t -> p m t", p=128)[:, :, sl], reads=[ybr_bufs[i]], writes=[B["ybr%d" % i]])
        for m in range(8):
            for i in range(3):
                psg, pbg = C.next_ps()
                for kd in range(8):
                    mm(P, psg[:], wgs[i][:, kd, m * 128:(m + 1) * 128], xb[:, kd, sl], kd == 0, kd == 7, [B["wg%d" % i], B["xb"]], [pbg])
                psb, pbb = C.next_ps()
                for k in range(4):
                    mm(P, psb[:], wbr[i][:, k, m * 128:(m + 1) * 128], T["ybr%d" % i][:, k, :], k == 0, k == 3, [B["wbr%d" % i], B["ybr%d" % i]], [pbb])
                v_act(P, T["e_sig"][:], psg[:], AF.Sigmoid, [pbg, B["bg"]], [B["e_sig"]], bias=bg[:, i * 8 + m:i * 8 + m + 1])
                if i == 0:
                    v_tt(P, "dve", T["e_acc"][:], psb[:], T["e_sig"][:], ALU.mult, [pbb, B["e_sig"]], [B["e_acc"]])
                else:
                    v_tt(P, "dve", T["e_tmp"][:], psb[:], T["e_sig"][:], ALU.mult, [pbb, B["e_sig"]], [B["e_tmp"]])
                    if i == 1:
                        v_tt(P, "pool", T["e_acc"][:], T["e_acc"][:], T["e_tmp"][:], ALU.add, [B["e_acc"], B["e_tmp"]], [B["e_acc"]])
                    else:
                        v_tt(P, "dve", T["e_mb"][:, m, :], T["e_acc"][:], T["e_tmp"][:], ALU.add, [B["e_acc"], B["e_tmp"]], [B["e_mb"]])
        for m in range(8):
            pso, pbo = C.next_ps()
            for k in range(8):
                mm(P, pso[:], wout[:, k, m * 128:(m + 1) * 128], T["e_mb"][:, k, :], k == 0, k == 7, [B["wout"], B["e_mb"]], [pbo])
            ex, exb = T["e_x%d" % (m % 2)], B["e_x%d" % (m % 2)]
            P.dma("sp", ex[:], xv[:, m, sl], writes=[exb])
            P.op("dve", lambda e, m=m, pso=pso, ex=ex: e.scalar_tensor_tensor(out=T["e_z"][:, m, :], in0=ex[:], scalar=ALPHA, in1=pso[:],
                                                                             op0=ALU.mult, op1=ALU.add), reads=[exb, pbo], writes=[B["e_z"]])

        def sink(m, ap, buf, sl=sl):
            P.dma("sp", ov[:, m, sl], ap, reads=[buf], writes=[x1_buf])
        emit_ln_block(P, C, T["e_z"], B["e_z"], 512, (g1, "ln1g"), (b1, "ln1b"), "l1_", sink)


def emit_moe(P, C, W, x1_in, x1_buf, x_out, xout_buf):
    T, B = C.t, C.b
    acc = C.sb("x_acc", [128, 8, NT], F32)
    x1b = C.sb("x_1b", [128, 8, NT], BF16)
    P.dma("sp", acc[:], x1_in.rearrange("(m p) t -> p m t", p=128), reads=[x1_buf], writes=[B["x_acc"]])
    P.dma("pool", x1b[:], x1_in.rearrange("(m p) t -> p m t", p=128), reads=[x1_buf], writes=[B["x_1b"]])
    for par in range(2):
        for j in range(2):
            C.sb("xg%d%d" % (par, j), [128, 8, 256], BF16)
            C.sb("xu%d%d" % (par, j), [128, 8, 256], BF16)
            C.sb("xd%d%d" % (par, j), [128, 2, 1024], BF16)

    def load_pair(pair):
        par = pair % 2
        for j in range(2):
            ex = pair * 2 + j
            P.dma("pool", T["xg%d%d" % (par, j)][:], kview(W["w_exp_gate"][ex]), writes=[B["xg%d%d" % (par, j)]])
            P.dma("pool", T["xu%d%d" % (par, j)][:], kview(W["w_exp_up"][ex]), writes=[B["xu%d%d" % (par, j)]])
            P.dma("pool", T["xd%d%d" % (par, j)][:], kview(W["w_exp_down"][ex]), writes=[B["xd%d%d" % (par, j)]])
    load_pair(0)
    load_pair(1)
    wr = load_w(P, C, "wr", [128, 8, 36], kview(W["wr"]), F32)
    br = load_w(P, C, "br", [128, 36], W["br"], F32)
    g2 = load_w(P, C, "ln2g", [128, 8], W["ln2gT"], F32)
    b2 = load_w(P, C, "ln2b", [128, 8], W["ln2bT"], F32)
    selE = load_w(P, C, "selE", [128, 32, 128], W["c_selE"].rearrange("p (e m) -> p e m", e=32))
    alloc_ln(C, "l2_", 512)
    NTT = NT // 128
    lg = C.sb("r_lg", [128, NTT, 36], F32)
    for half in range(2):
        ps, pb = C.next_ps()
        for t8 in range(8):
            tt = half * 8 + t8
            for kd in range(8):
                mm(P, ps[:, t8 * 36:(t8 + 1) * 36], acc[:, kd, tt * 128:(tt + 1) * 128], wr[:, kd, :], kd == 0, kd == 7, [B["x_acc"], B["wr"]], [pb])
        v_tt(P, "dve", lg[:, half * 8:(half + 1) * 8, :], ps[:, 0:288].rearrange("p (t c) -> p t c", c=36),
             br[:].unsqueeze(1).broadcast_to([128, 8, 36]), ALU.add, [pb, B["br"]], [B["r_lg"]])
    for n, shp in (("r_mg", [128, NTT]), ("r_goh", [128, NTT, 4]), ("r_eg", [128, NTT, 4]), ("r_pg", [128, NTT]),
                   ("r_t48", [128, NTT, 4, 8]), ("r_les", [128, NTT, 8]), ("r_m1", [128, NTT]), ("r_oh1", [128, NTT, 8]),
                   ("r_le2", [128, NTT, 8]), ("r_m2", [128, NTT]), ("r_oh2", [128, NTT, 8]), ("r_pe1", [128, NTT]),
                   ("r_pe2", [128, NTT]), ("r_w8", [128, NTT, 8]), ("r_W32", [128, NTT, 4, 8])):
        C.sb(n, shp, F32)
    e = "dve"
    lgg = lg[:, :, 0:4]
    le = lg[:, :, 4:36].rearrange("p t (g e) -> p t g e", g=4)
    s3 = [128, NTT, 4]
    s8 = [128, NTT, 8]
    s48 = [128, NTT, 4, 8]
    P.op(e, lambda en: en.tensor_reduce(out=T["r_mg"][:], in_=lgg, axis=AX.X, op=ALU.max), reads=[B["r_lg"]], writes=[B["r_mg"]])
    v_tt(P, e, T["r_goh"][:], lgg, T["r_mg"][:].unsqueeze(2).broadcast_to(s3), ALU.is_equal, [B["r_lg"], B["r_mg"]], [B["r_goh"]])
    v_tt(P, e, T["r_eg"][:], lgg, T["r_mg"][:].unsqueeze(2).broadcast_to(s3), ALU.subtract, [B["r_lg"], B["r_mg"]], [B["r_eg"]])
    v_act(P, T["r_eg"][:], T["r_eg"][:], AF.Exp, [B["r_eg"]], [B["r_eg"]])
    P.op(e, lambda en: en.reduce_sum(out=T["r_pg"][:], in_=T["r_eg"][:], axis=AX.X), reads=[B["r_eg"]], writes=[B["r_pg"]])
    P.op(e, lambda en: en.reciprocal(out=T["r_pg"][:], in_=T["r_pg"][:]), reads=[B["r_pg"]], writes=[B["r_pg"]])
    v_tt(P, e, T["r_t48"][:], le, T["r_goh"][:].unsqueeze(3).broadcast_to(s48), ALU.mult, [B["r_lg"], B["r_goh"]], [B["r_t48"]])
    P.op(e, lambda en: en.reduce_sum(out=T["r_les"][:], in_=T["r_t48"][:].rearrange("p t g e -> p t e g"), axis=AX.X),
         reads=[B["r_t48"]], writes=[B["r_les"]])
    P.op(e, lambda en: en.tensor_reduce(out=T["r_m1"][:], in_=T["r_les"][:], axis=AX.X, op=ALU.max), reads=[B["r_les"]], writes=[B["r_m1"]])
    v_tt(P, e, T["r_oh1"][:], T["r_les"][:], T["r_m1"][:].unsqueeze(2).broadcast_to(s8), ALU.is_equal, [B["r_les"], B["r_m1"]], [B["r_oh1"]])
    P.op(e, lambda en: en.scalar_tensor_tensor(out=T["r_le2"][:], in0=T["r_oh1"][:], scalar=-1.0e30, in1=T["r_les"][:], op0=ALU.mult, op1=ALU.add),
         reads=[B["r_oh1"], B["r_les"]], writes=[B["r_le2"]])
    P.op(e, lambda en: en.tensor_reduce(out=T["r_m2"][:], in_=T["r_le2"][:], axis=AX.X, op=ALU.max), reads=[B["r_le2"]], writes=[B["r_m2"]])
    v_tt(P, e, T["r_oh2"][:], T["r_le2"][:], T["r_m2"][:].unsqueeze(2).broadcast_to(s8), ALU.is_equal, [B["r_le2"], B["r_m2"]], [B["r_oh2"]])
    v_tt(P, e, T["r_pe1"][:], T["r_m2"][:], T["r_m1"][:], ALU.subtract, [B["r_m1"], B["r_m2"]], [B["r_pe1"]])
    v_act(P, T["r_pe1"][:], T["r_pe1"][:], AF.Exp, [B["r_pe1"]], [B["r_pe1"]])
    v_ts(P, e, T["r_pe1"][:], T["r_pe1"][:], 1.0, None, ALU.add, None, [B["r_pe1"]], [B["r_pe1"]])
    P.op(e, lambda en: en.reciprocal(out=T["r_pe1"][:], in_=T["r_pe1"][:]), reads=[B["r_pe1"]], writes=[B["r_pe1"]])
    v_ts(P, e, T["r_pe2"][:], T["r_pe1"][:], -1.0, 1.0, ALU.mult, ALU.add, [B["r_pe1"]], [B["r_pe2"]])
    v_tt(P, e, T["r_pe1"][:], T["r_pe1"][:], T["r_pg"][:], ALU.mult, [B["r_pe1"], B["r_pg"]], [B["r_pe1"]])
    v_tt(P, e, T["r_pe2"][:], T["r_pe2"][:], T["r_pg"][:], ALU.mult, [B["r_pe2"], B["r_pg"]], [B["r_pe2"]])
    v_tt(P, e, T["r_w8"][:], T["r_oh1"][:], T["r_pe1"][:].unsqueeze(2).broadcast_to(s8), ALU.mult, [B["r_oh1"], B["r_pe1"]], [B["r_w8"]])
    v_tt(P, e, T["r_oh2"][:], T["r_oh2"][:], T["r_pe2"][:].unsqueeze(2).broadcast_to(s8), ALU.mult, [B["r_oh2"], B["r_pe2"]], [B["r_oh2"]])
    v_tt(P, e, T["r_w8"][:], T["r_w8"][:], T["r_oh2"][:], ALU.add, [B["r_w8"], B["r_oh2"]], [B["r_w8"]])
    v_tt(P, e, T["r_W32"][:], T["r_goh"][:].unsqueeze(3).broadcast_to(s48), T["r_w8"][:].unsqueeze(2).broadcast_to(s48), ALU.mult,
         [B["r_goh"], B["r_w8"]], [B["r_W32"]])
    wT = C.sb("r_wT", [128, NT], BF16)
    P.op("dve", lambda en: en.memset(wT[:], 0.0), writes=[B["r_wT"]])
    for q in range(NTT // 4):
        ps, pb = C.next_ps()
        for j in range(4):
            tt = q * 4 + j
            mm(P, ps[0:32, j * 128:(j + 1) * 128], T["r_W32"][:, tt].rearrange("p g e -> p (g e)"), T["c_ident"][:], True, True,
               [B["r_W32"], B["c_ident"]], [pb])
        v_cp(P, "dve", wT[0:32, q * 512:(q + 1) * 512], ps[0:32, :], [pb], [B["r_wT"]])
    NB = 256
    for n in ("x_sg", "x_h"):
        C.sb(n, [128, 2, NB], F32)
    for j in range(2):
        C.sb("x_h2%d" % j, [128, 2, NB], BF16)
    for j in range(2):
        C.sb("x_ev%d" % j, [128, 2, NB], F32)
    C.ps_lim = 4
    C.ps_rr = 0
    ev_rr = 0
    for pair in range(16):
        par = pair % 2
        for tb in range(NT // NB):
            sl = slice(tb * NB, (tb + 1) * NB)
            for j in range(2):
                ex = pair * 2 + j
                wg_, wu_ = T["xg%d%d" % (par, j)], T["xu%d%d" % (par, j)]
                psg, pbg = C.next_ps()
                psu, pbu = C.next_ps()
                psw, pbw = C.next_ps()
                for f in range(2):
                    for kd in range(8):
                        mm(P, psg[:, f * NB:(f + 1) * NB], wg_[:, kd, f * 128:(f + 1) * 128], x1b[:, kd, sl], kd == 0, kd == 7,
                           [B["xg%d%d" % (par, j)], B["x_1b"]], [pbg])
                for f in range(2):
                    for kd in range(8):
                        mm(P, psu[:, f * NB:(f + 1) * NB], wu_[:, kd, f * 128:(f + 1) * 128], x1b[:, kd, sl], kd == 0, kd == 7,
                           [B["xu%d%d" % (par, j)], B["x_1b"]], [pbu])
                mm(P, psw[:, 0:NB], selE[:, ex, :], wT[:, sl], True, True, [B["selE"], B["r_wT"]], [pbw])
                v_act(P, T["x_sg"][:].rearrange("p f n -> p (f n)"), psg[:], AF.Silu, [pbg], [B["x_sg"]])
                v_tt(P, "dve", T["x_h"][:].rearrange("p f n -> p (f n)"), T["x_sg"][:].rearrange("p f n -> p (f n)"), psu[:], ALU.mult,
                     [B["x_sg"], pbu], [B["x_h"]])
                v_tt(P, "dve", T["x_h2%d" % j][:], T["x_h"][:], psw[:, 0:NB].unsqueeze(1).broadcast_to([128, 2, NB]), ALU.mult,
                     [B["x_h"], pbw], [B["x_h2%d" % j]])
            for m in range(8):
                psa, pba = C.ps[4 + m // 2], C.psb[4 + m // 2]
                o = psa[:, (m % 2) * NB:(m % 2 + 1) * NB]
                for j in range(2):
                    for f in range(2):
                        mm(P, o, T["xd%d%d" % (par, j)][:, f, m * 128:(m + 1) * 128], T["x_h2%d" % j][:, f, :], j == 0 and f == 0, j == 1 and f == 1,
                           [B["xd%d%d" % (par, j)], B["x_h2%d" % j]], [pba])
            for bk in range(4):
                psa, pba = C.ps[4 + bk], C.psb[4 + bk]
                pv = psa[:].rearrange("p (f n) -> p f n", f=2)
                av = acc[:, 2 * bk:2 * bk + 2, sl]
                if pair == 0:
                    P.op("dve", lambda e, av=av, pv=pv: e.scalar_tensor_tensor(out=av, in0=av, scalar=ALPHA, in1=pv, op0=ALU.mult, op1=ALU.add),
                         reads=[B["x_acc"], pba], writes=[B["x_acc"]])
                else:
                    v_tt(P, "dve", av, av, pv, ALU.add, [B["x_acc"], pba], [B["x_acc"]])
        if pair + 2 < 16:
            load_pair(pair + 2)
    C.ps_lim = 8
    ov = x_out.rearrange("(m p) t -> p m t", p=128)
    for tb in range(NTB):
        sl = slice(tb * TB, (tb + 1) * TB)

        def sink(m, ap, buf, sl=sl):
            P.dma("sp", ov[:, m, sl], ap, reads=[buf], writes=[xout_buf])
        emit_ln_block(P, C, acc[:, :, sl], B["x_acc"], 512, (g2, "ln2g"), (b2, "ln2b"), "l2_", sink)


def host_layer_weights(inp, l):
    f = np.float32
    c = np.ascontiguousarray
    W = {}
    W["w_in"] = c(inp["w_in"][l])
    W["w_gate"] = c(inp["w_gate"][l])
    W["bgT"] = c(inp["b_gate"][l].reshape(24, 128).T)
    W["w_glu"] = c(inp["w_glu"][l])
    W["bglu"] = c(inp["b_glu"][l].reshape(4, 128).T)
    W["wsT"] = c(np.transpose(inp["w_spatial"][l], (2, 0, 1)).reshape(128, 1024))
    W["lng"] = c(np.broadcast_to(inp["gmlp_ln_g"][l][None, :], (128, 512)))
    W["lnb"] = c(np.broadcast_to(inp["gmlp_ln_b"][l][None, :], (128, 512)))
    W["bsT"] = c(inp["b_spatial"][l].T)
    W["w_kv"] = c(inp["w_kv"][l])
    W["w_br"] = c(inp["w_br"][l])
    W["w_out"] = c(inp["w_out"][l])
    W["ln1gT"] = c(inp["ln1_g"][l].reshape(8, 128).T)
    W["ln1bT"] = c(inp["ln1_b"][l].reshape(8, 128).T)
    W["wr"] = c(np.concatenate([inp["w_router_g"][l], np.transpose(inp["w_router_e"][l], (1, 0, 2)).reshape(D, 32)], axis=1))
    brow = np.concatenate([inp["b_router_g"][l], inp["b_router_e"][l].reshape(32)])
    W["br"] = c(np.broadcast_to(brow[None, :], (128, 36)))
    W["w_exp_gate"] = c(inp["w_exp_gate"][l])
    W["w_exp_up"] = c(inp["w_exp_up"][l])
    W["w_exp_down"] = c(inp["w_exp_down"][l])
    W["ln2gT"] = c(inp["ln2_g"][l].reshape(8, 128).T)
    W["ln2bT"] = c(inp["ln2_b"][l].reshape(8, 128).T)
    for k, v in host_s5_params(inp, l).items():
        W["s5_" + k] = v
    return {k: np.asarray(v, dtype=f) for k, v in W.items()}


def host_const_all():
    cst = host_consts()
    selE = np.zeros((128, 32, 128), np.float32)
    for e_ in range(32):
        selE[e_, e_, :] = 1.0
    cst["c_selE"] = np.ascontiguousarray(selE.reshape(128, 32 * 128))
    return cst


W_SHAPES = {
    "w_in": [1024, 2048], "w_gate": [1024, 3072], "bgT": [128, 24], "w_glu": [512, 512], "bglu": [128, 4], "wsT": [128, 1024],
    "lng": [128, 512], "lnb": [128, 512], "bsT": [128, 8], "w_kv": [1024, 1024], "w_br": [3, 512, 1024], "w_out": [1024, 1024],
    "ln1gT": [128, 8], "ln1bT": [128, 8], "wr": [1024, 36], "br": [128, 36], "w_exp_gate": [32, 1024, 256], "w_exp_up": [32, 1024, 256],
    "w_exp_down": [32, 256, 1024], "ln2gT": [128, 8], "ln2bT": [128, 8],
    "s5_lam_re": [128, 32], "s5_lam_im": [128, 32], "s5_lstep": [128, 32], "s5_dm": [128, 32],
    "s5_b_re": [128, 512], "s5_b_im": [128, 512], "s5_c_re": [128, 512], "s5_c_im": [128, 512],
}
A_KEYS = ["w_in", "s5_lam_re", "s5_lam_im", "s5_lstep", "s5_dm", "s5_b_re", "s5_b_im", "s5_c_re", "s5_c_im"]
CONST_SHAPES = {"c_ident": [128, 128], "c_wide": [128, 1920], "c_maskF": [128, 128], "c_maskB": [128, 128], "c_kvec": [128, NK],
                "c_selE": [128, 4096]}


def emit_s5_stage(P, C, W, xT, pred, lout, ybr0, ybr0_buf, stop_after_scan):
    T, B = C.t, C.b
    C.push()
    alloc_s5_persist(C)
    C.push()
    alloc_s5_prep(C)
    emit_s5_prep(P, C, {k[3:]: W[k] for k in W if k.startswith("s5_")})
    C.pop()
    alloc_s5_main(C)
    Hs = T["a_Hs"]
    if pred is None:
        P.op("dve", lambda e: e.memset(Hs[:, 0], 0.0), writes=[B["a_Hs"]])
    else:
        pr = C.sb("a_pr", [128, 3, 2, 32], F32)
        P.dma("sp", pr[:], pred.rearrange("m p (r g) -> p m r g", r=2), writes=[B["a_pr"]])
        st = T["a_st"]
        v_cp(P, "dve", Hs[:, 0], pr[:, 0], [B["a_pr"]], [B["a_Hs"]])
        Pm = T["q_P"]
        for m in range(2):
            cmul(P, "dve", (st[:, 0, 0], B["a_st"]), (st[:, 0, 1], B["a_st"]), (Pm[:, m, 0], B["q_P"]), (Pm[:, m, 1], B["q_P"]),
                 (pr[:, m + 1, 0], B["a_pr"]), (pr[:, m + 1, 1], B["a_pr"]), (st[:, 1, 0], B["a_st"]))
            v_tt(P, "dve", Hs[:, 0], Hs[:, 0], st[:, 0], ALU.add, [B["a_Hs"], B["a_st"]], [B["a_Hs"]])
    C.push()
    xb = load_w(P, C, "xb", [128, 8, NT], kview(xT))
    win = load_w(P, C, "win", [128, 8, 512], kview(W["w_in"])[:, :, 0:512])
    emit_s5_front(P, C, xb, B["xb"], win, B["win"])
    C.pop()
    emit_s5_mid(P, C)
    if stop_after_scan:
        P.dma("sp", lout, Hs[:, NCH].rearrange("p r g -> p (r g)"), reads=[B["a_Hs"]], writes=[Buf("lout")])
        C.pop()
        return
    alloc_gelu(C)
    wglu = load_w(P, C, "wglu", [128, 4, 512], kview(W["w_glu"]))
    bglu = load_w(P, C, "bglu", [128, 4], W["bglu"], F32)
    emit_s5_back(P, C, wglu, B["wglu"], bglu, B["bglu"])
    P.dma("sp", ybr0.rearrange("(m p) t -> p m t", p=128), T["a_U"][:], reads=[B["a_U"]], writes=[ybr0_buf])
    C.pop()


def build_program(mode, stages=("s5", "gmlp", "attn", "merge", "moe")):
    nc = bass.Bass("TRN2", target_bir_lowering=False)

    def din(n, shape):
        return nc.dram_tensor(n, list(shape), F32, kind="ExternalInput").ap()
    xT = din("xT", [D, NT])
    cst = {k: din(k, v) for k, v in CONST_SHAPES.items()}
    keys = A_KEYS if mode == "A" else list(W_SHAPES.keys())
    W = {k: din(k, W_SHAPES[k]) for k in keys}
    W["c_selE"] = cst["c_selE"]
    P = Prog(nc)
    C = Ctx(P)
    C.alloc_psum()
    load_consts(P, C, cst)
    T, B = C.t, C.b
    if mode == "A":
        lout = nc.dram_tensor("lout", [128, 64], F32, kind="ExternalOutput").ap()
        emit_s5_stage(P, C, W, xT, None, lout, None, None, True)
        P.finish()
        return nc
    memT = din("memT", [D, 256])
    pred = din("pred", [3, 128, 64])
    xout = nc.dram_tensor("xout", [D, NT], F32, kind="ExternalOutput").ap()
    ybr = [nc.dram_tensor("ybr%d" % i, [512, NT], BF16).ap() for i in range(3)]
    ybr_b = [Buf("ybr%d" % i) for i in range(3)]
    x1T = nc.dram_tensor("x1T", [D, NT], F32).ap()
    x1_b = Buf("x1T")
    xo_b = Buf("xout")
    if "s5" in stages:
        emit_s5_stage(P, C, W, xT, pred, None, ybr[0], ybr_b[0], False)
    if "gmlp" in stages:
        C.push()
        xb = load_w(P, C, "xb", [128, 8, NT], kview(xT))
        alloc_gelu(C)
        emit_gmlp(P, C, xb, W, ybr[1], ybr_b[1])
        C.pop()
    if "attn" in stages:
        C.push()
        xb = load_w(P, C, "xb", [128, 8, NT], kview(xT))
        emit_memattn(P, C, xb, W, memT, ybr[2], ybr_b[2])
        C.pop()
    if "merge" in stages:
        C.push()
        xb = load_w(P, C, "xb", [128, 8, NT], kview(xT))
        emit_merge(P, C, xb, W, xT, ybr, ybr_b, x1T, x1_b)
        C.pop()
    if "moe" in stages:
        C.push()
        emit_moe(P, C, W, x1T, x1_b, xout, xo_b)
        C.pop()
    print("n_ops", P.n_ops)
    P.finish()
    return nc


_PROGS = {}


def _prog(mode):
    if mode not in _PROGS:
        _PROGS[mode] = build_program(mode)
    return _PROGS[mode]


def kernel(**inputs):
    inp = {k: np.asarray(v) for k, v in inputs.items()}
    x = inp["x"].astype(np.float32)
    mem = inp["mem"].astype(np.float32)
    n = 8
    cst = host_const_all()
    xT = []
    memT = []
    for c_ in range(n):
        b, j = c_ // 4, c_ % 4
        xT.append(np.ascontiguousarray(x[b, j * NT:(j + 1) * NT].T))
        memT.append(np.ascontiguousarray(mem[b].T))
    for l in range(DEPTH):
        Wl = host_layer_weights(inp, l)
        ncA = _prog("A")
        mapsA = []
        for c_ in range(n):
            m_ = {"xT": xT[c_]}
            m_.update(cst)
            m_.update({k: Wl[k] for k in A_KEYS})
            mapsA.append(m_)
        resA = run_bass_kernel_spmd(ncA, mapsA, core_ids=list(range(n)))
        lo = [np.asarray(r["lout"], dtype=np.float32) for r in resA.results]
        ncB = _prog("B")
        mapsB = []
        for c_ in range(n):
            b, j = c_ // 4, c_ % 4
            pred = np.zeros((3, 128, 64), np.float32)
            for m in range(3):
                if j - 1 - m >= 0:
                    pred[m, 0:64] = lo[b * 4 + j - 1 - m][0:64]
                if j + 1 + m <= 3:
                    pred[m, 64:128] = lo[b * 4 + j + 1 + m][64:128]
            m_ = {"xT": xT[c_], "memT": memT[c_], "pred": pred}
            m_.update(cst)
            m_.update(Wl)
            mapsB.append(m_)
        resB = run_bass_kernel_spmd(ncB, mapsB, core_ids=list(range(n)))
        xT = [np.asarray(r["xout"], dtype=np.float32) for r in resB.results]
    out = np.zeros((2, 8192, D), np.float32)
    for c_ in range(n):
        b, j = c_ // 4, c_ % 4
        out[b, j * NT:(j + 1) * NT] = xT[c_].T
    return out


def prog_coll(P, kind, ins, outs, reads=(), writes=()):
    eng = "pool"
    waits = P._waits(eng, reads, writes)
    key = P.dma_keys[eng][P.dma_rr[eng]]
    P.dma_rr[eng] = (P.dma_rr[eng] + 1) % len(P.dma_keys[eng])
    prev = P.cnt[key]
    if prev > 0 and P.seen[eng].get(key, 0) < prev:
        P.seen[eng][key] = prev
        waits.append((key, prev))
    P.cnt[key] += 16
    tick = (key, P.cnt[key])
    kk = dict(op=ALU.bypass, replica_groups=[list(range(8))], ins=ins, outs=outs)
    P.q[eng].append((waits, ("collective_compute", (kind,), kk), key, 16))
    P._commit(tick, reads, writes)
    P.n_ops += 1
```

```python
import numpy as np
from contextlib import ExitStack
import concourse.bass as bass
import concourse.mybir as mybir
from concourse.bass_utils import run_bass_kernel_spmd

F32 = mybir.dt.float32
BF16 = mybir.dt.bfloat16
I32 = mybir.dt.int32
AF = mybir.ActivationFunctionType
ALU = mybir.AluOpType
AX = mybir.AxisListType


class Buf:
    __slots__ = ("name", "w", "r", "excl")

    def __init__(self, name="", excl=False):
        self.name = name
        self.excl = excl
        self.w = None
        self.r = {}


import os as _os0
SAME_ENGINE_SYNC = _os0.environ.get('SAME_ENGINE_SYNC', '1') == '1'


class _Rec:
    def __init__(self):
        self.call = None

    def __getattr__(self, name):
        def f(*a, **k):
            self.call = (name, a, k)
            return self
        return f


class Prog:
    ENGS = ("pe", "act", "dve", "pool", "sp")

    def __init__(self, nc, n_dma_sems=16, same_engine_sync=SAME_ENGINE_SYNC):
        self.nc = nc
        self.es = ExitStack()
        self.q = {e: [] for e in self.ENGS}
        self.cnt = {}
        self.seen = {e: {} for e in self.ENGS}
        self.sem = {}
        for e in self.ENGS:
            self.sem["c_" + e] = self.es.enter_context(nc.semaphore("c_" + e))
            self.cnt["c_" + e] = 0
        self.dma_keys = {}
        self.dma_rr = {}
        for qe in ("sp", "pool", "act"):
            self.dma_keys[qe] = []
            self.dma_rr[qe] = 0
            for i in range(n_dma_sems if qe != "act" else 4):
                k = "d_%s_%d" % (qe, i)
                self.sem[k] = self.es.enter_context(nc.semaphore(k))
                self.cnt[k] = 0
                self.dma_keys[qe].append(k)
        self.same_engine_sync = same_engine_sync
        self.n_ops = 0
        self.pending = {e: {} for e in self.ENGS}
        self.open_scopes = []

    def sbuf(self, name, shape, dtype):
        return self.es.enter_context(self.nc.sbuf_tensor(name, list(shape), dtype))

    def psum(self, name, shape, dtype=F32):
        return self.es.enter_context(self.nc.psum_tensor(name, list(shape), dtype))

    def _waits(self, eng, reads, writes):
        deps = {}
        for b in reads:
            if b.w is not None:
                k, v = b.w
                deps[k] = max(deps.get(k, 0), v)
        for b in writes:
            if b.w is not None:
                k, v = b.w
                deps[k] = max(deps.get(k, 0), v)
            for k, v in b.r.items():
                deps[k] = max(deps.get(k, 0), v)
        for k, v in self.pending[eng].items():
            deps[k] = max(deps.get(k, 0), v)
        self.pending[eng] = {}
        waits = []
        own = "c_" + eng
        for k, v in deps.items():
            if k == own and (eng == "pe" or not self.same_engine_sync):
                continue
            if self.seen[eng].get(k, 0) >= v:
                continue
            self.seen[eng][k] = v
            waits.append((k, v))
        return waits

    def _commit(self, tick, reads, writes):
        k, v = tick
        for b in reads:
            b.r[k] = max(b.r.get(k, 0), v)
        for b in writes:
            b.w = tick
            b.r = {}

    def op(self, eng, fn, reads=(), writes=()):
        ex = [b for b in reads if b.excl]
        if ex:
            writes = list(writes) + ex
        waits = self._waits(eng, reads, writes)
        key = "c_" + eng
        self.cnt[key] += 1
        tick = (key, self.cnt[key])
        rec = _Rec()
        fn(rec)
        self.q[eng].append((waits, rec.call, key, 1))
        self._commit(tick, reads, writes)
        self.n_ops += 1

    def dma(self, eng, out, in_, reads=(), writes=(), **kw):
        waits = self._waits(eng, reads, writes)
        key = self.dma_keys[eng][self.dma_rr[eng]]
        self.dma_rr[eng] = (self.dma_rr[eng] + 1) % len(self.dma_keys[eng])
        prev = self.cnt[key]
        if prev > 0 and self.seen[eng].get(key, 0) < prev:
            self.seen[eng][key] = prev
            waits.append((key, prev))
        self.cnt[key] += 16
        tick = (key, self.cnt[key])
        kk = dict(kw)
        kk["out"] = out
        kk["in_"] = in_
        self.q[eng].append((waits, ("dma_start", (), kk), key, 16))
        self._commit(tick, reads, writes)
        self.n_ops += 1

    def barrier(self):
        for e in self.ENGS:
            for k, v in self.cnt.items():
                if v > 0:
                    self.pending[e][k] = max(self.pending[e].get(k, 0), v)

    def finish(self, final_bufs=()):
        fin = []
        for k, v in self.cnt.items():
            if v > 0 and self.seen["sp"].get(k, 0) < v and k != "c_sp":
                fin.append((k, v))
        nc = self.nc
        sem = self.sem
        q = self.q
        with nc.Block() as block:
            def emit(e, lst, extra=()):
                for waits, call, key, inc in lst:
                    for k, v in waits:
                        e.wait_ge(sem[k], v)
                    name, a, kw = call
                    getattr(e, name)(*a, **kw).then_inc(sem[key], inc)
                for k, v in extra:
                    e.wait_ge(sem[k], v)

            @block.sync
            def _(e):
                emit(e, q["sp"], fin)

            @block.scalar
            def _(e):
                emit(e, q["act"])

            @block.vector
            def _(e):
                emit(e, q["dve"])

            @block.gpsimd
            def _(e):
                emit(e, q["pool"])

            @block.tensor
            def _(e):
                emit(e, q["pe"])
        while self.open_scopes:
            self.open_scopes.pop().close()
        self.es.close()


D = 1024
NT = 2048
TB = 512
NTB = NT // TB
NCH = NT // 8
SCAN_K = 8
SCAN_L = NCH // SCAN_K
DEPTH = 4
ALPHA = (2.0 * DEPTH) ** 0.25
LN_EPS = 1e-5
KVEC = [0, -1, -2, -3, -4, -5, -6, -7, 0, 1, 2, 3, 4, 5, 6, 7, 1, 7, 8]
NK = len(KVEC)
MAGIC = 12582912.0
TWO_PI_HI = 6.28125
TWO_PI_LO = 2.0 * np.pi - 6.28125
GELU_C = 0.044715
GELU_S = 2.0 * 0.7978845608028654


def host_consts():
    ident = np.eye(128, dtype=np.float32)
    wide = np.zeros((128, 8, 240), np.float32)
    for a in range(8):
        for hc in range(16):
            wide[16 * a + hc, a, 112 + hc] = 1.0
    s_idx = np.arange(128) // 16
    maskF = (s_idx[None, :] >= s_idx[:, None]).astype(np.float32)
    maskB = (s_idx[:, None] >= s_idx[None, :]).astype(np.float32)
    maskF = maskF - maskB
    kvec = np.tile(np.array(KVEC, np.float32)[None, :], (128, 1))
    return dict(c_ident=ident, c_wide=np.ascontiguousarray(wide.reshape(128, 8 * 240)),
                c_maskF=maskF, c_maskB=maskB, c_kvec=kvec)


def host_s5_params(inp, l):
    f = np.float32

    def dp(a):
        return np.ascontiguousarray(np.transpose(a, (0, 2, 1)).reshape(128, 32)).astype(f)
    lam_re = dp(inp["ssm_lam_re"][l])
    lam_im = dp(inp["ssm_lam_im"][l])
    lstep = np.ascontiguousarray(np.broadcast_to(inp["ssm_log_step"][l][:, None, :], (2, 64, 32)).reshape(128, 32)).astype(f)

    def rep_b(a):
        t = np.transpose(a, (1, 0, 2))
        return np.ascontiguousarray(np.concatenate([t, t], 0).reshape(128, 32 * 16)).astype(f)

    def rep_c(a):
        t = np.transpose(a, (2, 0, 1))
        return np.ascontiguousarray(np.concatenate([t, t], 0).reshape(128, 32 * 16)).astype(f)
    dm = inp["ssm_d"][l].reshape(32, 16)
    dm = np.ascontiguousarray(np.tile(dm.T, (8, 1))).astype(f)
    return dict(lam_re=lam_re, lam_im=lam_im, lstep=lstep,
                b_re=rep_b(inp["ssm_b_re"][l]), b_im=rep_b(inp["ssm_b_im"][l]),
                c_re=rep_c(inp["ssm_c_re"][l]), c_im=rep_c(inp["ssm_c_im"][l]), dm=dm)


class Ctx:
    def __init__(self, P):
        self.P = P
        self.t = {}
        self.b = {}
        self.ps = []
        self.psb = []
        self.ps_rr = 0
        self.scopes = P.open_scopes
        self.uid = 0
        self.ps_lim = 8

    def sb(self, name, shape, dtype, nbuf=None):
        es = self.scopes[-1] if self.scopes else self.P.es
        self.uid += 1
        self.t[name] = es.enter_context(self.P.nc.sbuf_tensor("%s_%d" % (name, self.uid), list(shape), dtype))
        self.b[name] = Buf(name)
        return self.t[name]

    def push(self):
        self.scopes.append(ExitStack())

    def pop(self):
        self.P.barrier()
        self.scopes.pop().close()

    def alloc_psum(self):
        for i in range(8):
            self.ps.append(self.P.psum("psb%d" % i, [128, 512], F32))
            self.psb.append(Buf("psb%d" % i, excl=True))

    def next_ps(self):
        i = self.ps_rr
        self.ps_rr = (self.ps_rr + 1) % self.ps_lim
        return self.ps[i], self.psb[i]


def v_tt(P, eng, out, a, b, op, r, w):
    P.op(eng, lambda e: e.tensor_tensor(out=out, in0=a, in1=b, op=op), reads=r, writes=w)


def v_ts(P, eng, out, a, s1, s2, op0, op1, r, w):
    if s2 is None:
        P.op(eng, lambda e: e.tensor_scalar(out=out, in0=a, scalar1=s1, scalar2=None, op0=op0), reads=r, writes=w)
    else:
        P.op(eng, lambda e: e.tensor_scalar(out=out, in0=a, scalar1=s1, scalar2=s2, op0=op0, op1=op1), reads=r, writes=w)


def v_cp(P, eng, out, a, r, w):
    if eng == "act":
        P.op(eng, lambda e: e.copy(out=out, in_=a), reads=r, writes=w)
    else:
        P.op(eng, lambda e: e.tensor_copy(out=out, in_=a), reads=r, writes=w)


def v_act(P, out, a, func, r, w, bias=None, scale=None):
    kw = {}
    if bias is not None:
        kw["bias"] = bias
    if scale is not None:
        kw["scale"] = scale
    P.op("act", lambda e: e.activation(out=out, in_=a, func=func, **kw), reads=r, writes=w)


def mm(P, out, lhsT, rhs, start, stop, r, w):
    P.op("pe", lambda e: e.matmul(out, lhsT, rhs, start=start, stop=stop), reads=r, writes=w)


def cmul(P, eng, o_re, o_im, a_re, a_im, b_re, b_im, tmp):
    v_tt(P, eng, o_re[0], a_re[0], b_re[0], ALU.mult, [a_re[1], b_re[1]], [o_re[1]])
    v_tt(P, eng, tmp[0], a_im[0], b_im[0], ALU.mult, [a_im[1], b_im[1]], [tmp[1]])
    v_tt(P, eng, o_re[0], o_re[0], tmp[0], ALU.subtract, [o_re[1], tmp[1]], [o_re[1]])
    v_tt(P, eng, o_im[0], a_re[0], b_im[0], ALU.mult, [a_re[1], b_im[1]], [o_im[1]])
    v_tt(P, eng, tmp[0], a_im[0], b_re[0], ALU.mult, [a_im[1], b_re[1]], [tmp[1]])
    v_tt(P, eng, o_im[0], o_im[0], tmp[0], ALU.add, [o_im[1], tmp[1]], [o_im[1]])


def cmul_big(P, eng, o_re, o_im, a_re, a_im, b_re, b_im, tmp):
    for g0 in (0, 16):
        def sl(v):
            return (v[0][:, g0:g0 + 16], v[1])
        cmul(P, eng, sl(o_re), sl(o_im), sl(a_re), sl(a_im), sl(b_re), sl(b_im), (tmp[0], tmp[1]))


def alloc_s5_persist(C):
    C.sb("q_A2", [128, 2, 2, 32], F32)
    C.sb("q_P", [128, 3, 2, 32], F32)
    C.sb("w_Glre", [128, 32, 8, 16], BF16)
    C.sb("w_GlimN", [128, 32, 8, 16], BF16)
    C.sb("w_Msum", [128, 32, 128], BF16)
    C.sb("w_Wl", [128, 32, 2, 128], BF16)


def alloc_s5_prep(C):
    for n in ("p_lam_re", "p_lam_im", "p_lstep", "p_dm"):
        C.sb(n, [128, 32], F32)
    for n in ("p_b_re", "p_b_im", "p_c_re", "p_c_im"):
        C.sb(n, [128, 32, 16], F32)
    for n in ("q_step", "q_a", "q_b", "q_den", "q_em1", "q_t1", "q_t2", "q_bre", "q_bim"):
        C.sb(n, [128, 32], F32)
    for n in ("q_ka", "q_kb", "q_n", "q_r", "q_mag", "q_Ere", "q_Eim"):
        C.sb(n, [128, 32, NK], F32)
    for n in ("q_bBre", "q_bBim", "q_t3"):
        C.sb(n, [128, 32, 16], F32)
    for n in ("s_are", "s_aim", "s_bre", "s_bim", "s_cre", "s_cim"):
        C.sb(n, [128, 32, 8, 16], F32)
    C.sb("s_tmp", [128, 16, 8, 16], F32)
    C.sb("s_m1", [128, 128], F32)
    C.sb("s_m2", [128, 128], F32)


def emit_s5_prep(P, C, prm):
    T, B = C.t, C.b

    def V(n, ap=None):
        return (T[n][:] if ap is None else ap, B[n])
    for n in ("lam_re", "lam_im", "lstep", "dm"):
        P.dma("sp", T["p_" + n][:], prm[n], writes=[B["p_" + n]])
    for n in ("b_re", "b_im", "c_re", "c_im"):
        P.dma("sp", T["p_" + n][:].rearrange("p g h -> p (g h)"), prm[n], writes=[B["p_" + n]])
    e = "dve"
    v_act(P, T["q_step"][:], T["p_lstep"][:], AF.Exp, [B["p_lstep"]], [B["q_step"]])
    v_tt(P, e, T["q_a"][:], T["p_lam_re"][:], T["q_step"][:], ALU.mult, [B["p_lam_re"], B["q_step"]], [B["q_a"]])
    v_tt(P, e, T["q_b"][:], T["p_lam_im"][:], T["q_step"][:], ALU.mult, [B["p_lam_im"], B["q_step"]], [B["q_b"]])
    kv = T["c_kvec"][:].unsqueeze(1).broadcast_to([128, 32, NK])
    sh3 = [128, 32, NK]
    v_tt(P, e, T["q_ka"][:], T["q_a"][:].unsqueeze(2).broadcast_to(sh3), kv, ALU.mult, [B["q_a"], B["c_kvec"]], [B["q_ka"]])
    v_tt(P, e, T["q_kb"][:], T["q_b"][:].unsqueeze(2).broadcast_to(sh3), kv, ALU.mult, [B["q_b"], B["c_kvec"]], [B["q_kb"]])
    v_act(P, T["q_mag"][:], T["q_ka"][:], AF.Exp, [B["q_ka"]], [B["q_mag"]])
    inv2pi = float(1.0 / (2.0 * np.pi))
    for which, off, dst in (("sin", 0.0, "q_Eim"), ("cos", 0.25, "q_Ere")):
        v_ts(P, e, T["q_n"][:], T["q_kb"][:], inv2pi, off, ALU.mult, ALU.add, [B["q_kb"]], [B["q_n"]])
        v_ts(P, e, T["q_n"][:], T["q_n"][:], MAGIC, None, ALU.add, None, [B["q_n"]], [B["q_n"]])
        v_ts(P, e, T["q_n"][:], T["q_n"][:], -MAGIC, None, ALU.add, None, [B["q_n"]], [B["q_n"]])
        P.op(e, lambda en: en.scalar_tensor_tensor(out=T["q_r"][:], in0=T["q_n"][:], scalar=-TWO_PI_HI, in1=T["q_kb"][:],
                                                   op0=ALU.mult, op1=ALU.add), reads=[B["q_n"], B["q_kb"]], writes=[B["q_r"]])
        P.op(e, lambda en: en.scalar_tensor_tensor(out=T["q_r"][:], in0=T["q_n"][:], scalar=-TWO_PI_LO, in1=T["q_r"][:],
                                                   op0=ALU.mult, op1=ALU.add), reads=[B["q_n"], B["q_r"]], writes=[B["q_r"]])
        if off != 0.0:
            v_ts(P, e, T["q_r"][:], T["q_r"][:], float(np.pi / 2), None, ALU.add, None, [B["q_r"]], [B["q_r"]])
        v_ts(P, e, T["q_r"][:], T["q_r"][:], float(np.pi), float(-np.pi), ALU.min, ALU.max, [B["q_r"]], [B["q_r"]])
        v_act(P, T[dst][:], T["q_r"][:], AF.Sin, [B["q_r"]], [B[dst]])
    v_tt(P, e, T["q_Ere"][:], T["q_mag"][:], T["q_Ere"][:], ALU.mult, [B["q_mag"], B["q_Ere"]], [B["q_Ere"]])
    v_tt(P, e, T["q_Eim"][:], T["q_mag"][:], T["q_Eim"][:], ALU.mult, [B["q_mag"], B["q_Eim"]], [B["q_Eim"]])
    Ere, Eim = T["q_Ere"], T["q_Eim"]
    v_tt(P, e, T["q_den"][:], T["p_lam_re"][:], T["p_lam_re"][:], ALU.mult, [B["p_lam_re"]], [B["q_den"]])
    v_tt(P, e, T["q_t1"][:], T["p_lam_im"][:], T["p_lam_im"][:], ALU.mult, [B["p_lam_im"]], [B["q_t1"]])
    v_tt(P, e, T["q_den"][:], T["q_den"][:], T["q_t1"][:], ALU.add, [B["q_den"], B["q_t1"]], [B["q_den"]])
    P.op(e, lambda en: en.reciprocal(out=T["q_den"][:], in_=T["q_den"][:]), reads=[B["q_den"]], writes=[B["q_den"]])
    v_ts(P, e, T["q_em1"][:], Ere[:, :, 16], -1.0, None, ALU.add, None, [B["q_Ere"]], [B["q_em1"]])
    v_tt(P, e, T["q_bre"][:], T["q_em1"][:], T["p_lam_re"][:], ALU.mult, [B["q_em1"], B["p_lam_re"]], [B["q_bre"]])
    v_tt(P, e, T["q_t1"][:], Eim[:, :, 16], T["p_lam_im"][:], ALU.mult, [B["q_Eim"], B["p_lam_im"]], [B["q_t1"]])
    v_tt(P, e, T["q_bre"][:], T["q_bre"][:], T["q_t1"][:], ALU.add, [B["q_bre"], B["q_t1"]], [B["q_bre"]])
    v_tt(P, e, T["q_bim"][:], Eim[:, :, 16], T["p_lam_re"][:], ALU.mult, [B["q_Eim"], B["p_lam_re"]], [B["q_bim"]])
    v_tt(P, e, T["q_t2"][:], T["q_em1"][:], T["p_lam_im"][:], ALU.mult, [B["q_em1"], B["p_lam_im"]], [B["q_t2"]])
    v_tt(P, e, T["q_bim"][:], T["q_bim"][:], T["q_t2"][:], ALU.subtract, [B["q_bim"], B["q_t2"]], [B["q_bim"]])
    v_tt(P, e, T["q_bre"][:], T["q_bre"][:], T["q_den"][:], ALU.mult, [B["q_bre"], B["q_den"]], [B["q_bre"]])
    v_tt(P, e, T["q_bim"][:], T["q_bim"][:], T["q_den"][:], ALU.mult, [B["q_bim"], B["q_den"]], [B["q_bim"]])
    s16 = [128, 32, 16]
    bre_b = (T["q_bre"][:].unsqueeze(2).broadcast_to(s16), B["q_bre"])
    bim_b = (T["q_bim"][:].unsqueeze(2).broadcast_to(s16), B["q_bim"])
    cmul(P, e, V("q_bBre"), V("q_bBim"), bre_b, bim_b, V("p_b_re"), V("p_b_im"), V("q_t3"))
    A2 = T["q_A2"]
    v_cp(P, e, A2[:, 0, 0, :], Ere[:, :, 18], [B["q_Ere"]], [B["q_A2"]])
    v_cp(P, e, A2[:, 0, 1, :], Ere[:, :, 18], [B["q_Ere"]], [B["q_A2"]])
    v_ts(P, e, A2[:, 1, 0, :], Eim[:, :, 18], -1.0, None, ALU.mult, None, [B["q_Eim"]], [B["q_A2"]])
    v_cp(P, e, A2[:, 1, 1, :], Eim[:, :, 18], [B["q_Eim"]], [B["q_A2"]])
    Pm = T["q_P"]
    v_cp(P, e, Pm[:, 0, 0, :], Ere[:, :, 18], [B["q_Ere"]], [B["q_P"]])
    v_cp(P, e, Pm[:, 0, 1, :], Eim[:, :, 18], [B["q_Eim"]], [B["q_P"]])

    def csq(dst_re, dst_im, a_re, a_im, b_re, b_im):
        v_tt(P, e, T["q_t1"][:], a_re, b_re, ALU.mult, [B["q_P"]], [B["q_t1"]])
        v_tt(P, e, T["q_t2"][:], a_im, b_im, ALU.mult, [B["q_P"]], [B["q_t2"]])
        v_tt(P, e, T["q_t1"][:], T["q_t1"][:], T["q_t2"][:], ALU.subtract, [B["q_t1"], B["q_t2"]], [B["q_t1"]])
        v_tt(P, e, T["q_t2"][:], a_re, b_im, ALU.mult, [B["q_P"]], [B["q_t2"]])
        v_tt(P, e, T["q_den"][:], a_im, b_re, ALU.mult, [B["q_P"]], [B["q_den"]])
        v_tt(P, e, dst_im, T["q_t2"][:], T["q_den"][:], ALU.add, [B["q_t2"], B["q_den"]], [B["q_P"]])
        v_cp(P, e, dst_re, T["q_t1"][:], [B["q_t1"]], [B["q_P"]])
    for _ in range(8):
        csq(Pm[:, 0, 0, :], Pm[:, 0, 1, :], Pm[:, 0, 0, :], Pm[:, 0, 1, :], Pm[:, 0, 0, :], Pm[:, 0, 1, :])
    csq(Pm[:, 1, 0, :], Pm[:, 1, 1, :], Pm[:, 0, 0, :], Pm[:, 0, 1, :], Pm[:, 0, 0, :], Pm[:, 0, 1, :])
    csq(Pm[:, 2, 0, :], Pm[:, 2, 1, :], Pm[:, 1, 0, :], Pm[:, 1, 1, :], Pm[:, 0, 0, :], Pm[:, 0, 1, :])
    s4 = [128, 32, 8, 16]

    def Ek(lo, hi):
        return ((Ere[:, :, lo:hi].unsqueeze(3).broadcast_to(s4), B["q_Ere"]),
                (Eim[:, :, lo:hi].unsqueeze(3).broadcast_to(s4), B["q_Eim"]))

    def E1(idx):
        return ((Ere[:, :, idx:idx + 1].unsqueeze(3).broadcast_to(s4), B["q_Ere"]),
                (Eim[:, :, idx:idx + 1].unsqueeze(3).broadcast_to(s4), B["q_Eim"]))
    bB_re = (T["q_bBre"][:].unsqueeze(2).broadcast_to(s4), B["q_bBre"])
    bB_im = (T["q_bBim"][:].unsqueeze(2).broadcast_to(s4), B["q_bBim"])
    def ctab(o_re_n, o_im_n, a_re, a_im, b_re, b_im, rev_bwd):
        for lo, hi, rev in ((0, 64, False), (64, 128, rev_bwd)):
            o_re = (T[o_re_n][lo:hi, :, ::-1, :] if rev else T[o_re_n][lo:hi], B[o_re_n])
            o_im = (T[o_im_n][lo:hi, :, ::-1, :] if rev else T[o_im_n][lo:hi], B[o_im_n])
            cmul_big(P, e, o_re, o_im, (a_re[0][lo:hi], a_re[1]), (a_im[0][lo:hi], a_im[1]),
                     (b_re[0][lo:hi], b_re[1]), (b_im[0][lo:hi], b_im[1]), (T["s_tmp"][lo:hi], B["s_tmp"]))
    er, ei = Ek(0, 8)
    ctab("s_are", "s_aim", er, ei, bB_re, bB_im, True)
    c_re = (T["p_c_re"][:].unsqueeze(2).broadcast_to(s4), B["p_c_re"])
    c_im = (T["p_c_im"][:].unsqueeze(2).broadcast_to(s4), B["p_c_im"])
    er, ei = Ek(8, 16)
    ctab("s_bre", "s_bim", er, ei, c_re, c_im, True)
    e1r, e1i = E1(16)
    ctab("s_cre", "s_cim", e1r, e1i, V("s_bre"), V("s_bim"), False)
    v_cp(P, "act", T["w_Glre"][:], T["s_cre"][:], [B["s_cre"]], [B["w_Glre"]])
    v_ts(P, e, T["w_GlimN"][:], T["s_cim"][:], -1.0, None, ALU.mult, None, [B["s_cim"]], [B["w_GlimN"]])
    e7r, e7i = E1(17)
    ctab("s_cre", "s_cim", e7r, e7i, V("s_are"), V("s_aim"), False)
    v_ts(P, e, T["s_bim"][:], T["s_bim"][:], -1.0, None, ALU.mult, None, [B["s_bim"]], [B["s_bim"]])
    idf = T["c_ident"]

    def fl(ap):
        return ap.rearrange("p s h -> p (s h)")
    for g in range(32):
        psA, pbA = C.next_ps()
        mm(P, psA[:, 0:128], fl(T["s_are"][0:64, g]), fl(T["s_bre"][0:64, g]), True, False, [B["s_are"], B["s_bre"]], [pbA])
        mm(P, psA[:, 0:128], fl(T["s_aim"][0:64, g]), fl(T["s_bim"][0:64, g]), False, True, [B["s_aim"], B["s_bim"]], [pbA])
        mm(P, psA[:, 128:256], fl(T["s_are"][:, g]), fl(T["s_bre"][:, g]), True, False, [B["s_are"], B["s_bre"]], [pbA])
        mm(P, psA[:, 128:256], fl(T["s_aim"][:, g]), fl(T["s_bim"][:, g]), False, True, [B["s_aim"], B["s_bim"]], [pbA])
        for ri, tn in ((0, "s_cre"), (1, "s_cim")):
            o = 256 + ri * 128
            mm(P, psA[:, o:o + 128], fl(T[tn][:, g]), idf[:, :], True, True, [B[tn], B["c_ident"]], [pbA])
        v_tt(P, e, T["s_m1"][:], psA[:, 0:128], T["c_maskF"][:], ALU.mult, [pbA, B["c_maskF"]], [B["s_m1"]])
        v_tt(P, e, T["s_m2"][:], psA[:, 128:256], T["c_maskB"][:], ALU.mult, [pbA, B["c_maskB"]], [B["s_m2"]])
        v_tt(P, "pool", T["s_m1"][:], T["s_m1"][:], T["s_m2"][:], ALU.add, [B["s_m1"], B["s_m2"]], [B["s_m1"]])
        P.op("dve", lambda en, g=g: en.scalar_tensor_tensor(out=T["w_Msum"][:, g, :], in0=T["c_ident"][:], scalar=T["p_dm"][:, g:g + 1],
                                                             in1=T["s_m1"][:], op0=ALU.mult, op1=ALU.add),
             reads=[B["c_ident"], B["p_dm"], B["s_m1"]], writes=[B["w_Msum"]])
        v_cp(P, "act", T["w_Wl"][:, g].rearrange("p r m -> p (r m)"), psA[:, 256:512], [pbA], [B["w_Wl"]])


def alloc_s5_main(C):
    C.sb("a_U", [128, 4, NT], BF16)
    C.sb("a_X8", [128, 32, NCH], BF16)
    C.sb("a_Hs", [128, NCH + 1 + SCAN_L, 2, 32], F32)
    C.sb("a_I", [128, SCAN_K + 1, 2, 32], F32)
    C.sb("a_P32", [128, 2, 2, 32], F32)
    C.sb("a_st", [128, 2, 2, 32], F32)
    C.sb("a_st9", [128, 2, SCAN_K + 1, 2, 32], F32)
    C.sb("a_st1", [128, 2, 2, 32], F32)
    C.bX8 = [Buf("x8_%d" % g) for g in range(32)]


def emit_s5_scan(P, C):
    T, B = C.t, C.b
    Hs, A2, st, I, P32 = T["a_Hs"], T["q_A2"], T["a_st9"], T["a_I"], T["a_P32"]
    K, L = SCAN_K, SCAN_L
    V = Hs[:, 1:1 + (K + 1) * L].rearrange("p (b i) r g -> p b i r g", b=K + 1)
    hb = [B["a_Hs"]]
    s9 = [128, K + 1, 2, 32]
    Are = A2[:, 0].unsqueeze(1).broadcast_to(s9)
    Aim = A2[:, 1].unsqueeze(1).broadcast_to(s9)
    P.op("dve", lambda e: e.memset(Hs[:, 1 + K * L:1 + (K + 1) * L], 0.0), writes=hb)
    v_cp(P, "dve", V[:, K, 0, 0, :], A2[:, 0, 0, :], [B["q_A2"]], hb)
    v_cp(P, "dve", V[:, K, 0, 1, :], A2[:, 1, 1, :], [B["q_A2"]], hb)
    for i in range(1, L):
        v_tt(P, "dve", st[:, 0], V[:, :, i - 1], Are, ALU.mult, hb + [B["q_A2"]], [B["a_st9"]])
        v_tt(P, "dve", st[:, 1], V[:, :, i - 1, ::-1, :], Aim, ALU.mult, hb + [B["q_A2"]], [C.b_st2])
        v_tt(P, "dve", V[:, :, i], V[:, :, i], st[:, 0], ALU.add, hb + [B["a_st9"]], hb)
        v_tt(P, "dve", V[:, :, i], V[:, :, i], st[:, 1], ALU.add, hb + [C.b_st2], hb)
    v_cp(P, "dve", P32[:, 0, 0, :], V[:, K, L - 1, 0, :], hb, [B["a_P32"]])
    v_cp(P, "dve", P32[:, 0, 1, :], V[:, K, L - 1, 0, :], hb, [B["a_P32"]])
    v_ts(P, "dve", P32[:, 1, 0, :], V[:, K, L - 1, 1, :], -1.0, None, ALU.mult, None, hb, [B["a_P32"]])
    v_cp(P, "dve", P32[:, 1, 1, :], V[:, K, L - 1, 1, :], hb, [B["a_P32"]])
    ib = [B["a_I"]]
    s1 = T["a_st1"]
    v_cp(P, "dve", I[:, 0], Hs[:, 0], hb, ib)
    for b_ in range(K):
        v_tt(P, "dve", s1[:, 0], I[:, b_], P32[:, 0], ALU.mult, ib + [B["a_P32"]], [B["a_st1"]])
        v_tt(P, "dve", s1[:, 1], I[:, b_, ::-1, :], P32[:, 1], ALU.mult, ib + [B["a_P32"]], [B["a_st1"]])
        v_tt(P, "dve", I[:, b_ + 1], V[:, b_, L - 1], s1[:, 0], ALU.add, hb + [B["a_st1"]], ib)
        v_tt(P, "dve", I[:, b_ + 1], I[:, b_ + 1], s1[:, 1], ALU.add, ib + [B["a_st1"]], ib)
    sF = [128, K, L, 32]
    Tm = T["a_T"]
    Pw = V[:, K]

    def pw(r):
        return Pw[:, :, r, :].unsqueeze(1).broadcast_to(sF)

    def ii(r):
        return I[:, 0:K, r, :].unsqueeze(2).broadcast_to(sF)
    for k, (pr_, ir_, out_r, op) in enumerate(((0, 0, 0, ALU.add), (1, 1, 0, ALU.subtract), (0, 1, 1, ALU.add), (1, 0, 1, ALU.add))):
        tm, tb_ = Tm[:], C.b_T[0]
        v_tt(P, "pool", tm, pw(pr_), ii(ir_), ALU.mult, hb + ib, [tb_])
        v_tt(P, "dve", V[:, 0:K, :, out_r, :], V[:, 0:K, :, out_r, :], tm, op, hb + [tb_], [C.b_Hfix[out_r]])
    P.op("dve", lambda e: e.tensor_copy(out=I[:, 0, 0, 0:1], in_=I[:, 0, 0, 0:1]), reads=[C.b_Hfix[0], C.b_Hfix[1]] + ib, writes=hb + ib)


def emit_s5_front(P, C, xb, xb_buf, win, win_buf):
    T, B = C.t, C.b
    U, X8, Hs = T["a_U"], T["a_X8"], T["a_Hs"]
    C.b_st2 = Buf("st2")
    for tb in range(NTB):
        for m in range(4):
            ps, pb = C.next_ps()
            for kd in range(8):
                mm(P, ps[:], win[:, kd, m * 128:(m + 1) * 128], xb[:, kd, tb * TB:(tb + 1) * TB], kd == 0, kd == 7,
                   [win_buf, xb_buf], [pb])
            v_cp(P, "act" if (m % 2) else "dve", U[:, m, tb * TB:(tb + 1) * TB], ps[:], [pb], [B["a_U"]])


def emit_s5_mid(P, C):
    T, B = C.t, C.b
    U, X8, Hs = T["a_U"], T["a_X8"], T["a_Hs"]
    C.push()
    C.sb("a_T", [128, SCAN_K, SCAN_L, 32], F32)
    C.b_T = [Buf("aT0")]
    C.b_Hfix = [Buf("hfix0"), Buf("hfix1")]
    wide = T["c_wide_bf"]
    for g2 in range(16):
        ps, pb = C.next_ps()
        for gg in range(2):
            g = 2 * g2 + gg
            gl, m = g % 8, g // 8
            Uv = U[:, m, :].rearrange("p (c s) -> p s c", s=8)
            for s in range(8):
                mm(P, ps[:, gg * 256:(gg + 1) * 256], wide[:, gl, 112 - 16 * s:112 - 16 * s + 128], Uv[:, s, :], s == 0, s == 7,
                   [B["c_wide_bf"], B["a_U"]], [pb])
        v_cp(P, "act" if (g2 % 2) else "dve", X8[:, 2 * g2:2 * g2 + 2, :].rearrange("p g c -> p (g c)"), ps[:], [pb],
             [C.bX8[2 * g2], C.bX8[2 * g2 + 1]])
    Wl = T["w_Wl"]
    for g in range(32):
        ps, pb = C.next_ps()
        for ri in range(2):
            mm(P, ps[:, ri * 256:(ri + 1) * 256], Wl[:, g, ri, :], X8[:, g, :], True, True, [B["w_Wl"], C.bX8[g]], [pb])
        psv = ps[:].rearrange("p (r c) -> p c r", r=2)
        v_cp(P, "act", Hs[0:64, 1:NCH + 1, :, g], psv[0:64], [pb], [B["a_Hs"]])
        v_cp(P, "dve", Hs[64:128, 1:NCH + 1, :, g], psv[64:128, ::-1, :], [pb], [B["a_Hs"]])
    emit_s5_scan(P, C)
    C.pop()


import os as _os
S5_STOP = int(_os.environ.get("S5_STOP", "0"))


def emit_s5_back(P, C, wglu, wglu_buf, bglu, bglu_buf):
    T, B = C.t, C.b
    U, X8, Hs = T["a_U"], T["a_X8"], T["a_Hs"]
    HsB = C.sb("a_HsB", [128, NCH + 1, 2, 32], BF16)
    v_cp(P, "act", HsB[0:64, 0:NCH], Hs[0:64, 0:NCH], [B["a_Hs"]], [B["a_HsB"]])
    v_cp(P, "dve", HsB[64:128, 0:NCH], Hs[64:128, NCH - 1::-1], [B["a_Hs"]], [B["a_HsB"]])
    for g2 in range(16):
        ps, pb = C.next_ps()
        for gg in range(2):
            g = 2 * g2 + gg
            o = ps[:, gg * 256:(gg + 1) * 256]
            mm(P, o, T["w_Msum"][:, g, :], X8[:, g, :], True, False, [B["w_Msum"], C.bX8[g]], [pb])
            for ri, tn in ((0, "w_Glre"), (1, "w_GlimN")):
                mm(P, o, T[tn][:, g].rearrange("p j h -> p (j h)"), HsB[:, 0:NCH, ri, g], False, ri == 1, [B[tn], B["a_HsB"]], [pb])
        v_cp(P, "act" if (g2 % 2) else "dve", X8[:, 2 * g2:2 * g2 + 2, :].rearrange("p g c -> p (g c)"), ps[:], [pb],
             [C.bX8[2 * g2], C.bX8[2 * g2 + 1]])
    if S5_STOP == 3:
        return
    wide = T["c_wide_bf"]
    for m in range(4):
        Uv = U[:, m, :].rearrange("p (c s) -> p s c", s=8)
        for j2 in range(4):
            ps, pb = C.next_ps()
            for jj in range(2):
                j = 2 * j2 + jj
                for gl in range(8):
                    mm(P, ps[:, jj * 256:(jj + 1) * 256], wide[:, j, 112 - 16 * gl:112 - 16 * gl + 128], X8[:, m * 8 + gl, :], gl == 0, gl == 7,
                       [B["c_wide_bf"], C.bX8[m * 8 + gl]], [pb])
            emit_gelu(P, C, Uv[:, 2 * j2:2 * j2 + 2, :], ps[:].rearrange("p (j c) -> p j c", j=2), pb, [B["a_U"]], [128, 2, 256])
    if S5_STOP == 4:
        return
    for tb in range(NTB):
        sl = slice(tb * TB, (tb + 1) * TB)
        pss = []
        for m in range(4):
            ps, pb = C.next_ps()
            for k in range(4):
                mm(P, ps[:], wglu[:, k, m * 128:(m + 1) * 128], U[:, k, sl], k == 0, k == 3, [wglu_buf, B["a_U"]], [pb])
            pss.append((ps, pb))
        for m in range(4):
            ps, pb = pss[m]
            v_act(P, T["g_gate"][:, m, :], ps[:], AF.Sigmoid, [pb, bglu_buf], [B["g_gate"]], bias=bglu[:, m:m + 1])
        for m in range(4):
            v_tt(P, "dve", U[:, m, sl], U[:, m, sl], T["g_gate"][:, m, :], ALU.mult, [B["a_U"], B["g_gate"]], [B["a_U"]])


def emit_gelu(P, C, out, src, src_buf, out_bufs, shape):
    T, B = C.t, C.b
    n = 1
    for d_ in shape[1:]:
        n *= d_
    x = T["g_x"][:, 0:n]
    t = T["g_t"][:, 0:n]
    if len(shape) == 3:
        x = x.rearrange("p (a b) -> p a b", a=shape[1])
        t = t.rearrange("p (a b) -> p a b", a=shape[1])
    v_cp(P, "act", x, src, [src_buf], [B["g_x"]])
    v_tt(P, "pool", t, x, x, ALU.mult, [B["g_x"]], [B["g_t"]])
    v_ts(P, "pool", t, t, GELU_C * GELU_S, GELU_S, ALU.mult, ALU.add, [B["g_t"]], [B["g_t"]])
    v_tt(P, "pool", t, t, x, ALU.mult, [B["g_t"], B["g_x"]], [B["g_t"]])
    v_act(P, t, t, AF.Sigmoid, [B["g_t"]], [B["g_t"]])
    v_tt(P, "dve", out, x, t, ALU.mult, [B["g_x"], B["g_t"]], out_bufs)


def alloc_gelu(C):
    C.sb("g_x", [128, 512], F32)
    C.sb("g_t", [128, 512], F32)
    C.sb("g_gate", [128, 4, 512], BF16)


def load_consts(P, C, cst):
    T, B = C.t, C.b
    C.sb("c_ident", [128, 128], F32)
    C.sb("c_ident_bf", [128, 128], BF16)
    C.sb("c_wide_bf", [128, 8, 240], BF16)
    C.sb("c_maskF", [128, 128], F32)
    C.sb("c_maskB", [128, 128], F32)
    C.sb("c_kvec", [128, NK], F32)
    P.dma("sp", T["c_ident"][:], cst["c_ident"], writes=[B["c_ident"]])
    P.dma("pool", T["c_ident_bf"][:], cst["c_ident"], writes=[B["c_ident_bf"]])
    P.dma("pool", T["c_wide_bf"][:].rearrange("p a w -> p (a w)"), cst["c_wide"], writes=[B["c_wide_bf"]])
    P.dma("sp", T["c_maskF"][:], cst["c_maskF"], writes=[B["c_maskF"]])
    P.dma("sp", T["c_maskB"][:], cst["c_maskB"], writes=[B["c_maskB"]])
    P.dma("sp", T["c_kvec"][:], cst["c_kvec"], writes=[B["c_kvec"]])


def load_w(P, C, name, shape, src_ap, dtype=BF16, eng=None):
    t = C.sb(name, shape, dtype)
    if eng is None:
        eng = "pool" if dtype != F32 else "sp"
    dst = t[:]
    P.dma(eng, dst, src_ap, writes=[C.b[name]])
    return t


def kview(ap2d, ncols=None):
    return ap2d.rearrange("(k p) c -> p k c", p=128)


def emit_rstd(P, C, var_ap, var_buf):
    v_ts(P, "dve", var_ap, var_ap, LN_EPS, None, ALU.add, None, [var_buf], [var_buf])
    v_act(P, var_ap, var_ap, AF.Sqrt, [var_buf], [var_buf])
    P.op("dve", lambda e: e.reciprocal(out=var_ap, in_=var_ap), reads=[var_buf], writes=[var_buf])


def emit_gmlp(P, C, xb, W, ygm_out, ygm_buf):
    T, B = C.t, C.b
    wuv = load_w(P, C, "wuv", [128, 8, 1024], kview(W["w_in"])[:, :, 512:1536])
    wsT = load_w(P, C, "wsT", [128, 8, 128], W["wsT"].rearrange("p (g i) -> p g i", g=8))
    lng = load_w(P, C, "lng", [128, 512], W["lng"], F32)
    lnb = load_w(P, C, "lnb", [128, 512], W["lnb"], F32)
    bsT = load_w(P, C, "bsT", [128, 8], W["bsT"], F32)
    C.sb("m_u", [128, 512], BF16)
    C.sb("m_v", [128, 512], F32)
    C.sb("m_vb", [128, 512], BF16)
    C.sb("m_sq", [128, 512], F32)
    C.sb("m_st", [128, 4], F32)
    C.sb("m_t", [128, 512], F32)
    C.sb("m_y", [128, 512], BF16)
    C.sb("m_yf", [128, 4, 128], BF16)
    idb = T["c_ident_bf"]
    for tt in range(NT // 128):
        tok = slice(tt * 128, (tt + 1) * 128)
        psu, pbu = C.next_ps()
        psv, pbv = C.next_ps()
        for kd in range(8):
            mm(P, psu[:], xb[:, kd, tok], wuv[:, kd, 0:512], kd == 0, kd == 7, [B["xb"], B["wuv"]], [pbu])
        for kd in range(8):
            mm(P, psv[:], xb[:, kd, tok], wuv[:, kd, 512:1024], kd == 0, kd == 7, [B["xb"], B["wuv"]], [pbv])
        emit_gelu(P, C, T["m_u"][:], psu[:], pbu, [B["m_u"]], [128, 512])
        emit_gelu(P, C, T["m_v"][:], psv[:], pbv, [B["m_v"]], [128, 512])
        st = T["m_st"]
        P.op("dve", lambda e: e.reduce_sum(out=st[:, 0:1], in_=T["m_v"][:], axis=AX.X), reads=[B["m_v"]], writes=[B["m_st"]])
        v_tt(P, "pool", T["m_sq"][:], T["m_v"][:], T["m_v"][:], ALU.mult, [B["m_v"]], [B["m_sq"]])
        P.op("dve", lambda e: e.reduce_sum(out=st[:, 1:2], in_=T["m_sq"][:], axis=AX.X), reads=[B["m_sq"]], writes=[B["m_st"]])
        v_ts(P, "dve", st[:, 0:2], st[:, 0:2], 1.0 / 512.0, None, ALU.mult, None, [B["m_st"]], [B["m_st"]])
        v_tt(P, "dve", st[:, 2:3], st[:, 0:1], st[:, 0:1], ALU.mult, [B["m_st"]], [B["m_st"]])
        v_tt(P, "dve", st[:, 1:2], st[:, 1:2], st[:, 2:3], ALU.subtract, [B["m_st"]], [B["m_st"]])
        emit_rstd(P, C, st[:, 1:2], B["m_st"])
        P.op("dve", lambda e: e.tensor_scalar(out=T["m_t"][:], in0=T["m_v"][:], scalar1=st[:, 0:1], scalar2=st[:, 1:2],
                                              op0=ALU.subtract, op1=ALU.mult), reads=[B["m_v"], B["m_st"]], writes=[B["m_t"]])
        v_tt(P, "pool", T["m_t"][:], T["m_t"][:], lng[:], ALU.mult, [B["m_t"], B["lng"]], [B["m_t"]])
        v_tt(P, "dve", T["m_vb"][:], T["m_t"][:], lnb[:], ALU.add, [B["m_t"], B["lnb"]], [B["m_vb"]])
        pss, pbs = C.next_ps()
        for g in range(8):
            mm(P, pss[:, g * 64:(g + 1) * 64], wsT[:, g, :], T["m_vb"][:, g * 64:(g + 1) * 64], True, True, [B["wsT"], B["m_vb"]], [pbs])
        v_tt(P, "dve", T["m_t"][:].rearrange("p (g d) -> p g d", g=8), pss[:].rearrange("p (g d) -> p g d", g=8),
             bsT[:].unsqueeze(2).broadcast_to([128, 8, 64]), ALU.add, [pbs, B["bsT"]], [B["m_t"]])
        v_tt(P, "dve", T["m_y"][:], T["m_t"][:], T["m_u"][:], ALU.mult, [B["m_t"], B["m_u"]], [B["m_y"]])
        pst, pbt = C.next_ps()
        for m in range(4):
            mm(P, pst[:, m * 128:(m + 1) * 128], T["m_y"][:, m * 128:(m + 1) * 128], idb[:], True, True, [B["m_y"], B["c_ident_bf"]], [pbt])
        v_cp(P, "act", T["m_yf"][:].rearrange("p m t -> p (m t)"), pst[:], [pbt], [B["m_yf"]])
        P.dma("sp", ygm_out.rearrange("(m p) t -> p m t", p=128)[:, :, tok], T["m_yf"][:], reads=[B["m_yf"]], writes=[ygm_buf])


def emit_memattn(P, C, xb, W, memT, ymem_out, ymem_buf):
    T, B = C.t, C.b
    wq = load_w(P, C, "wq", [128, 8, 512], kview(W["w_in"])[:, :, 1536:2048])
    wkv = load_w(P, C, "wkv", [128, 8, 1024], kview(W["w_kv"]))
    mT = load_w(P, C, "mT", [128, 8, 256], kview(memT))
    C.sb("k_kT", [128, 4, 256], BF16)
    C.sb("k_v", [128, 2, 512], BF16)
    C.sb("k_q", [128, 4, NT], BF16)
    C.sb("k_e", [128, 2, 512], BF16)
    C.sb("k_rs", [128, 512], F32)
    C.sb("k_o", [128, 4, 512], BF16)
    C.sb("k_ones", [128, 128], BF16)
    P.op("dve", lambda e: e.memset(T["k_ones"][:], 1.0), writes=[B["k_ones"]])
    for h in range(4):
        ps, pb = C.next_ps()
        for kd in range(8):
            mm(P, ps[:, 0:256], wkv[:, kd, h * 128:(h + 1) * 128], mT[:, kd, :], kd == 0, kd == 7, [B["wkv"], B["mT"]], [pb])
        v_cp(P, "act", T["k_kT"][:, h, :], ps[:, 0:256], [pb], [B["k_kT"]])
    for mt in range(2):
        ps, pb = C.next_ps()
        for kd in range(8):
            mm(P, ps[:], mT[:, kd, mt * 128:(mt + 1) * 128], wkv[:, kd, 512:1024], kd == 0, kd == 7, [B["mT"], B["wkv"]], [pb])
        v_cp(P, "dve", T["k_v"][:, mt, :], ps[:], [pb], [B["k_v"]])
    for tb in range(NTB):
        sl = slice(tb * TB, (tb + 1) * TB)
        for h in range(4):
            ps, pb = C.next_ps()
            for kd in range(8):
                mm(P, ps[:], wq[:, kd, h * 128:(h + 1) * 128], xb[:, kd, sl], kd == 0, kd == 7, [B["wq"], B["xb"]], [pb])
            v_cp(P, "act" if h % 2 else "dve", T["k_q"][:, h, sl], ps[:], [pb], [B["k_q"]])
    scale = float(128.0 ** -0.5)
    for tb in range(NTB):
        sl = slice(tb * TB, (tb + 1) * TB)
        for h in range(4):
            for mt in range(2):
                ps, pb = C.next_ps()
                mm(P, ps[:], T["k_kT"][:, h, mt * 128:(mt + 1) * 128], T["k_q"][:, h, sl], True, True, [B["k_kT"], B["k_q"]], [pb])
                v_act(P, T["k_e"][:, mt, :], ps[:], AF.Exp, [pb], [B["k_e"]], scale=scale)
            pss, pbs = C.next_ps()
            pso, pbo = C.next_ps()
            for mt in range(2):
                mm(P, pss[:], T["k_ones"][:], T["k_e"][:, mt, :], mt == 0, mt == 1, [B["k_ones"], B["k_e"]], [pbs])
            for mt in range(2):
                mm(P, pso[:], T["k_v"][:, mt, h * 128:(h + 1) * 128], T["k_e"][:, mt, :], mt == 0, mt == 1, [B["k_v"], B["k_e"]], [pbo])
            P.op("dve", lambda e, pss=pss: e.reciprocal(out=T["k_rs"][:], in_=pss[:]), reads=[pbs], writes=[B["k_rs"]])
            v_tt(P, "dve", T["k_o"][:, h, :], pso[:], T["k_rs"][:], ALU.mult, [pbo, B["k_rs"]], [B["k_o"]])
        P.dma("sp", ymem_out.rearrange("(m p) t -> p m t", p=128)[:, :, sl], T["k_o"][:], reads=[B["k_o"]], writes=[ymem_buf])


def emit_ln_block(P, C, z, zbuf, nblk, g_t, b_t, pref, sink):
    T, B = C.t, C.b
    ps1, pb1 = C.next_ps()
    ps2, pb2 = C.next_ps()
    for m in range(8):
        mm(P, ps1[:, 0:nblk], T["k_onesf"][:], z[:, m, 0:nblk], m == 0, m == 7, [B["k_onesf"], zbuf], [pb1])
    for m in range(8):
        sq, sqb = T[pref + "sq%d" % (m % 2)], B[pref + "sq%d" % (m % 2)]
        v_tt(P, "pool", sq[:, 0:nblk], z[:, m, 0:nblk], z[:, m, 0:nblk], ALU.mult, [zbuf], [sqb])
        mm(P, ps2[:, 0:nblk], T["k_onesf"][:], sq[:, 0:nblk], m == 0, m == 7, [B["k_onesf"], sqb], [pb2])
    mean, var = T[pref + "mean"], T[pref + "var"]
    v_ts(P, "dve", mean[:, 0:nblk], ps1[:, 0:nblk], 1.0 / D, None, ALU.mult, None, [pb1], [B[pref + "mean"]])
    v_ts(P, "dve", var[:, 0:nblk], ps2[:, 0:nblk], 1.0 / D, None, ALU.mult, None, [pb2], [B[pref + "var"]])
    sq, sqb = T[pref + "sq0"], B[pref + "sq0"]
    v_tt(P, "pool", sq[:, 0:nblk], mean[:, 0:nblk], mean[:, 0:nblk], ALU.mult, [B[pref + "mean"]], [sqb])
    v_tt(P, "dve", var[:, 0:nblk], var[:, 0:nblk], sq[:, 0:nblk], ALU.subtract, [B[pref + "var"], sqb], [B[pref + "var"]])
    emit_rstd(P, C, var[:, 0:nblk], B[pref + "var"])
    for m in range(8):
        t, tb_ = T[pref + "sq%d" % (m % 2)], B[pref + "sq%d" % (m % 2)]
        o, ob = T[pref + "o%d" % (m % 2)], B[pref + "o%d" % (m % 2)]
        v_tt(P, "dve", t[:, 0:nblk], z[:, m, 0:nblk], mean[:, 0:nblk], ALU.subtract, [zbuf, B[pref + "mean"]], [tb_])
        v_tt(P, "dve", t[:, 0:nblk], t[:, 0:nblk], var[:, 0:nblk], ALU.mult, [tb_, B[pref + "var"]], [tb_])
        v_act(P, o[:, 0:nblk], t[:, 0:nblk], AF.Identity, [tb_, B[g_t[1]], B[b_t[1]]], [ob],
              bias=b_t[0][:, m:m + 1], scale=g_t[0][:, m:m + 1])
        sink(m, o[:, 0:nblk], ob)


def alloc_ln(C, pref, nblk):
    for n in ("sq0", "sq1", "o0", "o1", "mean", "var"):
        C.sb(pref + n, [128, nblk], F32)
    if "k_onesf" not in C.t or True:
        C.sb("k_onesf", [128, 128], F32)
        C.P.op("dve", lambda e: e.memset(C.t["k_onesf"][:], 1.0), writes=[C.b["k_onesf"]])


def emit_merge(P, C, xb, W, xT_in, ybr, ybr_bufs, x1_out, x1_buf):
    T, B = C.t, C.b
    wbr = [load_w(P, C, "wbr%d" % i, [128, 4, 1024], kview(W["w_br"][i])) for i in range(3)]
    wout = load_w(P, C, "wout", [128, 8, 1024], kview(W["w_out"]))
    bg = load_w(P, C, "bg", [128, 24], W["bgT"], F32)
    g1 = load_w(P, C, "ln1g", [128, 8], W["ln1gT"], F32)
    b1 = load_w(P, C, "ln1b", [128, 8], W["ln1bT"], F32)
    wgs = [load_w(P, C, "wg%d" % i, [128, 8, 1024], kview(W["w_gate"])[:, :, i * 1024:(i + 1) * 1024]) for i in range(3)]
    for i in range(3):
        C.sb("ybr%d" % i, [128, 4, TB], BF16)
    C.sb("e_sig", [128, 512], F32)
    C.sb("e_tmp", [128, 512], F32)
    C.sb("e_acc", [128, 512], F32)
    C.sb("e_mb", [128, 8, 512], BF16)
    C.sb("e_z", [128, 8, 512], F32)
    C.sb("e_x0", [128, 512], F32)
    C.sb("e_x1", [128, 512], F32)
    alloc_ln(C, "l1_", 512)
    xv = xT_in.rearrange("(m p) t -> p m t", p=128)
    ov = x1_out.rearrange("(m p) t -> p m t", p=128)
    for tb in range(NTB):
        sl = slice(tb * TB, (tb + 1) * TB)
        for i in range(3):
            P.dma("sp", T["ybr%d" % i][:], ybr[i].rearrange("(m p) t -> p m t", p=128)[:, :, sl], reads=[ybr_bufs[i]], writes=[B["ybr%d" % i]])
        for m in range(8):
            for i in range(3):
                psg, pbg = C.next_ps()
                for kd in range(8):
                    mm(P, psg[:], wgs[i][:, kd, m * 128:(m + 1) * 128], xb[:, kd, sl], kd == 0, kd == 7, [B["wg%d" % i], B["xb"]], [pbg])
                psb, pbb = C.next_ps()
                for k in range(4):
                    mm(P, psb[:], wbr[i][:, k, m * 128:(m + 1) * 128], T["ybr%d" % i][:, k, :], k == 0, k == 3, [B["wbr%d" % i], B["ybr%d" % i]], [pbb])
                v_act(P, T["e_sig"][:], psg[:], AF.Sigmoid, [pbg, B["bg"]], [B["e_sig"]], bias=bg[:, i * 8 + m:i * 8 + m + 1])
                if i == 0:
                    v_tt(P, "dve", T["e_acc"][:], psb[:], T["e_sig"][:], ALU.mult, [pbb, B["e_sig"]], [B["e_acc"]])
                else:
                    v_tt(P, "dve", T["e_tmp"][:], psb[:], T["e_sig"][:], ALU.mult, [pbb, B["e_sig"]], [B["e_tmp"]])
                    if i == 1:
                        v_tt(P, "pool", T["e_acc"][:], T["e_acc"][:], T["e_tmp"][:], ALU.add, [B["e_acc"], B["e_tmp"]], [B["e_acc"]])
                    else:
                        v_tt(P, "dve", T["e_mb"][:, m, :], T["e_acc"][:], T["e_tmp"][:], ALU.add, [B["e_acc"], B["e_tmp"]], [B["e_mb"]])
        for m in range(8):
            pso, pbo = C.next_ps()
            for k in range(8):
                mm(P, pso[:], wout[:, k, m * 128:(m + 1) * 128], T["e_mb"][:, k, :], k == 0, k == 7, [B["wout"], B["e_mb"]], [pbo])
            ex, exb = T["e_x%d" % (m % 2)], B["e_x%d" % (m % 2)]
            P.dma("sp", ex[:], xv[:, m, sl], writes=[exb])
            P.op("dve", lambda e, m=m, pso=pso, ex=ex: e.scalar_tensor_tensor(out=T["e_z"][:, m, :], in0=ex[:], scalar=ALPHA, in1=pso[:],
                                                                             op0=ALU.mult, op1=ALU.add), reads=[exb, pbo], writes=[B["e_z"]])

        def sink(m, ap, buf, sl=sl):
            P.dma("sp", ov[:, m, sl], ap, reads=[buf], writes=[x1_buf])
        emit_ln_block(P, C, T["e_z"], B["e_z"], 512, (g1, "ln1g"), (b1, "ln1b"), "l1_", sink)


def emit_moe(P, C, W, x1_in, x1_buf, x_out, xout_buf):
    T, B = C.t, C.b
    acc = C.sb("x_acc", [128, 8, NT], F32)
    x1b = C.sb("x_1b", [128, 8, NT], BF16)
    P.dma("sp", acc[:], x1_in.rearrange("(m p) t -> p m t", p=128), reads=[x1_buf], writes=[B["x_acc"]])
    P.dma("pool", x1b[:], x1_in.rearrange("(m p) t -> p m t", p=128), reads=[x1_buf], writes=[B["x_1b"]])
    for par in range(2):
        for j in range(2):
            C.sb("xg%d%d" % (par, j), [128, 8, 256], BF16)
            C.sb("xu%d%d" % (par, j), [128, 8, 256], BF16)
            C.sb("xd%d%d" % (par, j), [128, 2, 1024], BF16)

    def load_pair(pair):
        par = pair % 2
        for j in range(2):
            ex = pair * 2 + j
            P.dma("pool", T["xg%d%d" % (par, j)][:], kview(W["w_exp_gate"][ex]), writes=[B["xg%d%d" % (par, j)]])
            P.dma("pool", T["xu%d%d" % (par, j)][:], kview(W["w_exp_up"][ex]), writes=[B["xu%d%d" % (par, j)]])
            P.dma("pool", T["xd%d%d" % (par, j)][:], kview(W["w_exp_down"][ex]), writes=[B["xd%d%d" % (par, j)]])
    load_pair(0)
    load_pair(1)
    wr = load_w(P, C, "wr", [128, 8, 36], kview(W["wr"]), F32)
    br = load_w(P, C, "br", [128, 36], W["br"], F32)
    g2 = load_w(P, C, "ln2g", [128, 8], W["ln2gT"], F32)
    b2 = load_w(P, C, "ln2b", [128, 8], W["ln2bT"], F32)
    selE = load_w(P, C, "selE", [128, 32, 128], W["c_selE"].rearrange("p (e m) -> p e m", e=32))
    alloc_ln(C, "l2_", 512)
    NTT = NT // 128
    lg = C.sb("r_lg", [128, NTT, 36], F32)
    for half in range(2):
        ps, pb = C.next_ps()
        for t8 in range(8):
            tt = half * 8 + t8
            for kd in range(8):
                mm(P, ps[:, t8 * 36:(t8 + 1) * 36], acc[:, kd, tt * 128:(tt + 1) * 128], wr[:, kd, :], kd == 0, kd == 7, [B["x_acc"], B["wr"]], [pb])
        v_tt(P, "dve", lg[:, half * 8:(half + 1) * 8, :], ps[:, 0:288].rearrange("p (t c) -> p t c", c=36),
             br[:].unsqueeze(1).broadcast_to([128, 8, 36]), ALU.add, [pb, B["br"]], [B["r_lg"]])
    for n, shp in (("r_mg", [128, NTT]), ("r_goh", [128, NTT, 4]), ("r_eg", [128, NTT, 4]), ("r_pg", [128, NTT]),
                   ("r_t48", [128, NTT, 4, 8]), ("r_les", [128, NTT, 8]), ("r_m1", [128, NTT]), ("r_oh1", [128, NTT, 8]),
                   ("r_le2", [128, NTT, 8]), ("r_m2", [128, NTT]), ("r_oh2", [128, NTT, 8]), ("r_pe1", [128, NTT]),
                   ("r_pe2", [128, NTT]), ("r_w8", [128, NTT, 8]), ("r_W32", [128, NTT, 4, 8])):
        C.sb(n, shp, F32)
    e = "dve"
    lgg = lg[:, :, 0:4]
    le = lg[:, :, 4:36].rearrange("p t (g e) -> p t g e", g=4)
    s3 = [128, NTT, 4]
    s8 = [128, NTT, 8]
    s48 = [128, NTT, 4, 8]
    P.op(e, lambda en: en.tensor_reduce(out=T["r_mg"][:], in_=lgg, axis=AX.X, op=ALU.max), reads=[B["r_lg"]], writes=[B["r_mg"]])
    v_tt(P, e, T["r_goh"][:], lgg, T["r_mg"][:].unsqueeze(2).broadcast_to(s3), ALU.is_equal, [B["r_lg"], B["r_mg"]], [B["r_goh"]])
    v_tt(P, e, T["r_eg"][:], lgg, T["r_mg"][:].unsqueeze(2).broadcast_to(s3), ALU.subtract, [B["r_lg"], B["r_mg"]], [B["r_eg"]])
    v_act(P, T["r_eg"][:], T["r_eg"][:], AF.Exp, [B["r_eg"]], [B["r_eg"]])
    P.op(e, lambda en: en.reduce_sum(out=T["r_pg"][:], in_=T["r_eg"][:], axis=AX.X), reads=[B["r_eg"]], writes=[B["r_pg"]])
    P.op(e, lambda en: en.reciprocal(out=T["r_pg"][:], in_=T["r_pg"][:]), reads=[B["r_pg"]], writes=[B["r_pg"]])
    v_tt(P, e, T["r_t48"][:], le, T["r_goh"][:].unsqueeze(3).broadcast_to(s48), ALU.mult, [B["r_lg"], B["r_goh"]], [B["r_t48"]])
    P.op(e, lambda en: en.reduce_sum(out=T["r_les"][:], in_=T["r_t48"][:].rearrange("p t g e -> p t e g"), axis=AX.X),
         reads=[B["r_t48"]], writes=[B["r_les"]])
    P.op(e, lambda en: en.tensor_reduce(out=T["r_m1"][:], in_=T["r_les"][:], axis=AX.X, op=ALU.max), reads=[B["r_les"]], writes=[B["r_m1"]])
    v_tt(P, e, T["r_oh1"][:], T["r_les"][:], T["r_m1"][:].unsqueeze(2).broadcast_to(s8), ALU.is_equal, [B["r_les"], B["r_m1"]], [B["r_oh1"]])
    P.op(e, lambda en: en.scalar_tensor_tensor(out=T["r_le2"][:], in0=T["r_oh1"][:], scalar=-1.0e30, in1=T["r_les"][:], op0=ALU.mult, op1=ALU.add),
         reads=[B["r_oh1"], B["r_les"]], writes=[B["r_le2"]])
    P.op(e, lambda en: en.tensor_reduce(out=T["r_m2"][:], in_=T["r_le2"][:], axis=AX.X, op=ALU.max), reads=[B["r_le2"]], writes=[B["r_m2"]])
    v_tt(P, e, T["r_oh2"][:], T["r_le2"][:], T["r_m2"][:].unsqueeze(2).broadcast_to(s8), ALU.is_equal, [B["r_le2"], B["r_m2"]], [B["r_oh2"]])
    v_tt(P, e, T["r_pe1"][:], T["r_m2"][:], T["r_m1"][:], ALU.subtract, [B["r_m1"], B["r_m2"]], [B["r_pe1"]])
    v_act(P, T["r_pe1"][:], T["r_pe1"][:], AF.Exp, [B["r_pe1"]], [B["r_pe1"]])
    v_ts(P, e, T["r_pe1"][:], T["r_pe1"][:], 1.0, None, ALU.add, None, [B["r_pe1"]], [B["r_pe1"]])
    P.op(e, lambda en: en.reciprocal(out=T["r_pe1"][:], in_=T["r_pe1"][:]), reads=[B["r_pe1"]], writes=[B["r_pe1"]])
    v_ts(P, e, T["r_pe2"][:], T["r_pe1"][:], -1.0, 1.0, ALU.mult, ALU.add, [B["r_pe1"]], [B["r_pe2"]])
    v_tt(P, e, T["r_pe1"][:], T["r_pe1"][:], T["r_pg"][:], ALU.mult, [B["r_pe1"], B["r_pg"]], [B["r_pe1"]])
    v_tt(P, e, T["r_pe2"][:], T["r_pe2"][:], T["r_pg"][:], ALU.mult, [B["r_pe2"], B["r_pg"]], [B["r_pe2"]])
    v_tt(P, e, T["r_w8"][:], T["r_oh1"][:], T["r_pe1"][:].unsqueeze(2).broadcast_to(s8), ALU.mult, [B["r_oh1"], B["r_pe1"]], [B["r_w8"]])
    v_tt(P, e, T["r_oh2"][:], T["r_oh2"][:], T["r_pe2"][:].unsqueeze(2).broadcast_to(s8), ALU.mult, [B["r_oh2"], B["r_pe2"]], [B["r_oh2"]])
    v_tt(P, e, T["r_w8"][:], T["r_w8"][:], T["r_oh2"][:], ALU.add, [B["r_w8"], B["r_oh2"]], [B["r_w8"]])
    v_tt(P, e, T["r_W32"][:], T["r_goh"][:].unsqueeze(3).broadcast_to(s48), T["r_w8"][:].unsqueeze(2).broadcast_to(s48), ALU.mult,
         [B["r_goh"], B["r_w8"]], [B["r_W32"]])
    wT = C.sb("r_wT", [128, NT], BF16)
    P.op("dve", lambda en: en.memset(wT[:], 0.0), writes=[B["r_wT"]])
    for q in range(NTT // 4):
        ps, pb = C.next_ps()
        for j in range(4):
            tt = q * 4 + j
            mm(P, ps[0:32, j * 128:(j + 1) * 128], T["r_W32"][:, tt].rearrange("p g e -> p (g e)"), T["c_ident"][:], True, True,
               [B["r_W32"], B["c_ident"]], [pb])
        v_cp(P, "dve", wT[0:32, q * 512:(q + 1) * 512], ps[0:32, :], [pb], [B["r_wT"]])
    NB = 256
    for n in ("x_sg", "x_h"):
        C.sb(n, [128, 2, NB], F32)
    for j in range(2):
        C.sb("x_h2%d" % j, [128, 2, NB], BF16)
    for j in range(2):
        C.sb("x_ev%d" % j, [128, 2, NB], F32)
    C.ps_lim = 4
    C.ps_rr = 0
    ev_rr = 0
    for pair in range(16):
        par = pair % 2
        for tb in range(NT // NB):
            sl = slice(tb * NB, (tb + 1) * NB)
            for j in range(2):
                ex = pair * 2 + j
                wg_, wu_ = T["xg%d%d" % (par, j)], T["xu%d%d" % (par, j)]
                psg, pbg = C.next_ps()
                psu, pbu = C.next_ps()
                psw, pbw = C.next_ps()
                for f in range(2):
                    for kd in range(8):
                        mm(P, psg[:, f * NB:(f + 1) * NB], wg_[:, kd, f * 128:(f + 1) * 128], x1b[:, kd, sl], kd == 0, kd == 7,
                           [B["xg%d%d" % (par, j)], B["x_1b"]], [pbg])
                for f in range(2):
                    for kd in range(8):
                        mm(P, psu[:, f * NB:(f + 1) * NB], wu_[:, kd, f * 128:(f + 1) * 128], x1b[:, kd, sl], kd == 0, kd == 7,
                           [B["xu%d%d" % (par, j)], B["x_1b"]], [pbu])
                mm(P, psw[:, 0:NB], selE[:, ex, :], wT[:, sl], True, True, [B["selE"], B["r_wT"]], [pbw])
                v_act(P, T["x_sg"][:].rearrange("p f n -> p (f n)"), psg[:], AF.Silu, [pbg], [B["x_sg"]])
                v_tt(P, "dve", T["x_h"][:].rearrange("p f n -> p (f n)"), T["x_sg"][:].rearrange("p f n -> p (f n)"), psu[:], ALU.mult,
                     [B["x_sg"], pbu], [B["x_h"]])
                v_tt(P, "dve", T["x_h2%d" % j][:], T["x_h"][:], psw[:, 0:NB].unsqueeze(1).broadcast_to([128, 2, NB]), ALU.mult,
                     [B["x_h"], pbw], [B["x_h2%d" % j]])
            for m in range(8):
                psa, pba = C.ps[4 + m // 2], C.psb[4 + m // 2]
                o = psa[:, (m % 2) * NB:(m % 2 + 1) * NB]
                for j in range(2):
                    for f in range(2):
                        mm(P, o, T["xd%d%d" % (par, j)][:, f, m * 128:(m + 1) * 128], T["x_h2%d" % j][:, f, :], j == 0 and f == 0, j == 1 and f == 1,
                           [B["xd%d%d" % (par, j)], B["x_h2%d" % j]], [pba])
            for bk in range(4):
                psa, pba = C.ps[4 + bk], C.psb[4 + bk]
                pv = psa[:].rearrange("p (f n) -> p f n", f=2)
                av = acc[:, 2 * bk:2 * bk + 2, sl]
                if pair == 0:
                    P.op("dve", lambda e, av=av, pv=pv: e.scalar_tensor_tensor(out=av, in0=av, scalar=ALPHA, in1=pv, op0=ALU.mult, op1=ALU.add),
                         reads=[B["x_acc"], pba], writes=[B["x_acc"]])
                else:
                    v_tt(P, "dve", av, av, pv, ALU.add, [B["x_acc"], pba], [B["x_acc"]])
        if pair + 2 < 16:
            load_pair(pair + 2)
    C.ps_lim = 8
    ov = x_out.rearrange("(m p) t -> p m t", p=128)
    for tb in range(NTB):
        sl = slice(tb * TB, (tb + 1) * TB)

        def sink(m, ap, buf, sl=sl):
            P.dma("sp", ov[:, m, sl], ap, reads=[buf], writes=[xout_buf])
        emit_ln_block(P, C, acc[:, :, sl], B["x_acc"], 512, (g2, "ln2g"), (b2, "ln2b"), "l2_", sink)


def host_layer_weights(inp, l):
    f = np.float32
    c = np.ascontiguousarray
    W = {}
    W["w_in"] = c(inp["w_in"][l])
    W["w_gate"] = c(inp["w_gate"][l])
    W["bgT"] = c(inp["b_gate"][l].reshape(24, 128).T)
    W["w_glu"] = c(inp["w_glu"][l])
    W["bglu"] = c(inp["b_glu"][l].reshape(4, 128).T)
    W["wsT"] = c(np.transpose(inp["w_spatial"][l], (2, 0, 1)).reshape(128, 1024))
    W["lng"] = c(np.broadcast_to(inp["gmlp_ln_g"][l][None, :], (128, 512)))
    W["lnb"] = c(np.broadcast_to(inp["gmlp_ln_b"][l][None, :], (128, 512)))
    W["bsT"] = c(inp["b_spatial"][l].T)
    W["w_kv"] = c(inp["w_kv"][l])
    W["w_br"] = c(inp["w_br"][l])
    W["w_out"] = c(inp["w_out"][l])
    W["ln1gT"] = c(inp["ln1_g"][l].reshape(8, 128).T)
    W["ln1bT"] = c(inp["ln1_b"][l].reshape(8, 128).T)
    W["wr"] = c(np.concatenate([inp["w_router_g"][l], np.transpose(inp["w_router_e"][l], (1, 0, 2)).reshape(D, 32)], axis=1))
    brow = np.concatenate([inp["b_router_g"][l], inp["b_router_e"][l].reshape(32)])
    W["br"] = c(np.broadcast_to(brow[None, :], (128, 36)))
    W["w_exp_gate"] = c(inp["w_exp_gate"][l])
    W["w_exp_up"] = c(inp["w_exp_up"][l])
    W["w_exp_down"] = c(inp["w_exp_down"][l])
    W["ln2gT"] = c(inp["ln2_g"][l].reshape(8, 128).T)
    W["ln2bT"] = c(inp["ln2_b"][l].reshape(8, 128).T)
    for k, v in host_s5_params(inp, l).items():
        W["s5_" + k] = v
    return {k: np.asarray(v, dtype=f) for k, v in W.items()}


def host_const_all():
    cst = host_consts()
    selE = np.zeros((128, 32, 128), np.float32)
    for e_ in range(32):
        selE[e_, e_, :] = 1.0
    cst["c_selE"] = np.ascontiguousarray(selE.reshape(128, 32 * 128))
    return cst


W_SHAPES = {
    "w_in": [1024, 2048], "w_gate": [1024, 3072], "bgT": [128, 24], "w_glu": [512, 512], "bglu": [128, 4], "wsT": [128, 1024],
    "lng": [128, 512], "lnb": [128, 512], "bsT": [128, 8], "w_kv": [1024, 1024], "w_br": [3, 512, 1024], "w_out": [1024, 1024],
    "ln1gT": [128, 8], "ln1bT": [128, 8], "wr": [1024, 36], "br": [128, 36], "w_exp_gate": [32, 1024, 256], "w_exp_up": [32, 1024, 256],
    "w_exp_down": [32, 256, 1024], "ln2gT": [128, 8], "ln2bT": [128, 8],
    "s5_lam_re": [128, 32], "s5_lam_im": [128, 32], "s5_lstep": [128, 32], "s5_dm": [128, 32],
    "s5_b_re": [128, 512], "s5_b_im": [128, 512], "s5_c_re": [128, 512], "s5_c_im": [128, 512],
}
A_KEYS = ["w_in", "s5_lam_re", "s5_lam_im", "s5_lstep", "s5_dm", "s5_b_re", "s5_b_im", "s5_c_re", "s5_c_im"]
CONST_SHAPES = {"c_ident": [128, 128], "c_wide": [128, 1920], "c_maskF": [128, 128], "c_maskB": [128, 128], "c_kvec": [128, NK],
                "c_selE": [128, 4096]}


S5_TABLES = (("w_Msum", 32 * 128), ("w_Wl", 32 * 2 * 128), ("w_Glre", 32 * 8 * 16), ("w_GlimN", 32 * 8 * 16), ("q_A2", 128), ("q_P", 192))


def _flat(ap, name):
    if name in ("w_Msum",):
        return ap.rearrange("p g m -> p (g m)")
    if name == "w_Wl":
        return ap.rearrange("p g r m -> p (g r m)")
    if name in ("w_Glre", "w_GlimN"):
        return ap.rearrange("p g j h -> p (g j h)")
    if name == "q_A2":
        return ap.rearrange("p a r g -> p (a r g)")
    return ap.rearrange("p m r g -> p (m r g)")


def emit_s5_stage(P, C, W, xT, pred, lout, ybr0, ybr0_buf, stop_after_scan, tabs=None):
    T, B = C.t, C.b
    C.push()
    alloc_s5_persist(C)
    if stop_after_scan or tabs is None:
        C.push()
        alloc_s5_prep(C)
        emit_s5_prep(P, C, {k[3:]: W[k] for k in W if k.startswith("s5_")})
        C.pop()
        if tabs is not None:
            for n_, _w in S5_TABLES:
                P.dma("pool", tabs[n_], _flat(T[n_][:], n_), reads=[B[n_]], writes=[Buf("tab_" + n_)])
    else:
        for n_, _w in S5_TABLES:
            P.dma("pool", _flat(T[n_][:], n_), tabs[n_], writes=[B[n_]])
    alloc_s5_main(C)
    Hs = T["a_Hs"]
    if pred is None:
        P.op("dve", lambda e: e.memset(Hs[:, 0], 0.0), writes=[B["a_Hs"]])
    else:
        pr = C.sb("a_pr", [128, 3, 2, 32], F32)
        P.dma("sp", pr[:], pred.rearrange("m p (r g) -> p m r g", r=2), writes=[B["a_pr"]])
        st = T["a_st"]
        v_cp(P, "dve", Hs[:, 0], pr[:, 0], [B["a_pr"]], [B["a_Hs"]])
        Pm = T["q_P"]
        for m in range(2):
            cmul(P, "dve", (st[:, 0, 0], B["a_st"]), (st[:, 0, 1], B["a_st"]), (Pm[:, m, 0], B["q_P"]), (Pm[:, m, 1], B["q_P"]),
                 (pr[:, m + 1, 0], B["a_pr"]), (pr[:, m + 1, 1], B["a_pr"]), (st[:, 1, 0], B["a_st"]))
            v_tt(P, "dve", Hs[:, 0], Hs[:, 0], st[:, 0], ALU.add, [B["a_Hs"], B["a_st"]], [B["a_Hs"]])
    C.push()
    xb = load_w(P, C, "xb", [128, 8, NT], kview(xT))
    win = load_w(P, C, "win", [128, 8, 512], kview(W["w_in"])[:, :, 0:512])
    emit_s5_front(P, C, xb, B["xb"], win, B["win"])
    C.pop()
    emit_s5_mid(P, C)
    if stop_after_scan:
        P.dma("sp", lout, Hs[:, NCH].rearrange("p r g -> p (r g)"), reads=[B["a_Hs"]], writes=[Buf("lout")])
        C.pop()
        return
    alloc_gelu(C)
    wglu = load_w(P, C, "wglu", [128, 4, 512], kview(W["w_glu"]))
    bglu = load_w(P, C, "bglu", [128, 4], W["bglu"], F32)
    emit_s5_back(P, C, wglu, B["wglu"], bglu, B["bglu"])
    P.dma("sp", ybr0.rearrange("(m p) t -> p m t", p=128), T["a_U"][:], reads=[B["a_U"]], writes=[ybr0_buf])
    C.pop()


def build_program(mode, stages=("s5", "gmlp", "attn", "merge", "moe")):
    nc = bass.Bass("TRN2", target_bir_lowering=False)

    def din(n, shape):
        return nc.dram_tensor(n, list(shape), F32, kind="ExternalInput").ap()
    xT = din("xT", [D, NT])
    cst = {k: din(k, v) for k, v in CONST_SHAPES.items()}
    keys = A_KEYS if mode == "A" else list(W_SHAPES.keys())
    W = {k: din(k, W_SHAPES[k]) for k in keys}
    W["c_selE"] = cst["c_selE"]
    P = Prog(nc)
    C = Ctx(P)
    C.alloc_psum()
    load_consts(P, C, cst)
    T, B = C.t, C.b
    if mode == "A":
        lout = nc.dram_tensor("lout", [128, 64], F32, kind="ExternalOutput").ap()
        tabs = {n_: nc.dram_tensor("tab_" + n_, [128, w_], F32, kind="ExternalOutput").ap() for n_, w_ in S5_TABLES}
        emit_s5_stage(P, C, W, xT, None, lout, None, None, True, tabs)
        P.finish()
        return nc
    memT = din("memT", [D, 256])
    pred = din("pred", [3, 128, 64])
    tabs = {n_: din("tab_" + n_, [128, w_]) for n_, w_ in S5_TABLES}
    xout = nc.dram_tensor("xout", [D, NT], F32, kind="ExternalOutput").ap()
    ybr = [nc.dram_tensor("ybr%d" % i, [512, NT], BF16).ap() for i in range(3)]
    ybr_b = [Buf("ybr%d" % i) for i in range(3)]
    x1T = nc.dram_tensor("x1T", [D, NT], F32).ap()
    x1_b = Buf("x1T")
    xo_b = Buf("xout")
    if "s5" in stages:
        emit_s5_stage(P, C, W, xT, pred, None, ybr[0], ybr_b[0], False, tabs)
    if "gmlp" in stages:
        C.push()
        xb = load_w(P, C, "xb", [128, 8, NT], kview(xT))
        alloc_gelu(C)
        emit_gmlp(P, C, xb, W, ybr[1], ybr_b[1])
        C.pop()
    if "attn" in stages:
        C.push()
        xb = load_w(P, C, "xb", [128, 8, NT], kview(xT))
        emit_memattn(P, C, xb, W, memT, ybr[2], ybr_b[2])
        C.pop()
    if "merge" in stages:
        C.push()
        xb = load_w(P, C, "xb", [128, 8, NT], kview(xT))
        emit_merge(P, C, xb, W, xT, ybr, ybr_b, x1T, x1_b)
        C.pop()
    if "moe" in stages:
        C.push()
        emit_moe(P, C, W, x1T, x1_b, xout, xo_b)
        C.pop()
    print("n_ops", P.n_ops)
    P.finish()
    return nc


_PROGS = {}


def _prog(mode):
    if mode not in _PROGS:
        _PROGS[mode] = build_program(mode)
    return _PROGS[mode]


def kernel(**inputs):
    inp = {k: np.asarray(v) for k, v in inputs.items()}
    x = inp["x"].astype(np.float32)
    mem = inp["mem"].astype(np.float32)
    n = 8
    cst = host_const_all()
    xT = []
    memT = []
    for c_ in range(n):
        b, j = c_ // 4, c_ % 4
        xT.append(np.ascontiguousarray(x[b, j * NT:(j + 1) * NT].T))
        memT.append(np.ascontiguousarray(mem[b].T))
    for l in range(DEPTH):
        Wl = host_layer_weights(inp, l)
        ncA = _prog("A")
        mapsA = []
        for c_ in range(n):
            m_ = {"xT": xT[c_]}
            m_.update(cst)
            m_.update({k: Wl[k] for k in A_KEYS})
            mapsA.append(m_)
        resA = run_bass_kernel_spmd(ncA, mapsA, core_ids=list(range(n)))
        lo = [np.asarray(r["lout"], dtype=np.float32) for r in resA.results]
        ncB = _prog("B")
        mapsB = []
        for c_ in range(n):
            b, j = c_ // 4, c_ % 4
            pred = np.zeros((3, 128, 64), np.float32)
            for m in range(3):
                if j - 1 - m >= 0:
                    pred[m, 0:64] = lo[b * 4 + j - 1 - m][0:64]
                if j + 1 + m <= 3:
                    pred[m, 64:128] = lo[b * 4 + j + 1 + m][64:128]
            m_ = {"xT": xT[c_], "memT": memT[c_], "pred": pred}
            for n_, _w in S5_TABLES:
                m_["tab_" + n_] = np.asarray(resA.results[c_]["tab_" + n_], dtype=np.float32)
            m_.update(cst)
            m_.update(Wl)
            mapsB.append(m_)
        resB = run_bass_kernel_spmd(ncB, mapsB, core_ids=list(range(n)))
        xT = [np.asarray(r["xout"], dtype=np.float32) for r in resB.results]
    out = np.zeros((2, 8192, D), np.float32)
    for c_ in range(n):
        b, j = c_ // 4, c_ % 4
        out[b, j * NT:(j + 1) * NT] = xT[c_].T
    return out


def prog_coll(P, kind, ins, outs, reads=(), writes=()):
    eng = "pool"
    waits = P._waits(eng, reads, writes)
    key = P.dma_keys[eng][P.dma_rr[eng]]
    P.dma_rr[eng] = (P.dma_rr[eng] + 1) % len(P.dma_keys[eng])
    prev = P.cnt[key]
    if prev > 0 and P.seen[eng].get(key, 0) < prev:
        P.seen[eng][key] = prev
        waits.append((key, prev))
    P.cnt[key] += 16
    tick = (key, P.cnt[key])
    kk = dict(op=ALU.bypass, replica_groups=[list(range(8))], ins=ins, outs=outs)
    P.q[eng].append((waits, ("collective_compute", (kind,), kk), key, 16))
    P._commit(tick, reads, writes)
    P.n_ops += 1
```

```python
import numpy as np
from contextlib import ExitStack
import concourse.bass as bass
import concourse.mybir as mybir
from concourse.bass_utils import run_bass_kernel_spmd

F32 = mybir.dt.float32
BF16 = mybir.dt.bfloat16
I32 = mybir.dt.int32
AF = mybir.ActivationFunctionType
ALU = mybir.AluOpType
AX = mybir.AxisListType


class Buf:
    __slots__ = ("name", "w", "r", "excl")

    def __init__(self, name="", excl=False):
        self.name = name
        self.excl = excl
        self.w = None
        self.r = {}


import os as _os0
SAME_ENGINE_SYNC = _os0.environ.get('SAME_ENGINE_SYNC', '1') == '1'


class _Rec:
    def __init__(self):
        self.call = None

    def __getattr__(self, name):
        def f(*a, **k):
            self.call = (name, a, k)
            return self
        return f


class Prog:
    ENGS = ("pe", "act", "dve", "pool", "sp")

    def __init__(self, nc, n_dma_sems=16, same_engine_sync=SAME_ENGINE_SYNC):
        self.nc = nc
        self.es = ExitStack()
        self.q = {e: [] for e in self.ENGS}
        self.cnt = {}
        self.seen = {e: {} for e in self.ENGS}
        self.sem = {}
        for e in self.ENGS:
            self.sem["c_" + e] = self.es.enter_context(nc.semaphore("c_" + e))
            self.cnt["c_" + e] = 0
        self.dma_keys = {}
        self.dma_rr = {}
        for qe in ("sp", "pool", "act"):
            self.dma_keys[qe] = []
            self.dma_rr[qe] = 0
            for i in range(n_dma_sems if qe != "act" else 4):
                k = "d_%s_%d" % (qe, i)
                self.sem[k] = self.es.enter_context(nc.semaphore(k))
                self.cnt[k] = 0
                self.dma_keys[qe].append(k)
        self.same_engine_sync = same_engine_sync
        self.n_ops = 0
        self.pending = {e: {} for e in self.ENGS}
        self.open_scopes = []

    def sbuf(self, name, shape, dtype):
        return self.es.enter_context(self.nc.sbuf_tensor(name, list(shape), dtype))

    def psum(self, name, shape, dtype=F32):
        return self.es.enter_context(self.nc.psum_tensor(name, list(shape), dtype))

    def _waits(self, eng, reads, writes):
        deps = {}
        for b in reads:
            if b.w is not None:
                k, v = b.w
                deps[k] = max(deps.get(k, 0), v)
        for b in writes:
            if b.w is not None:
                k, v = b.w
                deps[k] = max(deps.get(k, 0), v)
            for k, v in b.r.items():
                deps[k] = max(deps.get(k, 0), v)
        for k, v in self.pending[eng].items():
            deps[k] = max(deps.get(k, 0), v)
        self.pending[eng] = {}
        waits = []
        own = "c_" + eng
        for k, v in deps.items():
            if k == own and (eng == "pe" or not self.same_engine_sync):
                continue
            if self.seen[eng].get(k, 0) >= v:
                continue
            self.seen[eng][k] = v
            waits.append((k, v))
        return waits

    def _commit(self, tick, reads, writes):
        k, v = tick
        for b in reads:
            b.r[k] = max(b.r.get(k, 0), v)
        for b in writes:
            b.w = tick
            b.r = {}

    def op(self, eng, fn, reads=(), writes=()):
        ex = [b for b in reads if b.excl]
        if ex:
            writes = list(writes) + ex
        waits = self._waits(eng, reads, writes)
        key = "c_" + eng
        self.cnt[key] += 1
        tick = (key, self.cnt[key])
        rec = _Rec()
        fn(rec)
        self.q[eng].append((waits, rec.call, key, 1))
        self._commit(tick, reads, writes)
        self.n_ops += 1

    def dma(self, eng, out, in_, reads=(), writes=(), **kw):
        waits = self._waits(eng, reads, writes)
        key = self.dma_keys[eng][self.dma_rr[eng]]
        self.dma_rr[eng] = (self.dma_rr[eng] + 1) % len(self.dma_keys[eng])
        prev = self.cnt[key]
        if prev > 0 and self.seen[eng].get(key, 0) < prev:
            self.seen[eng][key] = prev
            waits.append((key, prev))
        self.cnt[key] += 16
        tick = (key, self.cnt[key])
        kk = dict(kw)
        kk["out"] = out
        kk["in_"] = in_
        self.q[eng].append((waits, ("dma_start", (), kk), key, 16))
        self._commit(tick, reads, writes)
        self.n_ops += 1

    def barrier(self):
        for e in self.ENGS:
            for k, v in self.cnt.items():
                if v > 0:
                    self.pending[e][k] = max(self.pending[e].get(k, 0), v)

    def finish(self, final_bufs=()):
        fin = []
        for k, v in self.cnt.items():
            if v > 0 and self.seen["sp"].get(k, 0) < v and k != "c_sp":
                fin.append((k, v))
        nc = self.nc
        sem = self.sem
        q = self.q
        with nc.Block() as block:
            def emit(e, lst, extra=()):
                for waits, call, key, inc in lst:
                    for k, v in waits:
                        e.wait_ge(sem[k], v)
                    name, a, kw = call
                    getattr(e, name)(*a, **kw).then_inc(sem[key], inc)
                for k, v in extra:
                    e.wait_ge(sem[k], v)

            @block.sync
            def _(e):
                emit(e, q["sp"], fin)

            @block.scalar
            def _(e):
                emit(e, q["act"])

            @block.vector
            def _(e):
                emit(e, q["dve"])

            @block.gpsimd
            def _(e):
                emit(e, q["pool"])

            @block.tensor
            def _(e):
                emit(e, q["pe"])
        while self.open_scopes:
            self.open_scopes.pop().close()
        self.es.close()


D = 1024
NT = 2048
TB = 512
NTB = NT // TB
NCH = NT // 8
SCAN_K = 8
SCAN_L = NCH // SCAN_K
DEPTH = 4
ALPHA = (2.0 * DEPTH) ** 0.25
LN_EPS = 1e-5
KVEC = [0, -1, -2, -3, -4, -5, -6, -7, 0, 1, 2, 3, 4, 5, 6, 7, 1, 7, 8]
NK = len(KVEC)
MAGIC = 12582912.0
TWO_PI_HI = 6.28125
TWO_PI_LO = 2.0 * np.pi - 6.28125
GELU_C = 0.044715
GELU_S = 2.0 * 0.7978845608028654


def host_consts():
    ident = np.eye(128, dtype=np.float32)
    wide = np.zeros((128, 8, 240), np.float32)
    for a in range(8):
        for hc in range(16):
            wide[16 * a + hc, a, 112 + hc] = 1.0
    s_idx = np.arange(128) // 16
    maskF = (s_idx[None, :] >= s_idx[:, None]).astype(np.float32)
    maskB = (s_idx[:, None] >= s_idx[None, :]).astype(np.float32)
    maskF = maskF - maskB
    kvec = np.tile(np.array(KVEC, np.float32)[None, :], (128, 1))
    return dict(c_ident=ident, c_wide=np.ascontiguousarray(wide.reshape(128, 8 * 240)),
                c_maskF=maskF, c_maskB=maskB, c_kvec=kvec)


def host_s5_params(inp, l):
    f = np.float32

    def dp(a):
        return np.ascontiguousarray(np.transpose(a, (0, 2, 1)).reshape(128, 32)).astype(f)
    lam_re = dp(inp["ssm_lam_re"][l])
    lam_im = dp(inp["ssm_lam_im"][l])
    lstep = np.ascontiguousarray(np.broadcast_to(inp["ssm_log_step"][l][:, None, :], (2, 64, 32)).reshape(128, 32)).astype(f)

    def rep_b(a):
        t = np.transpose(a, (1, 0, 2))
        return np.ascontiguousarray(np.concatenate([t, t], 0).reshape(128, 32 * 16)).astype(f)

    def rep_c(a):
        t = np.transpose(a, (2, 0, 1))
        return np.ascontiguousarray(np.concatenate([t, t], 0).reshape(128, 32 * 16)).astype(f)
    dm = inp["ssm_d"][l].reshape(32, 16)
    dm = np.ascontiguousarray(np.tile(dm.T, (8, 1))).astype(f)
    return dict(lam_re=lam_re, lam_im=lam_im, lstep=lstep,
                b_re=rep_b(inp["ssm_b_re"][l]), b_im=rep_b(inp["ssm_b_im"][l]),
                c_re=rep_c(inp["ssm_c_re"][l]), c_im=rep_c(inp["ssm_c_im"][l]), dm=dm)


class Ctx:
    def __init__(self, P):
        self.P = P
        self.t = {}
        self.b = {}
        self.ps = []
        self.psb = []
        self.ps_rr = 0
        self.scopes = P.open_scopes
        self.uid = 0
        self.ps_lim = 8

    def sb(self, name, shape, dtype, nbuf=None):
        es = self.scopes[-1] if self.scopes else self.P.es
        self.uid += 1
        self.t[name] = es.enter_context(self.P.nc.sbuf_tensor("%s_%d" % (name, self.uid), list(shape), dtype))
        self.b[name] = Buf(name)
        return self.t[name]

    def push(self):
        self.scopes.append(ExitStack())

    def pop(self):
        self.P.barrier()
        self.scopes.pop().close()

    def alloc_psum(self):
        for i in range(8):
            self.ps.append(self.P.psum("psb%d" % i, [128, 512], F32))
            self.psb.append(Buf("psb%d" % i, excl=True))

    def next_ps(self):
        i = self.ps_rr
        self.ps_rr = (self.ps_rr + 1) % self.ps_lim
        return self.ps[i], self.psb[i]


def v_tt(P, eng, out, a, b, op, r, w):
    P.op(eng, lambda e: e.tensor_tensor(out=out, in0=a, in1=b, op=op), reads=r, writes=w)


def v_ts(P, eng, out, a, s1, s2, op0, op1, r, w):
    if s2 is None:
        P.op(eng, lambda e: e.tensor_scalar(out=out, in0=a, scalar1=s1, scalar2=None, op0=op0), reads=r, writes=w)
    else:
        P.op(eng, lambda e: e.tensor_scalar(out=out, in0=a, scalar1=s1, scalar2=s2, op0=op0, op1=op1), reads=r, writes=w)


def v_cp(P, eng, out, a, r, w):
    if eng == "act":
        P.op(eng, lambda e: e.copy(out=out, in_=a), reads=r, writes=w)
    else:
        P.op(eng, lambda e: e.tensor_copy(out=out, in_=a), reads=r, writes=w)


def v_act(P, out, a, func, r, w, bias=None, scale=None):
    kw = {}
    if bias is not None:
        kw["bias"] = bias
    if scale is not None:
        kw["scale"] = scale
    P.op("act", lambda e: e.activation(out=out, in_=a, func=func, **kw), reads=r, writes=w)


def mm(P, out, lhsT, rhs, start, stop, r, w):
    P.op("pe", lambda e: e.matmul(out, lhsT, rhs, start=start, stop=stop), reads=r, writes=w)


def cmul(P, eng, o_re, o_im, a_re, a_im, b_re, b_im, tmp):
    v_tt(P, eng, o_re[0], a_re[0], b_re[0], ALU.mult, [a_re[1], b_re[1]], [o_re[1]])
    v_tt(P, eng, tmp[0], a_im[0], b_im[0], ALU.mult, [a_im[1], b_im[1]], [tmp[1]])
    v_tt(P, eng, o_re[0], o_re[0], tmp[0], ALU.subtract, [o_re[1], tmp[1]], [o_re[1]])
    v_tt(P, eng, o_im[0], a_re[0], b_im[0], ALU.mult, [a_re[1], b_im[1]], [o_im[1]])
    v_tt(P, eng, tmp[0], a_im[0], b_re[0], ALU.mult, [a_im[1], b_re[1]], [tmp[1]])
    v_tt(P, eng, o_im[0], o_im[0], tmp[0], ALU.add, [o_im[1], tmp[1]], [o_im[1]])


def cmul_big(P, eng, o_re, o_im, a_re, a_im, b_re, b_im, tmp):
    for g0 in (0, 16):
        def sl(v):
            return (v[0][:, g0:g0 + 16], v[1])
        cmul(P, eng, sl(o_re), sl(o_im), sl(a_re), sl(a_im), sl(b_re), sl(b_im), (tmp[0], tmp[1]))


def alloc_s5_persist(C):
    C.sb("q_A2", [128, 2, 2, 32], F32)
    C.sb("q_P", [128, 3, 2, 32], F32)
    C.sb("w_Glre", [128, 32, 8, 16], BF16)
    C.sb("w_GlimN", [128, 32, 8, 16], BF16)
    C.sb("w_Msum", [128, 32, 128], BF16)
    C.sb("w_Wl", [128, 32, 2, 128], BF16)


def alloc_s5_prep(C):
    for n in ("p_lam_re", "p_lam_im", "p_lstep", "p_dm"):
        C.sb(n, [128, 32], F32)
    for n in ("p_b_re", "p_b_im", "p_c_re", "p_c_im"):
        C.sb(n, [128, 32, 16], F32)
    for n in ("q_step", "q_a", "q_b", "q_den", "q_em1", "q_t1", "q_t2", "q_bre", "q_bim"):
        C.sb(n, [128, 32], F32)
    for n in ("q_ka", "q_kb", "q_n", "q_r", "q_mag", "q_Ere", "q_Eim"):
        C.sb(n, [128, 32, NK], F32)
    for n in ("q_bBre", "q_bBim", "q_t3"):
        C.sb(n, [128, 32, 16], F32)
    for n in ("s_are", "s_aim", "s_bre", "s_bim", "s_cre", "s_cim"):
        C.sb(n, [128, 32, 8, 16], F32)
    C.sb("s_tmp", [128, 16, 8, 16], F32)
    C.sb("s_m1", [128, 128], F32)
    C.sb("s_m2", [128, 128], F32)


def emit_s5_prep(P, C, prm):
    T, B = C.t, C.b

    def V(n, ap=None):
        return (T[n][:] if ap is None else ap, B[n])
    for n in ("lam_re", "lam_im", "lstep", "dm"):
        P.dma("sp", T["p_" + n][:], prm[n], writes=[B["p_" + n]])
    for n in ("b_re", "b_im", "c_re", "c_im"):
        P.dma("sp", T["p_" + n][:].rearrange("p g h -> p (g h)"), prm[n], writes=[B["p_" + n]])
    e = "dve"
    v_act(P, T["q_step"][:], T["p_lstep"][:], AF.Exp, [B["p_lstep"]], [B["q_step"]])
    v_tt(P, e, T["q_a"][:], T["p_lam_re"][:], T["q_step"][:], ALU.mult, [B["p_lam_re"], B["q_step"]], [B["q_a"]])
    v_tt(P, e, T["q_b"][:], T["p_lam_im"][:], T["q_step"][:], ALU.mult, [B["p_lam_im"], B["q_step"]], [B["q_b"]])
    kv = T["c_kvec"][:].unsqueeze(1).broadcast_to([128, 32, NK])
    sh3 = [128, 32, NK]
    v_tt(P, e, T["q_ka"][:], T["q_a"][:].unsqueeze(2).broadcast_to(sh3), kv, ALU.mult, [B["q_a"], B["c_kvec"]], [B["q_ka"]])
    v_tt(P, e, T["q_kb"][:], T["q_b"][:].unsqueeze(2).broadcast_to(sh3), kv, ALU.mult, [B["q_b"], B["c_kvec"]], [B["q_kb"]])
    v_act(P, T["q_mag"][:], T["q_ka"][:], AF.Exp, [B["q_ka"]], [B["q_mag"]])
    inv2pi = float(1.0 / (2.0 * np.pi))
    for which, off, dst in (("sin", 0.0, "q_Eim"), ("cos", 0.25, "q_Ere")):
        v_ts(P, e, T["q_n"][:], T["q_kb"][:], inv2pi, off, ALU.mult, ALU.add, [B["q_kb"]], [B["q_n"]])
        v_ts(P, e, T["q_n"][:], T["q_n"][:], MAGIC, None, ALU.add, None, [B["q_n"]], [B["q_n"]])
        v_ts(P, e, T["q_n"][:], T["q_n"][:], -MAGIC, None, ALU.add, None, [B["q_n"]], [B["q_n"]])
        P.op(e, lambda en: en.scalar_tensor_tensor(out=T["q_r"][:], in0=T["q_n"][:], scalar=-TWO_PI_HI, in1=T["q_kb"][:],
                                                   op0=ALU.mult, op1=ALU.add), reads=[B["q_n"], B["q_kb"]], writes=[B["q_r"]])
        P.op(e, lambda en: en.scalar_tensor_tensor(out=T["q_r"][:], in0=T["q_n"][:], scalar=-TWO_PI_LO, in1=T["q_r"][:],
                                                   op0=ALU.mult, op1=ALU.add), reads=[B["q_n"], B["q_r"]], writes=[B["q_r"]])
        if off != 0.0:
            v_ts(P, e, T["q_r"][:], T["q_r"][:], float(np.pi / 2), None, ALU.add, None, [B["q_r"]], [B["q_r"]])
        v_ts(P, e, T["q_r"][:], T["q_r"][:], float(np.pi), float(-np.pi), ALU.min, ALU.max, [B["q_r"]], [B["q_r"]])
        v_act(P, T[dst][:], T["q_r"][:], AF.Sin, [B["q_r"]], [B[dst]])
    v_tt(P, e, T["q_Ere"][:], T["q_mag"][:], T["q_Ere"][:], ALU.mult, [B["q_mag"], B["q_Ere"]], [B["q_Ere"]])
    v_tt(P, e, T["q_Eim"][:], T["q_mag"][:], T["q_Eim"][:], ALU.mult, [B["q_mag"], B["q_Eim"]], [B["q_Eim"]])
    Ere, Eim = T["q_Ere"], T["q_Eim"]
    v_tt(P, e, T["q_den"][:], T["p_lam_re"][:], T["p_lam_re"][:], ALU.mult, [B["p_lam_re"]], [B["q_den"]])
    v_tt(P, e, T["q_t1"][:], T["p_lam_im"][:], T["p_lam_im"][:], ALU.mult, [B["p_lam_im"]], [B["q_t1"]])
    v_tt(P, e, T["q_den"][:], T["q_den"][:], T["q_t1"][:], ALU.add, [B["q_den"], B["q_t1"]], [B["q_den"]])
    P.op(e, lambda en: en.reciprocal(out=T["q_den"][:], in_=T["q_den"][:]), reads=[B["q_den"]], writes=[B["q_den"]])
    v_ts(P, e, T["q_em1"][:], Ere[:, :, 16], -1.0, None, ALU.add, None, [B["q_Ere"]], [B["q_em1"]])
    v_tt(P, e, T["q_bre"][:], T["q_em1"][:], T["p_lam_re"][:], ALU.mult, [B["q_em1"], B["p_lam_re"]], [B["q_bre"]])
    v_tt(P, e, T["q_t1"][:], Eim[:, :, 16], T["p_lam_im"][:], ALU.mult, [B["q_Eim"], B["p_lam_im"]], [B["q_t1"]])
    v_tt(P, e, T["q_bre"][:], T["q_bre"][:], T["q_t1"][:], ALU.add, [B["q_bre"], B["q_t1"]], [B["q_bre"]])
    v_tt(P, e, T["q_bim"][:], Eim[:, :, 16], T["p_lam_re"][:], ALU.mult, [B["q_Eim"], B["p_lam_re"]], [B["q_bim"]])
    v_tt(P, e, T["q_t2"][:], T["q_em1"][:], T["p_lam_im"][:], ALU.mult, [B["q_em1"], B["p_lam_im"]], [B["q_t2"]])
    v_tt(P, e, T["q_bim"][:], T["q_bim"][:], T["q_t2"][:], ALU.subtract, [B["q_bim"], B["q_t2"]], [B["q_bim"]])
    v_tt(P, e, T["q_bre"][:], T["q_bre"][:], T["q_den"][:], ALU.mult, [B["q_bre"], B["q_den"]], [B["q_bre"]])
    v_tt(P, e, T["q_bim"][:], T["q_bim"][:], T["q_den"][:], ALU.mult, [B["q_bim"], B["q_den"]], [B["q_bim"]])
    s16 = [128, 32, 16]
    bre_b = (T["q_bre"][:].unsqueeze(2).broadcast_to(s16), B["q_bre"])
    bim_b = (T["q_bim"][:].unsqueeze(2).broadcast_to(s16), B["q_bim"])
    cmul(P, e, V("q_bBre"), V("q_bBim"), bre_b, bim_b, V("p_b_re"), V("p_b_im"), V("q_t3"))
    A2 = T["q_A2"]
    v_cp(P, e, A2[:, 0, 0, :], Ere[:, :, 18], [B["q_Ere"]], [B["q_A2"]])
    v_cp(P, e, A2[:, 0, 1, :], Ere[:, :, 18], [B["q_Ere"]], [B["q_A2"]])
    v_ts(P, e, A2[:, 1, 0, :], Eim[:, :, 18], -1.0, None, ALU.mult, None, [B["q_Eim"]], [B["q_A2"]])
    v_cp(P, e, A2[:, 1, 1, :], Eim[:, :, 18], [B["q_Eim"]], [B["q_A2"]])
    Pm = T["q_P"]
    v_cp(P, e, Pm[:, 0, 0, :], Ere[:, :, 18], [B["q_Ere"]], [B["q_P"]])
    v_cp(P, e, Pm[:, 0, 1, :], Eim[:, :, 18], [B["q_Eim"]], [B["q_P"]])

    def csq(dst_re, dst_im, a_re, a_im, b_re, b_im):
        v_tt(P, e, T["q_t1"][:], a_re, b_re, ALU.mult, [B["q_P"]], [B["q_t1"]])
        v_tt(P, e, T["q_t2"][:], a_im, b_im, ALU.mult, [B["q_P"]], [B["q_t2"]])
        v_tt(P, e, T["q_t1"][:], T["q_t1"][:], T["q_t2"][:], ALU.subtract, [B["q_t1"], B["q_t2"]], [B["q_t1"]])
        v_tt(P, e, T["q_t2"][:], a_re, b_im, ALU.mult, [B["q_P"]], [B["q_t2"]])
        v_tt(P, e, T["q_den"][:], a_im, b_re, ALU.mult, [B["q_P"]], [B["q_den"]])
        v_tt(P, e, dst_im, T["q_t2"][:], T["q_den"][:], ALU.add, [B["q_t2"], B["q_den"]], [B["q_P"]])
        v_cp(P, e, dst_re, T["q_t1"][:], [B["q_t1"]], [B["q_P"]])
    for _ in range(8):
        csq(Pm[:, 0, 0, :], Pm[:, 0, 1, :], Pm[:, 0, 0, :], Pm[:, 0, 1, :], Pm[:, 0, 0, :], Pm[:, 0, 1, :])
    csq(Pm[:, 1, 0, :], Pm[:, 1, 1, :], Pm[:, 0, 0, :], Pm[:, 0, 1, :], Pm[:, 0, 0, :], Pm[:, 0, 1, :])
    csq(Pm[:, 2, 0, :], Pm[:, 2, 1, :], Pm[:, 1, 0, :], Pm[:, 1, 1, :], Pm[:, 0, 0, :], Pm[:, 0, 1, :])
    s4 = [128, 32, 8, 16]

    def Ek(lo, hi):
        return ((Ere[:, :, lo:hi].unsqueeze(3).broadcast_to(s4), B["q_Ere"]),
                (Eim[:, :, lo:hi].unsqueeze(3).broadcast_to(s4), B["q_Eim"]))

    def E1(idx):
        return ((Ere[:, :, idx:idx + 1].unsqueeze(3).broadcast_to(s4), B["q_Ere"]),
                (Eim[:, :, idx:idx + 1].unsqueeze(3).broadcast_to(s4), B["q_Eim"]))
    bB_re = (T["q_bBre"][:].unsqueeze(2).broadcast_to(s4), B["q_bBre"])
    bB_im = (T["q_bBim"][:].unsqueeze(2).broadcast_to(s4), B["q_bBim"])
    def ctab(o_re_n, o_im_n, a_re, a_im, b_re, b_im, rev_bwd):
        for lo, hi, rev in ((0, 64, False), (64, 128, rev_bwd)):
            o_re = (T[o_re_n][lo:hi, :, ::-1, :] if rev else T[o_re_n][lo:hi], B[o_re_n])
            o_im = (T[o_im_n][lo:hi, :, ::-1, :] if rev else T[o_im_n][lo:hi], B[o_im_n])
            cmul_big(P, e, o_re, o_im, (a_re[0][lo:hi], a_re[1]), (a_im[0][lo:hi], a_im[1]),
                     (b_re[0][lo:hi], b_re[1]), (b_im[0][lo:hi], b_im[1]), (T["s_tmp"][lo:hi], B["s_tmp"]))
    er, ei = Ek(0, 8)
    ctab("s_are", "s_aim", er, ei, bB_re, bB_im, True)
    c_re = (T["p_c_re"][:].unsqueeze(2).broadcast_to(s4), B["p_c_re"])
    c_im = (T["p_c_im"][:].unsqueeze(2).broadcast_to(s4), B["p_c_im"])
    er, ei = Ek(8, 16)
    ctab("s_bre", "s_bim", er, ei, c_re, c_im, True)
    e1r, e1i = E1(16)
    ctab("s_cre", "s_cim", e1r, e1i, V("s_bre"), V("s_bim"), False)
    v_cp(P, "act", T["w_Glre"][:], T["s_cre"][:], [B["s_cre"]], [B["w_Glre"]])
    v_ts(P, e, T["w_GlimN"][:], T["s_cim"][:], -1.0, None, ALU.mult, None, [B["s_cim"]], [B["w_GlimN"]])
    e7r, e7i = E1(17)
    ctab("s_cre", "s_cim", e7r, e7i, V("s_are"), V("s_aim"), False)
    v_ts(P, e, T["s_bim"][:], T["s_bim"][:], -1.0, None, ALU.mult, None, [B["s_bim"]], [B["s_bim"]])
    idf = T["c_ident"]

    def fl(ap):
        return ap.rearrange("p s h -> p (s h)")
    for g in range(32):
        psA, pbA = C.next_ps()
        mm(P, psA[:, 0:128], fl(T["s_are"][0:64, g]), fl(T["s_bre"][0:64, g]), True, False, [B["s_are"], B["s_bre"]], [pbA])
        mm(P, psA[:, 0:128], fl(T["s_aim"][0:64, g]), fl(T["s_bim"][0:64, g]), False, True, [B["s_aim"], B["s_bim"]], [pbA])
        mm(P, psA[:, 128:256], fl(T["s_are"][:, g]), fl(T["s_bre"][:, g]), True, False, [B["s_are"], B["s_bre"]], [pbA])
        mm(P, psA[:, 128:256], fl(T["s_aim"][:, g]), fl(T["s_bim"][:, g]), False, True, [B["s_aim"], B["s_bim"]], [pbA])
        for ri, tn in ((0, "s_cre"), (1, "s_cim")):
            o = 256 + ri * 128
            mm(P, psA[:, o:o + 128], fl(T[tn][:, g]), idf[:, :], True, True, [B[tn], B["c_ident"]], [pbA])
        v_tt(P, e, T["s_m1"][:], psA[:, 0:128], T["c_maskF"][:], ALU.mult, [pbA, B["c_maskF"]], [B["s_m1"]])
        v_tt(P, e, T["s_m2"][:], psA[:, 128:256], T["c_maskB"][:], ALU.mult, [pbA, B["c_maskB"]], [B["s_m2"]])
        v_tt(P, "pool", T["s_m1"][:], T["s_m1"][:], T["s_m2"][:], ALU.add, [B["s_m1"], B["s_m2"]], [B["s_m1"]])
        P.op("dve", lambda en, g=g: en.scalar_tensor_tensor(out=T["w_Msum"][:, g, :], in0=T["c_ident"][:], scalar=T["p_dm"][:, g:g + 1],
                                                             in1=T["s_m1"][:], op0=ALU.mult, op1=ALU.add),
             reads=[B["c_ident"], B["p_dm"], B["s_m1"]], writes=[B["w_Msum"]])
        v_cp(P, "act", T["w_Wl"][:, g].rearrange("p r m -> p (r m)"), psA[:, 256:512], [pbA], [B["w_Wl"]])


def alloc_s5_main(C):
    C.sb("a_U", [128, 4, NT], BF16)
    C.sb("a_X8", [128, 32, NCH], BF16)
    C.sb("a_Hs", [128, NCH + 1 + SCAN_L, 2, 32], F32)
    C.sb("a_I", [128, SCAN_K + 1, 2, 32], F32)
    C.sb("a_P32", [128, 2, 2, 32], F32)
    C.sb("a_st", [128, 2, 2, 32], F32)
    C.sb("a_st9", [128, 2, SCAN_K + 1, 2, 32], F32)
    C.sb("a_st1", [128, 2, 2, 32], F32)
    C.bX8 = [Buf("x8_%d" % g) for g in range(32)]


def emit_s5_scan(P, C):
    T, B = C.t, C.b
    Hs, A2, st, I, P32 = T["a_Hs"], T["q_A2"], T["a_st9"], T["a_I"], T["a_P32"]
    K, L = SCAN_K, SCAN_L
    V = Hs[:, 1:1 + (K + 1) * L].rearrange("p (b i) r g -> p b i r g", b=K + 1)
    hb = [B["a_Hs"]]
    s9 = [128, K + 1, 2, 32]
    Are = A2[:, 0].unsqueeze(1).broadcast_to(s9)
    Aim = A2[:, 1].unsqueeze(1).broadcast_to(s9)
    P.op("dve", lambda e: e.memset(Hs[:, 1 + K * L:1 + (K + 1) * L], 0.0), writes=hb)
    v_cp(P, "dve", V[:, K, 0, 0, :], A2[:, 0, 0, :], [B["q_A2"]], hb)
    v_cp(P, "dve", V[:, K, 0, 1, :], A2[:, 1, 1, :], [B["q_A2"]], hb)
    for i in range(1, L):
        v_tt(P, "dve", st[:, 0], V[:, :, i - 1], Are, ALU.mult, hb + [B["q_A2"]], [B["a_st9"]])
        v_tt(P, "dve", st[:, 1], V[:, :, i - 1, ::-1, :], Aim, ALU.mult, hb + [B["q_A2"]], [C.b_st2])
        v_tt(P, "dve", V[:, :, i], V[:, :, i], st[:, 0], ALU.add, hb + [B["a_st9"]], hb)
        v_tt(P, "dve", V[:, :, i], V[:, :, i], st[:, 1], ALU.add, hb + [C.b_st2], hb)
    v_cp(P, "dve", P32[:, 0, 0, :], V[:, K, L - 1, 0, :], hb, [B["a_P32"]])
    v_cp(P, "dve", P32[:, 0, 1, :], V[:, K, L - 1, 0, :], hb, [B["a_P32"]])
    v_ts(P, "dve", P32[:, 1, 0, :], V[:, K, L - 1, 1, :], -1.0, None, ALU.mult, None, hb, [B["a_P32"]])
    v_cp(P, "dve", P32[:, 1, 1, :], V[:, K, L - 1, 1, :], hb, [B["a_P32"]])
    ib = [B["a_I"]]
    s1 = T["a_st1"]
    v_cp(P, "dve", I[:, 0], Hs[:, 0], hb, ib)
    for b_ in range(K):
        v_tt(P, "dve", s1[:, 0], I[:, b_], P32[:, 0], ALU.mult, ib + [B["a_P32"]], [B["a_st1"]])
        v_tt(P, "dve", s1[:, 1], I[:, b_, ::-1, :], P32[:, 1], ALU.mult, ib + [B["a_P32"]], [B["a_st1"]])
        v_tt(P, "dve", I[:, b_ + 1], V[:, b_, L - 1], s1[:, 0], ALU.add, hb + [B["a_st1"]], ib)
        v_tt(P, "dve", I[:, b_ + 1], I[:, b_ + 1], s1[:, 1], ALU.add, ib + [B["a_st1"]], ib)
    sF = [128, K, L, 32]
    Tm = T["a_T"]
    Pw = V[:, K]

    def pw(r):
        return Pw[:, :, r, :].unsqueeze(1).broadcast_to(sF)

    def ii(r):
        return I[:, 0:K, r, :].unsqueeze(2).broadcast_to(sF)
    for k, (pr_, ir_, out_r, op) in enumerate(((0, 0, 0, ALU.add), (1, 1, 0, ALU.subtract), (0, 1, 1, ALU.add), (1, 0, 1, ALU.add))):
        tm, tb_ = Tm[:], C.b_T[0]
        v_tt(P, "pool", tm, pw(pr_), ii(ir_), ALU.mult, hb + ib, [tb_])
        v_tt(P, "dve", V[:, 0:K, :, out_r, :], V[:, 0:K, :, out_r, :], tm, op, hb + [tb_], [C.b_Hfix[out_r]])
    P.op("dve", lambda e: e.tensor_copy(out=I[:, 0, 0, 0:1], in_=I[:, 0, 0, 0:1]), reads=[C.b_Hfix[0], C.b_Hfix[1]] + ib, writes=hb + ib)


def emit_s5_front(P, C, xb, xb_buf, win, win_buf):
    T, B = C.t, C.b
    U, X8, Hs = T["a_U"], T["a_X8"], T["a_Hs"]
    C.b_st2 = Buf("st2")
    for tb in range(NTB):
        for m in range(4):
            ps, pb = C.next_ps()
            for kd in range(8):
                mm(P, ps[:], win[:, kd, m * 128:(m + 1) * 128], xb[:, kd, tb * TB:(tb + 1) * TB], kd == 0, kd == 7,
                   [win_buf, xb_buf], [pb])
            v_cp(P, "act" if (m % 2) else "dve", U[:, m, tb * TB:(tb + 1) * TB], ps[:], [pb], [B["a_U"]])


def emit_s5_mid(P, C, xs_src=None, xs_dst=None):
    T, B = C.t, C.b
    U, X8, Hs = T["a_U"], T["a_X8"], T["a_Hs"]
    if xs_src is not None:
        P.dma("pool", X8[:].rearrange("p g c -> p (g c)"), xs_src["X8"], writes=list(C.bX8))
        P.dma("sp", Hs[:, 1:NCH + 1].rearrange("p c r g -> p (c r g)"), xs_src["S"], writes=[B["a_Hs"]])
        C.push()
        C.sb("a_T", [128, SCAN_K, SCAN_L, 32], F32)
        C.b_T = [Buf("aT0")]
        C.b_Hfix = [Buf("hfix0"), Buf("hfix1")]
        emit_s5_scan(P, C)
        C.pop()
        return
    C.push()
    C.sb("a_T", [128, SCAN_K, SCAN_L, 32], F32)
    C.b_T = [Buf("aT0")]
    C.b_Hfix = [Buf("hfix0"), Buf("hfix1")]
    wide = T["c_wide_bf"]
    for g2 in range(16):
        ps, pb = C.next_ps()
        for gg in range(2):
            g = 2 * g2 + gg
            gl, m = g % 8, g // 8
            Uv = U[:, m, :].rearrange("p (c s) -> p s c", s=8)
            for s in range(8):
                mm(P, ps[:, gg * 256:(gg + 1) * 256], wide[:, gl, 112 - 16 * s:112 - 16 * s + 128], Uv[:, s, :], s == 0, s == 7,
                   [B["c_wide_bf"], B["a_U"]], [pb])
        v_cp(P, "act" if (g2 % 2) else "dve", X8[:, 2 * g2:2 * g2 + 2, :].rearrange("p g c -> p (g c)"), ps[:], [pb],
             [C.bX8[2 * g2], C.bX8[2 * g2 + 1]])
    Wl = T["w_Wl"]
    for g in range(32):
        ps, pb = C.next_ps()
        for ri in range(2):
            mm(P, ps[:, ri * 256:(ri + 1) * 256], Wl[:, g, ri, :], X8[:, g, :], True, True, [B["w_Wl"], C.bX8[g]], [pb])
        psv = ps[:].rearrange("p (r c) -> p c r", r=2)
        v_cp(P, "act", Hs[0:64, 1:NCH + 1, :, g], psv[0:64], [pb], [B["a_Hs"]])
        v_cp(P, "dve", Hs[64:128, 1:NCH + 1, :, g], psv[64:128, ::-1, :], [pb], [B["a_Hs"]])
    if xs_dst is not None:
        P.dma("pool", xs_dst["X8"], X8[:].rearrange("p g c -> p (g c)"), reads=list(C.bX8), writes=[Buf("tab_X8")])
        P.dma("sp", xs_dst["S"], Hs[:, 1:NCH + 1].rearrange("p c r g -> p (c r g)"), reads=[B["a_Hs"]], writes=[Buf("tab_S")])
    emit_s5_scan(P, C)
    C.pop()


import os as _os
S5_STOP = int(_os.environ.get("S5_STOP", "0"))


def emit_s5_back(P, C, wglu, wglu_buf, bglu, bglu_buf):
    T, B = C.t, C.b
    U, X8, Hs = T["a_U"], T["a_X8"], T["a_Hs"]
    HsB = C.sb("a_HsB", [128, NCH + 1, 2, 32], BF16)
    v_cp(P, "act", HsB[0:64, 0:NCH], Hs[0:64, 0:NCH], [B["a_Hs"]], [B["a_HsB"]])
    v_cp(P, "dve", HsB[64:128, 0:NCH], Hs[64:128, NCH - 1::-1], [B["a_Hs"]], [B["a_HsB"]])
    for g2 in range(16):
        ps, pb = C.next_ps()
        for gg in range(2):
            g = 2 * g2 + gg
            o = ps[:, gg * 256:(gg + 1) * 256]
            mm(P, o, T["w_Msum"][:, g, :], X8[:, g, :], True, False, [B["w_Msum"], C.bX8[g]], [pb])
            for ri, tn in ((0, "w_Glre"), (1, "w_GlimN")):
                mm(P, o, T[tn][:, g].rearrange("p j h -> p (j h)"), HsB[:, 0:NCH, ri, g], False, ri == 1, [B[tn], B["a_HsB"]], [pb])
        v_cp(P, "act" if (g2 % 2) else "dve", X8[:, 2 * g2:2 * g2 + 2, :].rearrange("p g c -> p (g c)"), ps[:], [pb],
             [C.bX8[2 * g2], C.bX8[2 * g2 + 1]])
    if S5_STOP == 3:
        return
    wide = T["c_wide_bf"]
    for m in range(4):
        Uv = U[:, m, :].rearrange("p (c s) -> p s c", s=8)
        for j2 in range(4):
            ps, pb = C.next_ps()
            for jj in range(2):
                j = 2 * j2 + jj
                for gl in range(8):
                    mm(P, ps[:, jj * 256:(jj + 1) * 256], wide[:, j, 112 - 16 * gl:112 - 16 * gl + 128], X8[:, m * 8 + gl, :], gl == 0, gl == 7,
                       [B["c_wide_bf"], C.bX8[m * 8 + gl]], [pb])
            emit_gelu(P, C, Uv[:, 2 * j2:2 * j2 + 2, :], ps[:].rearrange("p (j c) -> p j c", j=2), pb, [B["a_U"]], [128, 2, 256])
    if S5_STOP == 4:
        return
    for tb in range(NTB):
        sl = slice(tb * TB, (tb + 1) * TB)
        pss = []
        for m in range(4):
            ps, pb = C.next_ps()
            for k in range(4):
                mm(P, ps[:], wglu[:, k, m * 128:(m + 1) * 128], U[:, k, sl], k == 0, k == 3, [wglu_buf, B["a_U"]], [pb])
            pss.append((ps, pb))
        for m in range(4):
            ps, pb = pss[m]
            v_act(P, T["g_gate"][:, m, :], ps[:], AF.Sigmoid, [pb, bglu_buf], [B["g_gate"]], bias=bglu[:, m:m + 1])
        for m in range(4):
            v_tt(P, "dve", U[:, m, sl], U[:, m, sl], T["g_gate"][:, m, :], ALU.mult, [B["a_U"], B["g_gate"]], [B["a_U"]])


def emit_gelu(P, C, out, src, src_buf, out_bufs, shape):
    T, B = C.t, C.b
    n = 1
    for d_ in shape[1:]:
        n *= d_
    x = T["g_x"][:, 0:n]
    t = T["g_t"][:, 0:n]
    if len(shape) == 3:
        x = x.rearrange("p (a b) -> p a b", a=shape[1])
        t = t.rearrange("p (a b) -> p a b", a=shape[1])
    v_cp(P, "act", x, src, [src_buf], [B["g_x"]])
    v_tt(P, "pool", t, x, x, ALU.mult, [B["g_x"]], [B["g_t"]])
    v_ts(P, "pool", t, t, GELU_C * GELU_S, GELU_S, ALU.mult, ALU.add, [B["g_t"]], [B["g_t"]])
    v_tt(P, "pool", t, t, x, ALU.mult, [B["g_t"], B["g_x"]], [B["g_t"]])
    v_act(P, t, t, AF.Sigmoid, [B["g_t"]], [B["g_t"]])
    v_tt(P, "dve", out, x, t, ALU.mult, [B["g_x"], B["g_t"]], out_bufs)


def alloc_gelu(C):
    C.sb("g_x", [128, 512], F32)
    C.sb("g_t", [128, 512], F32)
    C.sb("g_gate", [128, 4, 512], BF16)


def load_consts(P, C, cst):
    T, B = C.t, C.b
    C.sb("c_ident", [128, 128], F32)
    C.sb("c_ident_bf", [128, 128], BF16)
    C.sb("c_wide_bf", [128, 8, 240], BF16)
    C.sb("c_maskF", [128, 128], F32)
    C.sb("c_maskB", [128, 128], F32)
    C.sb("c_kvec", [128, NK], F32)
    P.dma("sp", T["c_ident"][:], cst["c_ident"], writes=[B["c_ident"]])
    P.dma("pool", T["c_ident_bf"][:], cst["c_ident"], writes=[B["c_ident_bf"]])
    P.dma("pool", T["c_wide_bf"][:].rearrange("p a w -> p (a w)"), cst["c_wide"], writes=[B["c_wide_bf"]])
    P.dma("sp", T["c_maskF"][:], cst["c_maskF"], writes=[B["c_maskF"]])
    P.dma("sp", T["c_maskB"][:], cst["c_maskB"], writes=[B["c_maskB"]])
    P.dma("sp", T["c_kvec"][:], cst["c_kvec"], writes=[B["c_kvec"]])


def load_w(P, C, name, shape, src_ap, dtype=BF16, eng=None):
    t = C.sb(name, shape, dtype)
    if eng is None:
        eng = "pool" if dtype != F32 else "sp"
    dst = t[:]
    P.dma(eng, dst, src_ap, writes=[C.b[name]])
    return t


def kview(ap2d, ncols=None):
    return ap2d.rearrange("(k p) c -> p k c", p=128)


def emit_rstd(P, C, var_ap, var_buf):
    v_ts(P, "dve", var_ap, var_ap, LN_EPS, None, ALU.add, None, [var_buf], [var_buf])
    v_act(P, var_ap, var_ap, AF.Sqrt, [var_buf], [var_buf])
    P.op("dve", lambda e: e.reciprocal(out=var_ap, in_=var_ap), reads=[var_buf], writes=[var_buf])


def emit_gmlp(P, C, xb, W, ygm_out, ygm_buf):
    T, B = C.t, C.b
    wuv = load_w(P, C, "wuv", [128, 8, 1024], kview(W["w_in"])[:, :, 512:1536])
    wsT = load_w(P, C, "wsT", [128, 8, 128], W["wsT"].rearrange("p (g i) -> p g i", g=8))
    lng = load_w(P, C, "lng", [128, 512], W["lng"], F32)
    lnb = load_w(P, C, "lnb", [128, 512], W["lnb"], F32)
    bsT = load_w(P, C, "bsT", [128, 8], W["bsT"], F32)
    C.sb("m_u", [128, 512], BF16)
    C.sb("m_v", [128, 512], F32)
    C.sb("m_vb", [128, 512], BF16)
    C.sb("m_sq", [128, 512], F32)
    C.sb("m_st", [128, 4], F32)
    C.sb("m_t", [128, 512], F32)
    C.sb("m_y", [128, 512], BF16)
    C.sb("m_yf", [128, 4, 128], BF16)
    idb = T["c_ident_bf"]
    for tt in range(NT // 128):
        tok = slice(tt * 128, (tt + 1) * 128)
        psu, pbu = C.next_ps()
        psv, pbv = C.next_ps()
        for kd in range(8):
            mm(P, psu[:], xb[:, kd, tok], wuv[:, kd, 0:512], kd == 0, kd == 7, [B["xb"], B["wuv"]], [pbu])
        for kd in range(8):
            mm(P, psv[:], xb[:, kd, tok], wuv[:, kd, 512:1024], kd == 0, kd == 7, [B["xb"], B["wuv"]], [pbv])
        emit_gelu(P, C, T["m_u"][:], psu[:], pbu, [B["m_u"]], [128, 512])
        emit_gelu(P, C, T["m_v"][:], psv[:], pbv, [B["m_v"]], [128, 512])
        st = T["m_st"]
        P.op("dve", lambda e: e.reduce_sum(out=st[:, 0:1], in_=T["m_v"][:], axis=AX.X), reads=[B["m_v"]], writes=[B["m_st"]])
        v_tt(P, "pool", T["m_sq"][:], T["m_v"][:], T["m_v"][:], ALU.mult, [B["m_v"]], [B["m_sq"]])
        P.op("dve", lambda e: e.reduce_sum(out=st[:, 1:2], in_=T["m_sq"][:], axis=AX.X), reads=[B["m_sq"]], writes=[B["m_st"]])
        v_ts(P, "dve", st[:, 0:2], st[:, 0:2], 1.0 / 512.0, None, ALU.mult, None, [B["m_st"]], [B["m_st"]])
        v_tt(P, "dve", st[:, 2:3], st[:, 0:1], st[:, 0:1], ALU.mult, [B["m_st"]], [B["m_st"]])
        v_tt(P, "dve", st[:, 1:2], st[:, 1:2], st[:, 2:3], ALU.subtract, [B["m_st"]], [B["m_st"]])
        emit_rstd(P, C, st[:, 1:2], B["m_st"])
        P.op("dve", lambda e: e.tensor_scalar(out=T["m_t"][:], in0=T["m_v"][:], scalar1=st[:, 0:1], scalar2=st[:, 1:2],
                                              op0=ALU.subtract, op1=ALU.mult), reads=[B["m_v"], B["m_st"]], writes=[B["m_t"]])
        v_tt(P, "pool", T["m_t"][:], T["m_t"][:], lng[:], ALU.mult, [B["m_t"], B["lng"]], [B["m_t"]])
        v_tt(P, "dve", T["m_vb"][:], T["m_t"][:], lnb[:], ALU.add, [B["m_t"], B["lnb"]], [B["m_vb"]])
        pss, pbs = C.next_ps()
        for g in range(8):
            mm(P, pss[:, g * 64:(g + 1) * 64], wsT[:, g, :], T["m_vb"][:, g * 64:(g + 1) * 64], True, True, [B["wsT"], B["m_vb"]], [pbs])
        v_tt(P, "dve", T["m_t"][:].rearrange("p (g d) -> p g d", g=8), pss[:].rearrange("p (g d) -> p g d", g=8),
             bsT[:].unsqueeze(2).broadcast_to([128, 8, 64]), ALU.add, [pbs, B["bsT"]], [B["m_t"]])
        v_tt(P, "dve", T["m_y"][:], T["m_t"][:], T["m_u"][:], ALU.mult, [B["m_t"], B["m_u"]], [B["m_y"]])
        pst, pbt = C.next_ps()
        for m in range(4):
            mm(P, pst[:, m * 128:(m + 1) * 128], T["m_y"][:, m * 128:(m + 1) * 128], idb[:], True, True, [B["m_y"], B["c_ident_bf"]], [pbt])
        v_cp(P, "act", T["m_yf"][:].rearrange("p m t -> p (m t)"), pst[:], [pbt], [B["m_yf"]])
        P.dma("sp", ygm_out.rearrange("(m p) t -> p m t", p=128)[:, :, tok], T["m_yf"][:], reads=[B["m_yf"]], writes=[ygm_buf])


def emit_memattn(P, C, xb, W, memT, ymem_out, ymem_buf):
    T, B = C.t, C.b
    wq = load_w(P, C, "wq", [128, 8, 512], kview(W["w_in"])[:, :, 1536:2048])
    wkv = load_w(P, C, "wkv", [128, 8, 1024], kview(W["w_kv"]))
    mT = load_w(P, C, "mT", [128, 8, 256], kview(memT))
    C.sb("k_kT", [128, 4, 256], BF16)
    C.sb("k_v", [128, 2, 512], BF16)
    C.sb("k_q", [128, 4, NT], BF16)
    C.sb("k_e", [128, 2, 512], BF16)
    C.sb("k_rs", [128, 512], F32)
    C.sb("k_o", [128, 4, 512], BF16)
    C.sb("k_ones", [128, 128], BF16)
    P.op("dve", lambda e: e.memset(T["k_ones"][:], 1.0), writes=[B["k_ones"]])
    for h in range(4):
        ps, pb = C.next_ps()
        for kd in range(8):
            mm(P, ps[:, 0:256], wkv[:, kd, h * 128:(h + 1) * 128], mT[:, kd, :], kd == 0, kd == 7, [B["wkv"], B["mT"]], [pb])
        v_cp(P, "act", T["k_kT"][:, h, :], ps[:, 0:256], [pb], [B["k_kT"]])
    for mt in range(2):
        ps, pb = C.next_ps()
        for kd in range(8):
            mm(P, ps[:], mT[:, kd, mt * 128:(mt + 1) * 128], wkv[:, kd, 512:1024], kd == 0, kd == 7, [B["mT"], B["wkv"]], [pb])
        v_cp(P, "dve", T["k_v"][:, mt, :], ps[:], [pb], [B["k_v"]])
    for tb in range(NTB):
        sl = slice(tb * TB, (tb + 1) * TB)
        for h in range(4):
            ps, pb = C.next_ps()
            for kd in range(8):
                mm(P, ps[:], wq[:, kd, h * 128:(h + 1) * 128], xb[:, kd, sl], kd == 0, kd == 7, [B["wq"], B["xb"]], [pb])
            v_cp(P, "act" if h % 2 else "dve", T["k_q"][:, h, sl], ps[:], [pb], [B["k_q"]])
    scale = float(128.0 ** -0.5)
    for tb in range(NTB):
        sl = slice(tb * TB, (tb + 1) * TB)
        for h in range(4):
            for mt in range(2):
                ps, pb = C.next_ps()
                mm(P, ps[:], T["k_kT"][:, h, mt * 128:(mt + 1) * 128], T["k_q"][:, h, sl], True, True, [B["k_kT"], B["k_q"]], [pb])
                v_act(P, T["k_e"][:, mt, :], ps[:], AF.Exp, [pb], [B["k_e"]], scale=scale)
            pss, pbs = C.next_ps()
            pso, pbo = C.next_ps()
            for mt in range(2):
                mm(P, pss[:], T["k_ones"][:], T["k_e"][:, mt, :], mt == 0, mt == 1, [B["k_ones"], B["k_e"]], [pbs])
            for mt in range(2):
                mm(P, pso[:], T["k_v"][:, mt, h * 128:(h + 1) * 128], T["k_e"][:, mt, :], mt == 0, mt == 1, [B["k_v"], B["k_e"]], [pbo])
            P.op("dve", lambda e, pss=pss: e.reciprocal(out=T["k_rs"][:], in_=pss[:]), reads=[pbs], writes=[B["k_rs"]])
            v_tt(P, "dve", T["k_o"][:, h, :], pso[:], T["k_rs"][:], ALU.mult, [pbo, B["k_rs"]], [B["k_o"]])
        P.dma("sp", ymem_out.rearrange("(m p) t -> p m t", p=128)[:, :, sl], T["k_o"][:], reads=[B["k_o"]], writes=[ymem_buf])


def emit_ln_block(P, C, z, zbuf, nblk, g_t, b_t, pref, sink):
    T, B = C.t, C.b
    ps1, pb1 = C.next_ps()
    ps2, pb2 = C.next_ps()
    for m in range(8):
        mm(P, ps1[:, 0:nblk], T["k_onesf"][:], z[:, m, 0:nblk], m == 0, m == 7, [B["k_onesf"], zbuf], [pb1])
    for m in range(8):
        sq, sqb = T[pref + "sq%d" % (m % 2)], B[pref + "sq%d" % (m % 2)]
        v_tt(P, "pool", sq[:, 0:nblk], z[:, m, 0:nblk], z[:, m, 0:nblk], ALU.mult, [zbuf], [sqb])
        mm(P, ps2[:, 0:nblk], T["k_onesf"][:], sq[:, 0:nblk], m == 0, m == 7, [B["k_onesf"], sqb], [pb2])
    mean, var = T[pref + "mean"], T[pref + "var"]
    v_ts(P, "dve", mean[:, 0:nblk], ps1[:, 0:nblk], 1.0 / D, None, ALU.mult, None, [pb1], [B[pref + "mean"]])
    v_ts(P, "dve", var[:, 0:nblk], ps2[:, 0:nblk], 1.0 / D, None, ALU.mult, None, [pb2], [B[pref + "var"]])
    sq, sqb = T[pref + "sq0"], B[pref + "sq0"]
    v_tt(P, "pool", sq[:, 0:nblk], mean[:, 0:nblk], mean[:, 0:nblk], ALU.mult, [B[pref + "mean"]], [sqb])
    v_tt(P, "dve", var[:, 0:nblk], var[:, 0:nblk], sq[:, 0:nblk], ALU.subtract, [B[pref + "var"], sqb], [B[pref + "var"]])
    emit_rstd(P, C, var[:, 0:nblk], B[pref + "var"])
    for m in range(8):
        t, tb_ = T[pref + "sq%d" % (m % 2)], B[pref + "sq%d" % (m % 2)]
        o, ob = T[pref + "o%d" % (m % 2)], B[pref + "o%d" % (m % 2)]
        v_tt(P, "dve", t[:, 0:nblk], z[:, m, 0:nblk], mean[:, 0:nblk], ALU.subtract, [zbuf, B[pref + "mean"]], [tb_])
        v_tt(P, "dve", t[:, 0:nblk], t[:, 0:nblk], var[:, 0:nblk], ALU.mult, [tb_, B[pref + "var"]], [tb_])
        v_act(P, o[:, 0:nblk], t[:, 0:nblk], AF.Identity, [tb_, B[g_t[1]], B[b_t[1]]], [ob],
              bias=b_t[0][:, m:m + 1], scale=g_t[0][:, m:m + 1])
        sink(m, o[:, 0:nblk], ob)


def alloc_ln(C, pref, nblk):
    for n in ("sq0", "sq1", "o0", "o1", "mean", "var"):
        C.sb(pref + n, [128, nblk], F32)
    if "k_onesf" not in C.t or True:
        C.sb("k_onesf", [128, 128], F32)
        C.P.op("dve", lambda e: e.memset(C.t["k_onesf"][:], 1.0), writes=[C.b["k_onesf"]])


def emit_merge(P, C, xb, W, xT_in, ybr, ybr_bufs, x1_out, x1_buf):
    T, B = C.t, C.b
    wbr = [load_w(P, C, "wbr%d" % i, [128, 4, 1024], kview(W["w_br"][i])) for i in range(3)]
    wout = load_w(P, C, "wout", [128, 8, 1024], kview(W["w_out"]))
    bg = load_w(P, C, "bg", [128, 24], W["bgT"], F32)
    g1 = load_w(P, C, "ln1g", [128, 8], W["ln1gT"], F32)
    b1 = load_w(P, C, "ln1b", [128, 8], W["ln1bT"], F32)
    wgs = [load_w(P, C, "wg%d" % i, [128, 8, 1024], kview(W["w_gate"])[:, :, i * 1024:(i + 1) * 1024]) for i in range(3)]
    for i in range(3):
        C.sb("ybr%d" % i, [128, 4, TB], BF16)
    C.sb("e_sig", [128, 512], F32)
    C.sb("e_tmp", [128, 512], F32)
    C.sb("e_acc", [128, 512], F32)
    C.sb("e_mb", [128, 8, 512], BF16)
    C.sb("e_z", [128, 8, 512], F32)
    C.sb("e_x0", [128, 512], F32)
    C.sb("e_x1", [128, 512], F32)
    alloc_ln(C, "l1_", 512)
    xv = xT_in.rearrange("(m p) t -> p m t", p=128)
    ov = x1_out.rearrange("(m p) t -> p m t", p=128)
    for tb in range(NTB):
        sl = slice(tb * TB, (tb + 1) * TB)
        for i in range(3):
            P.dma("sp", T["ybr%d" % i][:], ybr[i].rearrange("(m p) t -> p m t", p=128)[:, :, sl], reads=[ybr_bufs[i]], writes=[B["ybr%d" % i]])
        for m in range(8):
            for i in range(3):
                psg, pbg = C.next_ps()
                for kd in range(8):
                    mm(P, psg[:], wgs[i][:, kd, m * 128:(m + 1) * 128], xb[:, kd, sl], kd == 0, kd == 7, [B["wg%d" % i], B["xb"]], [pbg])
                psb, pbb = C.next_ps()
                for k in range(4):
                    mm(P, psb[:], wbr[i][:, k, m * 128:(m + 1) * 128], T["ybr%d" % i][:, k, :], k == 0, k == 3, [B["wbr%d" % i], B["ybr%d" % i]], [pbb])
                v_act(P, T["e_sig"][:], psg[:], AF.Sigmoid, [pbg, B["bg"]], [B["e_sig"]], bias=bg[:, i * 8 + m:i * 8 + m + 1])
                if i == 0:
                    v_tt(P, "dve", T["e_acc"][:], psb[:], T["e_sig"][:], ALU.mult, [pbb, B["e_sig"]], [B["e_acc"]])
                else:
                    v_tt(P, "dve", T["e_tmp"][:], psb[:], T["e_sig"][:], ALU.mult, [pbb, B["e_sig"]], [B["e_tmp"]])
                    if i == 1:
                        v_tt(P, "pool", T["e_acc"][:], T["e_acc"][:], T["e_tmp"][:], ALU.add, [B["e_acc"], B["e_tmp"]], [B["e_acc"]])
                    else:
                        v_tt(P, "dve", T["e_mb"][:, m, :], T["e_acc"][:], T["e_tmp"][:], ALU.add, [B["e_acc"], B["e_tmp"]], [B["e_mb"]])
        for m in range(8):
            pso, pbo = C.next_ps()
            for k in range(8):
                mm(P, pso[:], wout[:, k, m * 128:(m + 1) * 128], T["e_mb"][:, k, :], k == 0, k == 7, [B["wout"], B["e_mb"]], [pbo])
            ex, exb = T["e_x%d" % (m % 2)], B["e_x%d" % (m % 2)]
            P.dma("sp", ex[:], xv[:, m, sl], writes=[exb])
            P.op("dve", lambda e, m=m, pso=pso, ex=ex: e.scalar_tensor_tensor(out=T["e_z"][:, m, :], in0=ex[:], scalar=ALPHA, in1=pso[:],
                                                                             op0=ALU.mult, op1=ALU.add), reads=[exb, pbo], writes=[B["e_z"]])

        def sink(m, ap, buf, sl=sl):
            P.dma("sp", ov[:, m, sl], ap, reads=[buf], writes=[x1_buf])
        emit_ln_block(P, C, T["e_z"], B["e_z"], 512, (g1, "ln1g"), (b1, "ln1b"), "l1_", sink)


def emit_moe(P, C, W, x1_in, x1_buf, x_out, xout_buf):
    T, B = C.t, C.b
    acc = C.sb("x_acc", [128, 8, NT], F32)
    x1b = C.sb("x_1b", [128, 8, NT], BF16)
    P.dma("sp", acc[:], x1_in.rearrange("(m p) t -> p m t", p=128), reads=[x1_buf], writes=[B["x_acc"]])
    P.dma("pool", x1b[:], x1_in.rearrange("(m p) t -> p m t", p=128), reads=[x1_buf], writes=[B["x_1b"]])
    for par in range(2):
        for j in range(2):
            C.sb("xg%d%d" % (par, j), [128, 8, 256], BF16)
            C.sb("xu%d%d" % (par, j), [128, 8, 256], BF16)
            C.sb("xd%d%d" % (par, j), [128, 2, 1024], BF16)

    def load_pair(pair):
        par = pair % 2
        for j in range(2):
            ex = pair * 2 + j
            P.dma("pool", T["xg%d%d" % (par, j)][:], kview(W["w_exp_gate"][ex]), writes=[B["xg%d%d" % (par, j)]])
            P.dma("pool", T["xu%d%d" % (par, j)][:], kview(W["w_exp_up"][ex]), writes=[B["xu%d%d" % (par, j)]])
            P.dma("pool", T["xd%d%d" % (par, j)][:], kview(W["w_exp_down"][ex]), writes=[B["xd%d%d" % (par, j)]])
    load_pair(0)
    load_pair(1)
    wr = load_w(P, C, "wr", [128, 8, 36], kview(W["wr"]), F32)
    br = load_w(P, C, "br", [128, 36], W["br"], F32)
    g2 = load_w(P, C, "ln2g", [128, 8], W["ln2gT"], F32)
    b2 = load_w(P, C, "ln2b", [128, 8], W["ln2bT"], F32)
    selE = load_w(P, C, "selE", [128, 32, 128], W["c_selE"].rearrange("p (e m) -> p e m", e=32))
    alloc_ln(C, "l2_", 512)
    NTT = NT // 128
    lg = C.sb("r_lg", [128, NTT, 36], F32)
    for half in range(2):
        ps, pb = C.next_ps()
        for t8 in range(8):
            tt = half * 8 + t8
            for kd in range(8):
                mm(P, ps[:, t8 * 36:(t8 + 1) * 36], acc[:, kd, tt * 128:(tt + 1) * 128], wr[:, kd, :], kd == 0, kd == 7, [B["x_acc"], B["wr"]], [pb])
        v_tt(P, "dve", lg[:, half * 8:(half + 1) * 8, :], ps[:, 0:288].rearrange("p (t c) -> p t c", c=36),
             br[:].unsqueeze(1).broadcast_to([128, 8, 36]), ALU.add, [pb, B["br"]], [B["r_lg"]])
    for n, shp in (("r_mg", [128, NTT]), ("r_goh", [128, NTT, 4]), ("r_eg", [128, NTT, 4]), ("r_pg", [128, NTT]),
                   ("r_t48", [128, NTT, 4, 8]), ("r_les", [128, NTT, 8]), ("r_m1", [128, NTT]), ("r_oh1", [128, NTT, 8]),
                   ("r_le2", [128, NTT, 8]), ("r_m2", [128, NTT]), ("r_oh2", [128, NTT, 8]), ("r_pe1", [128, NTT]),
                   ("r_pe2", [128, NTT]), ("r_w8", [128, NTT, 8]), ("r_W32", [128, NTT, 4, 8])):
        C.sb(n, shp, F32)
    e = "dve"
    lgg = lg[:, :, 0:4]
    le = lg[:, :, 4:36].rearrange("p t (g e) -> p t g e", g=4)
    s3 = [128, NTT, 4]
    s8 = [128, NTT, 8]
    s48 = [128, NTT, 4, 8]
    P.op(e, lambda en: en.tensor_reduce(out=T["r_mg"][:], in_=lgg, axis=AX.X, op=ALU.max), reads=[B["r_lg"]], writes=[B["r_mg"]])
    v_tt(P, e, T["r_goh"][:], lgg, T["r_mg"][:].unsqueeze(2).broadcast_to(s3), ALU.is_equal, [B["r_lg"], B["r_mg"]], [B["r_goh"]])
    v_tt(P, e, T["r_eg"][:], lgg, T["r_mg"][:].unsqueeze(2).broadcast_to(s3), ALU.subtract, [B["r_lg"], B["r_mg"]], [B["r_eg"]])
    v_act(P, T["r_eg"][:], T["r_eg"][:], AF.Exp, [B["r_eg"]], [B["r_eg"]])
    P.op(e, lambda en: en.reduce_sum(out=T["r_pg"][:], in_=T["r_eg"][:], axis=AX.X), reads=[B["r_eg"]], writes=[B["r_pg"]])
    P.op(e, lambda en: en.reciprocal(out=T["r_pg"][:], in_=T["r_pg"][:]), reads=[B["r_pg"]], writes=[B["r_pg"]])
    v_tt(P, e, T["r_t48"][:], le, T["r_goh"][:].unsqueeze(3).broadcast_to(s48), ALU.mult, [B["r_lg"], B["r_goh"]], [B["r_t48"]])
    P.op(e, lambda en: en.reduce_sum(out=T["r_les"][:], in_=T["r_t48"][:].rearrange("p t g e -> p t e g"), axis=AX.X),
         reads=[B["r_t48"]], writes=[B["r_les"]])
    P.op(e, lambda en: en.tensor_reduce(out=T["r_m1"][:], in_=T["r_les"][:], axis=AX.X, op=ALU.max), reads=[B["r_les"]], writes=[B["r_m1"]])
    v_tt(P, e, T["r_oh1"][:], T["r_les"][:], T["r_m1"][:].unsqueeze(2).broadcast_to(s8), ALU.is_equal, [B["r_les"], B["r_m1"]], [B["r_oh1"]])
    P.op(e, lambda en: en.scalar_tensor_tensor(out=T["r_le2"][:], in0=T["r_oh1"][:], scalar=-1.0e30, in1=T["r_les"][:], op0=ALU.mult, op1=ALU.add),
         reads=[B["r_oh1"], B["r_les"]], writes=[B["r_le2"]])
    P.op(e, lambda en: en.tensor_reduce(out=T["r_m2"][:], in_=T["r_le2"][:], axis=AX.X, op=ALU.max), reads=[B["r_le2"]], writes=[B["r_m2"]])
    v_tt(P, e, T["r_oh2"][:], T["r_le2"][:], T["r_m2"][:].unsqueeze(2).broadcast_to(s8), ALU.is_equal, [B["r_le2"], B["r_m2"]], [B["r_oh2"]])
    v_tt(P, e, T["r_pe1"][:], T["r_m2"][:], T["r_m1"][:], ALU.subtract, [B["r_m1"], B["r_m2"]], [B["r_pe1"]])
    v_act(P, T["r_pe1"][:], T["r_pe1"][:], AF.Exp, [B["r_pe1"]], [B["r_pe1"]])
    v_ts(P, e, T["r_pe1"][:], T["r_pe1"][:], 1.0, None, ALU.add, None, [B["r_pe1"]], [B["r_pe1"]])
    P.op(e, lambda en: en.reciprocal(out=T["r_pe1"][:], in_=T["r_pe1"][:]), reads=[B["r_pe1"]], writes=[B["r_pe1"]])
    v_ts(P, e, T["r_pe2"][:], T["r_pe1"][:], -1.0, 1.0, ALU.mult, ALU.add, [B["r_pe1"]], [B["r_pe2"]])
    v_tt(P, e, T["r_pe1"][:], T["r_pe1"][:], T["r_pg"][:], ALU.mult, [B["r_pe1"], B["r_pg"]], [B["r_pe1"]])
    v_tt(P, e, T["r_pe2"][:], T["r_pe2"][:], T["r_pg"][:], ALU.mult, [B["r_pe2"], B["r_pg"]], [B["r_pe2"]])
    v_tt(P, e, T["r_w8"][:], T["r_oh1"][:], T["r_pe1"][:].unsqueeze(2).broadcast_to(s8), ALU.mult, [B["r_oh1"], B["r_pe1"]], [B["r_w8"]])
    v_tt(P, e, T["r_oh2"][:], T["r_oh2"][:], T["r_pe2"][:].unsqueeze(2).broadcast_to(s8), ALU.mult, [B["r_oh2"], B["r_pe2"]], [B["r_oh2"]])
    v_tt(P, e, T["r_w8"][:], T["r_w8"][:], T["r_oh2"][:], ALU.add, [B["r_w8"], B["r_oh2"]], [B["r_w8"]])
    v_tt(P, e, T["r_W32"][:], T["r_goh"][:].unsqueeze(3).broadcast_to(s48), T["r_w8"][:].unsqueeze(2).broadcast_to(s48), ALU.mult,
         [B["r_goh"], B["r_w8"]], [B["r_W32"]])
    wT = C.sb("r_wT", [128, NT], BF16)
    P.op("dve", lambda en: en.memset(wT[:], 0.0), writes=[B["r_wT"]])
    for q in range(NTT // 4):
        ps, pb = C.next_ps()
        for j in range(4):
            tt = q * 4 + j
            mm(P, ps[0:32, j * 128:(j + 1) * 128], T["r_W32"][:, tt].rearrange("p g e -> p (g e)"), T["c_ident"][:], True, True,
               [B["r_W32"], B["c_ident"]], [pb])
        v_cp(P, "dve", wT[0:32, q * 512:(q + 1) * 512], ps[0:32, :], [pb], [B["r_wT"]])
    NB = 256
    for n in ("x_sg", "x_h"):
        C.sb(n, [128, 2, NB], F32)
    for j in range(2):
        C.sb("x_h2%d" % j, [128, 2, NB], BF16)
    for j in range(2):
        C.sb("x_ev%d" % j, [128, 2, NB], F32)
    C.ps_lim = 4
    C.ps_rr = 0
    ev_rr = 0
    for pair in range(16):
        par = pair % 2
        for tb in range(NT // NB):
            sl = slice(tb * NB, (tb + 1) * NB)
            for j in range(2):
                ex = pair * 2 + j
                wg_, wu_ = T["xg%d%d" % (par, j)], T["xu%d%d" % (par, j)]
                psg, pbg = C.next_ps()
                psu, pbu = C.next_ps()
                psw, pbw = C.next_ps()
                for f in range(2):
                    for kd in range(8):
                        mm(P, psg[:, f * NB:(f + 1) * NB], wg_[:, kd, f * 128:(f + 1) * 128], x1b[:, kd, sl], kd == 0, kd == 7,
                           [B["xg%d%d" % (par, j)], B["x_1b"]], [pbg])
                for f in range(2):
                    for kd in range(8):
                        mm(P, psu[:, f * NB:(f + 1) * NB], wu_[:, kd, f * 128:(f + 1) * 128], x1b[:, kd, sl], kd == 0, kd == 7,
                           [B["xu%d%d" % (par, j)], B["x_1b"]], [pbu])
                mm(P, psw[:, 0:NB], selE[:, ex, :], wT[:, sl], True, True, [B["selE"], B["r_wT"]], [pbw])
                v_act(P, T["x_sg"][:].rearrange("p f n -> p (f n)"), psg[:], AF.Silu, [pbg], [B["x_sg"]])
                v_tt(P, "dve", T["x_h"][:].rearrange("p f n -> p (f n)"), T["x_sg"][:].rearrange("p f n -> p (f n)"), psu[:], ALU.mult,
                     [B["x_sg"], pbu], [B["x_h"]])
                v_tt(P, "dve", T["x_h2%d" % j][:], T["x_h"][:], psw[:, 0:NB].unsqueeze(1).broadcast_to([128, 2, NB]), ALU.mult,
                     [B["x_h"], pbw], [B["x_h2%d" % j]])
            for m in range(8):
                psa, pba = C.ps[4 + m // 2], C.psb[4 + m // 2]
                o = psa[:, (m % 2) * NB:(m % 2 + 1) * NB]
                for j in range(2):
                    for f in range(2):
                        mm(P, o, T["xd%d%d" % (par, j)][:, f, m * 128:(m + 1) * 128], T["x_h2%d" % j][:, f, :], j == 0 and f == 0, j == 1 and f == 1,
                           [B["xd%d%d" % (par, j)], B["x_h2%d" % j]], [pba])
            for bk in range(4):
                psa, pba = C.ps[4 + bk], C.psb[4 + bk]
                pv = psa[:].rearrange("p (f n) -> p f n", f=2)
                av = acc[:, 2 * bk:2 * bk + 2, sl]
                if pair == 0:
                    P.op("dve", lambda e, av=av, pv=pv: e.scalar_tensor_tensor(out=av, in0=av, scalar=ALPHA, in1=pv, op0=ALU.mult, op1=ALU.add),
                         reads=[B["x_acc"], pba], writes=[B["x_acc"]])
                else:
                    v_tt(P, "dve", av, av, pv, ALU.add, [B["x_acc"], pba], [B["x_acc"]])
        if pair + 2 < 16:
            load_pair(pair + 2)
    C.ps_lim = 8
    ov = x_out.rearrange("(m p) t -> p m t", p=128)
    for tb in range(NTB):
        sl = slice(tb * TB, (tb + 1) * TB)

        def sink(m, ap, buf, sl=sl):
            P.dma("sp", ov[:, m, sl], ap, reads=[buf], writes=[xout_buf])
        emit_ln_block(P, C, acc[:, :, sl], B["x_acc"], 512, (g2, "ln2g"), (b2, "ln2b"), "l2_", sink)


def host_layer_weights(inp, l):
    f = np.float32
    c = np.ascontiguousarray
    W = {}
    W["w_in"] = c(inp["w_in"][l])
    W["w_gate"] = c(inp["w_gate"][l])
    W["bgT"] = c(inp["b_gate"][l].reshape(24, 128).T)
    W["w_glu"] = c(inp["w_glu"][l])
    W["bglu"] = c(inp["b_glu"][l].reshape(4, 128).T)
    W["wsT"] = c(np.transpose(inp["w_spatial"][l], (2, 0, 1)).reshape(128, 1024))
    W["lng"] = c(np.broadcast_to(inp["gmlp_ln_g"][l][None, :], (128, 512)))
    W["lnb"] = c(np.broadcast_to(inp["gmlp_ln_b"][l][None, :], (128, 512)))
    W["bsT"] = c(inp["b_spatial"][l].T)
    W["w_kv"] = c(inp["w_kv"][l])
    W["w_br"] = c(inp["w_br"][l])
    W["w_out"] = c(inp["w_out"][l])
    W["ln1gT"] = c(inp["ln1_g"][l].reshape(8, 128).T)
    W["ln1bT"] = c(inp["ln1_b"][l].reshape(8, 128).T)
    W["wr"] = c(np.concatenate([inp["w_router_g"][l], np.transpose(inp["w_router_e"][l], (1, 0, 2)).reshape(D, 32)], axis=1))
    brow = np.concatenate([inp["b_router_g"][l], inp["b_router_e"][l].reshape(32)])
    W["br"] = c(np.broadcast_to(brow[None, :], (128, 36)))
    W["w_exp_gate"] = c(inp["w_exp_gate"][l])
    W["w_exp_up"] = c(inp["w_exp_up"][l])
    W["w_exp_down"] = c(inp["w_exp_down"][l])
    W["ln2gT"] = c(inp["ln2_g"][l].reshape(8, 128).T)
    W["ln2bT"] = c(inp["ln2_b"][l].reshape(8, 128).T)
    for k, v in host_s5_params(inp, l).items():
        W["s5_" + k] = v
    return {k: np.asarray(v, dtype=f) for k, v in W.items()}


def host_const_all():
    cst = host_consts()
    selE = np.zeros((128, 32, 128), np.float32)
    for e_ in range(32):
        selE[e_, e_, :] = 1.0
    cst["c_selE"] = np.ascontiguousarray(selE.reshape(128, 32 * 128))
    return cst


W_SHAPES = {
    "w_in": [1024, 2048], "w_gate": [1024, 3072], "bgT": [128, 24], "w_glu": [512, 512], "bglu": [128, 4], "wsT": [128, 1024],
    "lng": [128, 512], "lnb": [128, 512], "bsT": [128, 8], "w_kv": [1024, 1024], "w_br": [3, 512, 1024], "w_out": [1024, 1024],
    "ln1gT": [128, 8], "ln1bT": [128, 8], "wr": [1024, 36], "br": [128, 36], "w_exp_gate": [32, 1024, 256], "w_exp_up": [32, 1024, 256],
    "w_exp_down": [32, 256, 1024], "ln2gT": [128, 8], "ln2bT": [128, 8],
    "s5_lam_re": [128, 32], "s5_lam_im": [128, 32], "s5_lstep": [128, 32], "s5_dm": [128, 32],
    "s5_b_re": [128, 512], "s5_b_im": [128, 512], "s5_c_re": [128, 512], "s5_c_im": [128, 512],
}
A_KEYS = ["w_in", "s5_lam_re", "s5_lam_im", "s5_lstep", "s5_dm", "s5_b_re", "s5_b_im", "s5_c_re", "s5_c_im"]
CONST_SHAPES = {"c_ident": [128, 128], "c_wide": [128, 1920], "c_maskF": [128, 128], "c_maskB": [128, 128], "c_kvec": [128, NK],
                "c_selE": [128, 4096]}


S5_TABLES = (("w_Msum", 32 * 128), ("w_Wl", 32 * 2 * 128), ("w_Glre", 32 * 8 * 16), ("w_GlimN", 32 * 8 * 16), ("q_A2", 128), ("q_P", 192))
S5_FWD = S5_TABLES + (("X8", 32 * NCH), ("S", NCH * 64))


def _flat(ap, name):
    if name in ("w_Msum",):
        return ap.rearrange("p g m -> p (g m)")
    if name == "w_Wl":
        return ap.rearrange("p g r m -> p (g r m)")
    if name in ("w_Glre", "w_GlimN"):
        return ap.rearrange("p g j h -> p (g j h)")
    if name == "q_A2":
        return ap.rearrange("p a r g -> p (a r g)")
    return ap.rearrange("p m r g -> p (m r g)")


def emit_s5_stage(P, C, W, xT, pred, lout, ybr0, ybr0_buf, stop_after_scan, tabs=None):
    T, B = C.t, C.b
    C.push()
    alloc_s5_persist(C)
    if stop_after_scan or tabs is None:
        C.push()
        alloc_s5_prep(C)
        emit_s5_prep(P, C, {k[3:]: W[k] for k in W if k.startswith("s5_")})
        C.pop()
        if tabs is not None:
            for n_, _w in S5_TABLES:
                P.dma("pool", tabs[n_], _flat(T[n_][:], n_), reads=[B[n_]], writes=[Buf("tab_" + n_)])
    else:
        for n_, _w in S5_TABLES:
            P.dma("pool", _flat(T[n_][:], n_), tabs[n_], writes=[B[n_]])
    alloc_s5_main(C)
    Hs = T["a_Hs"]
    if pred is None:
        P.op("dve", lambda e: e.memset(Hs[:, 0], 0.0), writes=[B["a_Hs"]])
    else:
        pr = C.sb("a_pr", [128, 3, 2, 32], F32)
        P.dma("sp", pr[:], pred.rearrange("m p (r g) -> p m r g", r=2), writes=[B["a_pr"]])
        st = T["a_st"]
        v_cp(P, "dve", Hs[:, 0], pr[:, 0], [B["a_pr"]], [B["a_Hs"]])
        Pm = T["q_P"]
        for m in range(2):
            cmul(P, "dve", (st[:, 0, 0], B["a_st"]), (st[:, 0, 1], B["a_st"]), (Pm[:, m, 0], B["q_P"]), (Pm[:, m, 1], B["q_P"]),
                 (pr[:, m + 1, 0], B["a_pr"]), (pr[:, m + 1, 1], B["a_pr"]), (st[:, 1, 0], B["a_st"]))
            v_tt(P, "dve", Hs[:, 0], Hs[:, 0], st[:, 0], ALU.add, [B["a_Hs"], B["a_st"]], [B["a_Hs"]])
    C.b_st2 = Buf("st2")
    if stop_after_scan or tabs is None:
        C.push()
        xb = load_w(P, C, "xb", [128, 8, NT], kview(xT))
        win = load_w(P, C, "win", [128, 8, 512], kview(W["w_in"])[:, :, 0:512])
        emit_s5_front(P, C, xb, B["xb"], win, B["win"])
        C.pop()
        emit_s5_mid(P, C, None, tabs)
    else:
        emit_s5_mid(P, C, tabs, None)
    if stop_after_scan:
        P.dma("sp", lout, Hs[:, NCH].rearrange("p r g -> p (r g)"), reads=[B["a_Hs"]], writes=[Buf("lout")])
        C.pop()
        return
    alloc_gelu(C)
    wglu = load_w(P, C, "wglu", [128, 4, 512], kview(W["w_glu"]))
    bglu = load_w(P, C, "bglu", [128, 4], W["bglu"], F32)
    emit_s5_back(P, C, wglu, B["wglu"], bglu, B["bglu"])
    P.dma("sp", ybr0.rearrange("(m p) t -> p m t", p=128), T["a_U"][:], reads=[B["a_U"]], writes=[ybr0_buf])
    C.pop()


def build_program(mode, stages=("s5", "gmlp", "attn", "merge", "moe")):
    nc = bass.Bass("TRN2", target_bir_lowering=False)

    def din(n, shape):
        return nc.dram_tensor(n, list(shape), F32, kind="ExternalInput").ap()
    xT = din("xT", [D, NT])
    cst = {k: din(k, v) for k, v in CONST_SHAPES.items()}
    keys = A_KEYS if mode == "A" else list(W_SHAPES.keys())
    W = {k: din(k, W_SHAPES[k]) for k in keys}
    W["c_selE"] = cst["c_selE"]
    P = Prog(nc)
    C = Ctx(P)
    C.alloc_psum()
    load_consts(P, C, cst)
    T, B = C.t, C.b
    if mode == "A":
        lout = nc.dram_tensor("lout", [128, 64], F32, kind="ExternalOutput").ap()
        tabs = {n_: nc.dram_tensor("tab_" + n_, [128, w_], F32, kind="ExternalOutput").ap() for n_, w_ in S5_FWD}
        emit_s5_stage(P, C, W, xT, None, lout, None, None, True, tabs)
        P.finish()
        return nc
    memT = din("memT", [D, 256])
    pred = din("pred", [3, 128, 64])
    tabs = {n_: din("tab_" + n_, [128, w_]) for n_, w_ in S5_FWD}
    xout = nc.dram_tensor("xout", [D, NT], F32, kind="ExternalOutput").ap()
    ybr = [nc.dram_tensor("ybr%d" % i, [512, NT], BF16).ap() for i in range(3)]
    ybr_b = [Buf("ybr%d" % i) for i in range(3)]
    x1T = nc.dram_tensor("x1T", [D, NT], F32).ap()
    x1_b = Buf("x1T")
    xo_b = Buf("xout")
    if "s5" in stages:
        emit_s5_stage(P, C, W, xT, pred, None, ybr[0], ybr_b[0], False, tabs)
    if "gmlp" in stages:
        C.push()
        xb = load_w(P, C, "xb", [128, 8, NT], kview(xT))
        alloc_gelu(C)
        emit_gmlp(P, C, xb, W, ybr[1], ybr_b[1])
        C.pop()
    if "attn" in stages:
        C.push()
        xb = load_w(P, C, "xb", [128, 8, NT], kview(xT))
        emit_memattn(P, C, xb, W, memT, ybr[2], ybr_b[2])
        C.pop()
    if "merge" in stages:
        C.push()
        xb = load_w(P, C, "xb", [128, 8, NT], kview(xT))
        emit_merge(P, C, xb, W, xT, ybr, ybr_b, x1T, x1_b)
        C.pop()
    if "moe" in stages:
        C.push()
        emit_moe(P, C, W, x1T, x1_b, xout, xo_b)
        C.pop()
    print("n_ops", P.n_ops)
    P.finish()
    return nc


_PROGS = {}


def _prog(mode):
    if mode not in _PROGS:
        _PROGS[mode] = build_program(mode)
    return _PROGS[mode]


def kernel(**inputs):
    inp = {k: np.asarray(v) for k, v in inputs.items()}
    x = inp["x"].astype(np.float32)
    mem = inp["mem"].astype(np.float32)
    n = 8
    cst = host_const_all()
    xT = []
    memT = []
    for c_ in range(n):
        b, j = c_ // 4, c_ % 4
        xT.append(np.ascontiguousarray(x[b, j * NT:(j + 1) * NT].T))
        memT.append(np.ascontiguousarray(mem[b].T))
    for l in range(DEPTH):
        Wl = host_layer_weights(inp, l)
        ncA = _prog("A")
        mapsA = []
        for c_ in range(n):
            m_ = {"xT": xT[c_]}
            m_.update(cst)
            m_.update({k: Wl[k] for k in A_KEYS})
            mapsA.append(m_)
        resA = run_bass_kernel_spmd(ncA, mapsA, core_ids=list(range(n)))
        lo = [np.asarray(r["lout"], dtype=np.float32) for r in resA.results]
        ncB = _prog("B")
        mapsB = []
        for c_ in range(n):
            b, j = c_ // 4, c_ % 4
            pred = np.zeros((3, 128, 64), np.float32)
            for m in range(3):
                if j - 1 - m >= 0:
                    pred[m, 0:64] = lo[b * 4 + j - 1 - m][0:64]
                if j + 1 + m <= 3:
                    pred[m, 64:128] = lo[b * 4 + j + 1 + m][64:128]
            m_ = {"xT": xT[c_], "memT": memT[c_], "pred": pred}
            for n_, _w in S5_FWD:
                m_["tab_" + n_] = np.asarray(resA.results[c_]["tab_" + n_], dtype=np.float32)
            m_.update(cst)
            m_.update(Wl)
            mapsB.append(m_)
        resB = run_bass_kernel_spmd(ncB, mapsB, core_ids=list(range(n)))
        xT = [np.asarray(r["xout"], dtype=np.float32) for r in resB.results]
    out = np.zeros((2, 8192, D), np.float32)
    for c_ in range(n):
        b, j = c_ // 4, c_ % 4
        out[b, j * NT:(j + 1) * NT] = xT[c_].T
    return out


def prog_coll(P, kind, ins, outs, reads=(), writes=()):
    eng = "pool"
    waits = P._waits(eng, reads, writes)
    key = P.dma_keys[eng][P.dma_rr[eng]]
    P.dma_rr[eng] = (P.dma_rr[eng] + 1) % len(P.dma_keys[eng])
    prev = P.cnt[key]
    if prev > 0 and P.seen[eng].get(key, 0) < prev:
        P.seen[eng][key] = prev
        waits.append((key, prev))
    P.cnt[key] += 16
    tick = (key, P.cnt[key])
    kk = dict(op=ALU.bypass, replica_groups=[list(range(8))], ins=ins, outs=outs)
    P.q[eng].append((waits, ("collective_compute", (kind,), kk), key, 16))
    P._commit(tick, reads, writes)
    P.n_ops += 1
```
